# Optimizing a Trainium2 kernel written in Bass

```python
import jax, jax.numpy as jnp
from jax import lax
import numpy as np

D_MODEL = 2048
BATCH = 8
SEQ = 2048
DEPTH = 1

GM_WIDTH = D_MODEL
GM_GROUPS = 16
GM_GROUP_DIM = GM_WIDTH // GM_GROUPS
GM_CHUNK = 128

NSA_HEADS = 16
NSA_KV_HEADS = 4
NSA_GROUP = NSA_HEADS // NSA_KV_HEADS
HEAD_DIM = D_MODEL // NSA_HEADS
NSA_BRANCHES = 3
CMP_BLOCK = 32
CMP_STRIDE = 16
CMP_HIDDEN = 256
SLC_BLOCK = 64
N_SELECT = 16
WINDOW = 512
WIN_BLOCK = 128
SLC_QUERY_CHUNK = 16
ROPE_THETA = 10000.0

PEER_HEADS = 8
PEER_QUERY_DIM = 256
PEER_N_KEYS = 128
PEER_N_EXPERTS = PEER_N_KEYS * PEER_N_KEYS
PEER_TOPK = 16
PEER_TOKEN_BLOCK = 128

EPS = 1e-6
NEG_BIG = -1e30
POS_BIG = 1e30

IN_SIZES = (GM_WIDTH, GM_WIDTH, NSA_HEADS * HEAD_DIM, 6 * NSA_KV_HEADS * HEAD_DIM, NSA_HEADS * NSA_BRANCHES, 2 * D_MODEL)
IN_TOTAL = 2 * GM_WIDTH + NSA_HEADS * HEAD_DIM + 6 * NSA_KV_HEADS * HEAD_DIM + NSA_HEADS * NSA_BRANCHES + 2 * D_MODEL

kernel_name = 'hybrid_gmlp_nsa_peer_block'


def rms_norm(x, g):
    xf = x.astype(jnp.float32)
    y = xf * lax.rsqrt(jnp.mean(xf * xf, axis=-1, keepdims=True) + EPS)
    return (y * g.astype(jnp.float32)).astype(x.dtype)


def layer_norm(x, g, b):
    xf = x.astype(jnp.float32)
    mu = jnp.mean(xf, axis=-1, keepdims=True)
    xc = xf - mu
    y = xc * lax.rsqrt(jnp.mean(xc * xc, axis=-1, keepdims=True) + EPS)
    return (y * g.astype(jnp.float32) + b.astype(jnp.float32)).astype(x.dtype)


def masked_softmax(s, mask, axis=-1):
    s = jnp.where(mask, s.astype(jnp.float32), NEG_BIG)
    p = jax.nn.softmax(s, axis=axis)
    return jnp.where(jnp.any(mask, axis=axis, keepdims=True), p, 0.0)


def rotary(x, pos):
    half = HEAD_DIM // 2
    inv = ROPE_THETA ** (-jnp.arange(half, dtype=jnp.float32) / half)
    ang = pos.astype(jnp.float32)[:, None] * inv[None, :]
    shp = (1, x.shape[1]) + (1,) * (x.ndim - 3) + (half,)
    cos = jnp.cos(ang).reshape(shp)
    sin = jnp.sin(ang).reshape(shp)
    xf = x.astype(jnp.float32)
    x1, x2 = xf[..., :half], xf[..., half:]
    return jnp.concatenate([x1 * cos - x2 * sin, x2 * cos + x1 * sin], axis=-1).astype(x.dtype)


def gmlp_branch(u, v, ln_g, ln_b, w_s, b_s):
    B, S, _ = u.shape
    u = jax.nn.gelu(u)
    v = layer_norm(jax.nn.gelu(v), ln_g, ln_b)
    v = v.reshape(B, S // GM_CHUNK, GM_CHUNK, GM_GROUPS, GM_GROUP_DIM)
    causal = jnp.tril(jnp.ones((GM_CHUNK, GM_CHUNK), dtype=bool))
    w = jnp.where(causal[None], w_s, 0.0)
    z = jnp.einsum('gts,bcsgd->bctgd', w, v) + b_s.T[None, None, :, :, None]
    return u * z.reshape(B, S, GM_WIDTH)


def compress_blocks(k, pos_emb, w1, w2):
    B, S = k.shape[0], k.shape[1]
    n_cmp = (S - CMP_BLOCK) // CMP_STRIDE + 1
    idx = jnp.arange(n_cmp)[:, None] * CMP_STRIDE + jnp.arange(CMP_BLOCK)[None, :]
    blocks = k[:, idx] + pos_emb[None, None, :, None, :]
    flat = jnp.moveaxis(blocks, 3, 2).reshape(B, n_cmp, NSA_KV_HEADS, CMP_BLOCK * HEAD_DIM)
    return jax.nn.gelu(flat @ w1) @ w2


def select_blocks(p_cmp, pos):
    B, S = p_cmp.shape[0], p_cmp.shape[3]
    n_cmp = p_cmp.shape[4]
    n_slc = S // SLC_BLOCK
    ratio = SLC_BLOCK // CMP_STRIDE
    span = CMP_BLOCK // CMP_STRIDE
    imp = jnp.sum(p_cmp, axis=2)
    right = max(0, ratio * n_slc - n_cmp)
    imp_pad = jnp.pad(imp, ((0, 0), (0, 0), (0, 0), (span - 1, right)))
    agg_w = jnp.asarray(np.convolve(np.ones(ratio), np.ones(span)), dtype=jnp.float32)
    gidx = ratio * jnp.arange(n_slc)[:, None] + jnp.arange(ratio + span - 1)[None, :]
    imp_slc = jnp.einsum('bhsjw,w->bhsj', imp_pad[..., gidx], agg_w)
    blk = jnp.arange(n_slc)
    allowed = (blk * SLC_BLOCK)[None, :] <= pos[:, None]
    forced = (blk[None, :] == 0) | (blk[None, :] == (pos // SLC_BLOCK)[:, None])
    score = jnp.where(forced, POS_BIG, jnp.where(allowed, imp_slc, NEG_BIG))
    _, sel = lax.top_k(score, min(N_SELECT, n_slc))
    return sel


def selected_attention(q_rot, k, v, sel, pos):
    B, S = q_rot.shape[0], q_rot.shape[1]
    n_slc = S // SLC_BLOCK
    n_sel = sel.shape[-1]
    n_chunks = S // SLC_QUERY_CHUNK
    scale = HEAD_DIM ** -0.5
    k_blk = k.reshape(B, n_slc, SLC_BLOCK, NSA_KV_HEADS, HEAD_DIM).transpose(0, 3, 1, 2, 4)
    v_blk = v.reshape(B, n_slc, SLC_BLOCK, NSA_KV_HEADS, HEAD_DIM).transpose(0, 3, 1, 2, 4)
    q_c = q_rot.reshape(B, n_chunks, SLC_QUERY_CHUNK, NSA_KV_HEADS, NSA_GROUP, HEAD_DIM).swapaxes(0, 1)
    sel_c = sel.transpose(0, 2, 1, 3).reshape(B, n_chunks, SLC_QUERY_CHUNK, NSA_KV_HEADS, n_sel).swapaxes(0, 1)
    pos_c = pos.reshape(n_chunks, SLC_QUERY_CHUNK)
    b_ar = jnp.arange(B)[:, None, None, None]
    h_ar = jnp.arange(NSA_KV_HEADS)[None, None, :, None]

    def attend(args):
        qc, ic, pc = args
        kg = k_blk[b_ar, h_ar, ic]
        vg = v_blk[b_ar, h_ar, ic]
        s = jnp.einsum('bthgd,bthnkd->bthgnk', qc, kg) * scale
        tokpos = ic[..., None] * SLC_BLOCK + jnp.arange(SLC_BLOCK)
        mask = (tokpos <= pc[None, :, None, None, None])[:, :, :, None]
        Bq, Tq = qc.shape[0], qc.shape[1]
        p = masked_softmax(s.reshape(Bq, Tq, NSA_KV_HEADS, NSA_GROUP, n_sel * SLC_BLOCK),
                           mask.reshape(Bq, Tq, NSA_KV_HEADS, 1, n_sel * SLC_BLOCK))
        p = p.reshape(s.shape).astype(vg.dtype)
        return jnp.einsum('bthgnk,bthnkd->bthgd', p, vg)

    o = lax.map(attend, (q_c, sel_c, pos_c))
    return o.swapaxes(0, 1).reshape(B, S, NSA_KV_HEADS, NSA_GROUP, HEAD_DIM)


def window_attention(q_rot, k, v):
    B, S = q_rot.shape[0], q_rot.shape[1]
    nb = S // WIN_BLOCK
    n_prev = WINDOW // WIN_BLOCK
    scale = HEAD_DIM ** -0.5

    def band(t):
        t_pad = jnp.pad(t, ((0, 0), (WINDOW, 0), (0, 0), (0, 0)))
        t_pad = t_pad.reshape(B, nb + n_prev, WIN_BLOCK, NSA_KV_HEADS, HEAD_DIM)
        return jnp.concatenate([t_pad[:, i:i + nb] for i in range(n_prev + 1)], axis=2)

    kb, vb = band(k), band(v)
    qb = q_rot.reshape(B, nb, WIN_BLOCK, NSA_KV_HEADS, NSA_GROUP, HEAD_DIM)
    s = jnp.einsum('bcqhgd,bckhd->bchgqk', qb, kb) * scale
    qpos = jnp.arange(S).reshape(nb, WIN_BLOCK)
    kpos = jnp.arange(nb)[:, None] * WIN_BLOCK - WINDOW + jnp.arange((n_prev + 1) * WIN_BLOCK)[None, :]
    diff = qpos[:, :, None] - kpos[:, None, :]
    mask = (diff >= 0) & (diff < WINDOW) & (kpos[:, None, :] >= 0)
    p = masked_softmax(s, mask[None, :, None, None]).astype(vb.dtype)
    o = jnp.einsum('bchgqk,bckhd->bcqhgd', p, vb)
    return o.reshape(B, S, NSA_KV_HEADS, NSA_GROUP, HEAD_DIM)


def nsa_branch(q, kv, gate_logits, cmp_pos_k, cmp_w1_k, cmp_w2_k, cmp_pos_v, cmp_w1_v, cmp_w2_v):
    B, S, _ = q.shape
    pos = jnp.arange(S)
    scale = HEAD_DIM ** -0.5
    q = q.reshape(B, S, NSA_KV_HEADS, NSA_GROUP, HEAD_DIM)
    kv = kv.reshape(B, S, 6, NSA_KV_HEADS, HEAD_DIM)
    k_cmp, v_cmp, k_slc, v_slc, k_win, v_win = (kv[:, :, i] for i in range(6))
    q_rot = rotary(q, pos)
    k_slc = rotary(k_slc, pos)
    k_win = rotary(k_win, pos)

    kc = compress_blocks(k_cmp, cmp_pos_k, cmp_w1_k, cmp_w2_k)
    vc = compress_blocks(v_cmp, cmp_pos_v, cmp_w1_v, cmp_w2_v)
    n_cmp = kc.shape[1]
    s = jnp.einsum('bshgd,bnhd->bhgsn', q, kc) * scale
    cmp_end = jnp.arange(n_cmp) * CMP_STRIDE + CMP_BLOCK - 1
    p_cmp = masked_softmax(s, cmp_end[None, :] <= pos[:, None])
    o_cmp = jnp.einsum('bhgsn,bnhd->bshgd', p_cmp.astype(vc.dtype), vc)

    sel = select_blocks(p_cmp, pos)
    o_slc = selected_attention(q_rot, k_slc, v_slc, sel, pos)

    o_win = window_attention(q_rot, k_win, v_win)

    g = jax.nn.sigmoid(gate_logits).reshape(B, S, NSA_KV_HEADS, NSA_GROUP, NSA_BRANCHES)
    o = g[..., 0:1] * o_cmp + g[..., 1:2] * o_slc + g[..., 2:3] * o_win
    return o.reshape(B, S, NSA_HEADS * HEAD_DIM)


def peer(h, w_q, keys1, keys2, u_tab, v_tab):
    B, S, D = h.shape
    T = B * S
    ht = h.reshape(T, D)
    q = (ht @ w_q).reshape(T, PEER_HEADS, 2, PEER_QUERY_DIM // 2)
    s1 = jnp.einsum('thd,kd->thk', q[:, :, 0], keys1)
    s2 = jnp.einsum('thd,kd->thk', q[:, :, 1], keys2)
    v1, i1 = lax.top_k(s1, PEER_TOPK)
    v2, i2 = lax.top_k(s2, PEER_TOPK)
    cand = (v1[..., :, None] + v2[..., None, :]).reshape(T, PEER_HEADS, PEER_TOPK * PEER_TOPK)
    cand_id = (i1[..., :, None] * PEER_N_KEYS + i2[..., None, :]).reshape(T, PEER_HEADS, PEER_TOPK * PEER_TOPK)
    top_s, top_pos = lax.top_k(cand, PEER_TOPK)
    experts = jnp.take_along_axis(cand_id, top_pos, axis=-1)
    gate = jax.nn.softmax(top_s.astype(jnp.float32), axis=-1).astype(h.dtype)
    nblk = T // PEER_TOKEN_BLOCK

    def apply(args):
        xb, eb, gb = args
        ue = u_tab[eb]
        act = jax.nn.gelu(jnp.einsum('td,thkd->thk', xb, ue))
        ve = v_tab[eb]
        return jnp.einsum('thk,thkd->td', gb * act, ve)

    out = lax.map(apply, (ht.reshape(nblk, PEER_TOKEN_BLOCK, D),
                          experts.reshape(nblk, PEER_TOKEN_BLOCK, PEER_HEADS, PEER_TOPK),
                          gate.reshape(nblk, PEER_TOKEN_BLOCK, PEER_HEADS, PEER_TOPK)))
    return out.reshape(B, S, D)


def setup_inputs(seed: int = 0) -> dict:
    key = jax.random.key(seed)
    ks = jax.random.split(key, 24)
    f32 = jnp.float32
    L = DEPTH

    def nrm(k, shape, scale):
        return jax.random.normal(k, shape, dtype=f32) * scale

    return {
        'x': nrm(ks[0], (BATCH, SEQ, D_MODEL), 1.0),
        'norm_mix_g': 1.0 + nrm(ks[1], (L, D_MODEL), 0.01),
        'w_in': nrm(ks[2], (L, D_MODEL, IN_TOTAL), D_MODEL ** -0.5),
        'w_out': nrm(ks[3], (L, D_MODEL, D_MODEL), D_MODEL ** -0.5),
        'gm_ln_g': 1.0 + nrm(ks[4], (L, GM_WIDTH), 0.01),
        'gm_ln_b': nrm(ks[5], (L, GM_WIDTH), 0.01),
        'gm_spatial_w': nrm(ks[6], (L, GM_GROUPS, GM_CHUNK, GM_CHUNK), 0.5 * GM_CHUNK ** -0.5),
        'gm_spatial_b': 1.0 + nrm(ks[7], (L, GM_GROUPS, GM_CHUNK), 0.01),
        'cmp_pos_k': nrm(ks[8], (L, CMP_BLOCK, HEAD_DIM), 0.02),
        'cmp_w1_k': nrm(ks[9], (L, CMP_BLOCK * HEAD_DIM, CMP_HIDDEN), (CMP_BLOCK * HEAD_DIM) ** -0.5),
        'cmp_w2_k': nrm(ks[10], (L, CMP_HIDDEN, HEAD_DIM), CMP_HIDDEN ** -0.5),
        'cmp_pos_v': nrm(ks[11], (L, CMP_BLOCK, HEAD_DIM), 0.02),
        'cmp_w1_v': nrm(ks[12], (L, CMP_BLOCK * HEAD_DIM, CMP_HIDDEN), (CMP_BLOCK * HEAD_DIM) ** -0.5),
        'cmp_w2_v': nrm(ks[13], (L, CMP_HIDDEN, HEAD_DIM), CMP_HIDDEN ** -0.5),
        'norm_ffn_g': 1.0 + nrm(ks[14], (L, D_MODEL), 0.01),
        'peer_w_q': nrm(ks[15], (L, D_MODEL, PEER_HEADS * PEER_QUERY_DIM), D_MODEL ** -0.5),
        'peer_keys1': nrm(ks[16], (L, PEER_N_KEYS, PEER_QUERY_DIM // 2), (PEER_QUERY_DIM // 2) ** -0.5),
        'peer_keys2': nrm(ks[17], (L, PEER_N_KEYS, PEER_QUERY_DIM // 2), (PEER_QUERY_DIM // 2) ** -0.5),
        'peer_u': nrm(ks[18], (L, PEER_N_EXPERTS, D_MODEL), D_MODEL ** -0.5),
        'peer_v': nrm(ks[19], (L, PEER_N_EXPERTS, D_MODEL), PEER_HEADS ** -0.5),
        'norm_final_g': 1.0 + nrm(ks[20], (D_MODEL,), 0.01),
    }


def reference(x, norm_mix_g, w_in, w_out, gm_ln_g, gm_ln_b, gm_spatial_w, gm_spatial_b,
              cmp_pos_k, cmp_w1_k, cmp_w2_k, cmp_pos_v, cmp_w1_v, cmp_w2_v,
              norm_ffn_g, peer_w_q, peer_keys1, peer_keys2, peer_u, peer_v, norm_final_g):
    splits = [int(c) for c in np.cumsum(IN_SIZES)[:-1]]
    for l in range(DEPTH):
        h = rms_norm(x, norm_mix_g[l])
        proj = h @ w_in[l]
        u, v, q, kv, nsa_gate, merge_gate = jnp.split(proj, splits, axis=-1)
        o_a = gmlp_branch(u, v, gm_ln_g[l], gm_ln_b[l], gm_spatial_w[l], gm_spatial_b[l])
        o_b = nsa_branch(q, kv, nsa_gate, cmp_pos_k[l], cmp_w1_k[l], cmp_w2_k[l],
                         cmp_pos_v[l], cmp_w1_v[l], cmp_w2_v[l])
        g_a, g_b = jnp.split(jax.nn.sigmoid(merge_gate), 2, axis=-1)
        x = x + (g_a * o_a + g_b * o_b) @ w_out[l]
        x = x + peer(rms_norm(x, norm_ffn_g[l]), peer_w_q[l], peer_keys1[l], peer_keys2[l], peer_u[l], peer_v[l])
    return rms_norm(x, norm_final_g)
```

```python
import numpy as np
import concourse.bass as bass
import concourse.mybir as mybir
from concourse.bass_utils import run_bass_kernel_spmd

F32 = mybir.dt.float32
BF16 = mybir.dt.bfloat16
I32 = mybir.dt.int32
U32 = mybir.dt.uint32
ALU = mybir.AluOpType
AF = mybir.ActivationFunctionType
AX = mybir.AxisListType

ENGS = ("pe", "act", "dve", "pool", "sp")
EPOCH = 30000
NDMASEM = 8
SB_BASE = 16512
SB_TOP = 229344


class Op:
    __slots__ = ("eng", "fn", "deps", "is_dma", "idx", "sig", "semref")

    def __init__(self, eng, fn, is_dma):
        self.eng = eng
        self.fn = fn
        self.deps = []
        self.is_dma = is_dma
        self.sig = False
        self.semref = None


class Prog:
    def __init__(self, nc):
        self.nc = nc
        self.ops = []
        self.last_w = {}
        self.readers = {}
        self.last_on_eng = {e: None for e in ENGS}
        self.barrier_deps = []
        self.need_barrier = {e: False for e in ENGS}
        self.sb_off = SB_BASE
        self.marks = []
        self.names = 0
        self.recent_dma = {}

    def sb(self, shape, dtype, name=None):
        self.names += 1
        name = (name or "t") + "_%d" % self.names
        esz = {F32: 4, BF16: 2, I32: 4, U32: 4}[dtype]
        n = 1
        for s in shape[1:]:
            n *= s
        nbytes = (n * esz + 31) // 32 * 32
        off = self.sb_off
        self.sb_off += nbytes
        assert self.sb_off <= SB_TOP, ("SBUF overflow", name, self.sb_off)
        return self.nc.alloc_sbuf_tensor_at(name, list(shape), dtype, offset=off)

    def mark(self):
        self.marks.append(self.sb_off)

    def release(self):
        self.sb_off = self.marks.pop()
        self.barrier()

    def barrier(self):
        self.barrier_deps = [self.last_on_eng[e] for e in ENGS if self.last_on_eng[e] is not None]
        for q in self.recent_dma.values():
            self.barrier_deps.extend(q)
        for e in ENGS:
            self.need_barrier[e] = True

    def op(self, eng, fn, reads=(), writes=(), dma=False):
        o = Op(eng, fn, dma or eng == "sp")
        o.idx = len(self.ops)
        deps = set()
        if self.need_barrier[eng]:
            deps.update(self.barrier_deps)
            self.need_barrier[eng] = False
        reads = list(reads)
        writes = list(writes)
        for k in list(reads):
            if isinstance(k, str) and k.startswith("ps"):
                reads.remove(k)
                if k not in writes:
                    writes.append(k)
        for k in reads:
            w = self.last_w.get(k)
            if w is not None:
                deps.add(w)
        for k in writes:
            w = self.last_w.get(k)
            if w is not None:
                deps.add(w)
            rd = self.readers.get(k)
            if rd:
                for v in rd.values():
                    if isinstance(v, list):
                        deps.update(v)
                    else:
                        deps.add(v)
        for k in reads:
            rd = self.readers.setdefault(k, {})
            if o.is_dma:
                rd.setdefault("dma_" + eng, []).append(o.idx)
            else:
                rd[eng] = o.idx
        for k in writes:
            self.last_w[k] = o.idx
            self.readers[k] = {}
        deps.discard(o.idx)
        o.deps = sorted(deps)
        self.ops.append(o)
        self.last_on_eng[eng] = o.idx
        if o.is_dma:
            q = self.recent_dma.setdefault(eng, [])
            q.append(o.idx)
            if len(q) > NDMASEM:
                q.pop(0)
        return o

    def emit(self):
        nc = self.nc
        ops = self.ops
        for o in ops:
            latest = {}
            keep = []
            for d in o.deps:
                p = ops[d]
                if p.is_dma:
                    keep.append(d)
                elif p.eng == "pe" and o.eng == "pe":
                    continue
                else:
                    if d > latest.get(p.eng, -1):
                        latest[p.eng] = d
            keep.extend(latest.values())
            o.deps = sorted(keep)
            for d in o.deps:
                ops[d].sig = True
        sems = {}

        def getsem(key):
            if key not in sems:
                sems[key] = nc.alloc_semaphore("s_%s_%s" % key)
            return sems[key]

        cnt = {e: 0 for e in ENGS}
        dcnt = {}
        dper = {}
        prev_on_dsem = {}
        for o in ops:
            if o.is_dma:
                q = o.eng
                i = dcnt.get(q, 0)
                dcnt[q] = i + 1
                j = i % NDMASEM
                key = ("d" + q, j)
                c = dper.get(key, 0) + 1
                dper[key] = c
                o.semref = (key, 16 * c)
                if c > 1:
                    o.deps = sorted(set(o.deps) | {prev_on_dsem[key]})
                prev_on_dsem[key] = o.idx
                o.sig = True
            elif o.sig:
                kk = cnt[o.eng]
                cnt[o.eng] = kk + 1
                o.semref = ((o.eng, kk // EPOCH), (kk % EPOCH) + 1)
        for o in ops:
            if o.semref is not None:
                getsem(o.semref[0])
        self.stats = dict(nops=len(ops), nsig=sum(1 for o in ops if o.sig), cnt=dict(cnt), dcnt=dict(dcnt),
                          nsem=len(sems))
        per_eng = {e: [o for o in ops if o.eng == e] for e in ENGS}
        final_dma = {key: 16 * c for key, c in dper.items()}

        def run(engname, eng):
            waited = {}
            for o in per_eng[engname]:
                for d in o.deps:
                    p = ops[d]
                    if p.semref is None:
                        continue
                    if p.eng == "pe" and o.eng == "pe" and not p.is_dma:
                        continue
                    key, val = p.semref
                    if waited.get(key, 0) >= val:
                        continue
                    waited[key] = val
                    eng.wait_ge(sems[key], val)
                inst = o.fn(eng)
                if o.semref is not None:
                    key, val = o.semref
                    inst.then_inc(sems[key], 16 if o.is_dma else 1)
            if engname == "sp":
                for key, val in final_dma.items():
                    if waited.get(key, 0) < val:
                        eng.wait_ge(sems[key], val)

        with nc.Block() as block:
            @block.tensor
            def _(e):
                run("pe", e)

            @block.scalar
            def _(e):
                run("act", e)

            @block.vector
            def _(e):
                run("dve", e)

            @block.gpsimd
            def _(e):
                run("pool", e)

            @block.sync
            def _(e):
                run("sp", e)


class K(Prog):
    def mm(self, out, lhsT, rhs, start, stop, r, w):
        return self.op("pe", lambda e: e.matmul(out=out, lhsT=lhsT, rhs=rhs, start=start, stop=stop), r, w)

    def tr(self, out, in_, ident, r, w):
        return self.op("pe", lambda e: e.transpose(out=out, in_=in_, identity=ident), r, w)

    def act(self, out, in_, func, r, w, scale=1.0, bias=None, accum=None):
        def f(e):
            kw = dict(out=out, in_=in_, func=func, scale=scale)
            if bias is not None:
                kw["bias"] = bias
            if accum is not None:
                kw["accum_out"] = accum
            return e.activation(**kw)
        return self.op("act", f, r, w)

    def tt(self, eng, out, a, b, op, r, w):
        return self.op(eng, lambda e: e.tensor_tensor(out=out, in0=a, in1=b, op=op), r, w)

    def ts(self, eng, out, a, s1, s2, op0, op1, r, w):
        if s2 is None:
            return self.op(eng, lambda e: e.tensor_scalar(out=out, in0=a, scalar1=s1, scalar2=None, op0=op0), r, w)
        return self.op(eng, lambda e: e.tensor_scalar(out=out, in0=a, scalar1=s1, scalar2=s2, op0=op0, op1=op1), r, w)

    def stt(self, out, a, s, b, op0, op1, r, w, accum=None):
        def f(e):
            kw = dict(out=out, in0=a, scalar=s, in1=b, op0=op0, op1=op1)
            if accum is not None:
                kw["accum_out"] = accum
            return e.scalar_tensor_tensor(**kw)
        return self.op("dve", f, r, w)

    def cp(self, eng, out, in_, r, w):
        if eng == "act":
            return self.op("act", lambda e: e.copy(out=out, in_=in_), r, w)
        return self.op(eng, lambda e: e.tensor_copy(out=out, in_=in_), r, w)

    def recip(self, out, in_, r, w):
        return self.op("dve", lambda e: e.reciprocal(out=out, in_=in_), r, w)

    def rsum(self, out, in_, r, w):
        return self.op("dve", lambda e: e.reduce_sum(out=out, in_=in_, axis=AX.X), r, w)

    def max8(self, out, in_, r, w):
        return self.op("dve", lambda e: e.max(out=out, in_=in_), r, w)

    def maxidx(self, out, in_max, in_values, r, w):
        return self.op("dve", lambda e: e.max_index(out=out, in_max=in_max, in_values=in_values), r, w)

    def matchrep(self, out, rep, vals, imm, r, w):
        return self.op("dve", lambda e: e.match_replace(out=out, in_to_replace=rep, in_values=vals, imm_value=imm), r, w)

    def memset(self, eng, ap, val, w):
        return self.op(eng, lambda e: e.memset(ap, val), (), w)

    def dma(self, out, in_, r, w):
        return self.op("sp", lambda e: e.dma_start(out=out, in_=in_), r, w)

    def rstd_from_ss(self, ss, rstd, tmp, nh, n, key):
        self.ts("dve", tmp, ss, 1.0 / n, 1e-6, ALU.mult, ALU.add, [key + "ss"], [key + "ms"])
        self.tt("pool", rstd, tmp, nh, ALU.pow, [key + "ms", "nh"], [key + "rstd"])

S = 2048
D = 2048
NT = 16
GC = 0.7978845608028654
SCALE = 128 ** -0.5
NEG = -30000.0
C_U, C_V, C_Q, C_KV, C_NG, C_MG = 0, 2048, 4096, 6144, 9216, 9264

CONST_SHAPES = {
    "c_cos": [128, S], "c_sin": [128, S], "c_rt": [128, 128], "c_caus": [128, 128], "c_winb": [128, 128],
    "c_cmpb": [128, S], "c_aagg": [128, 32], "c_am": [128, 16, 32], "c_add": [128, 16, 32],
    "c_esel": [32, 16, 128], "c_tril": [128, 16, 128], "c_ident": [128, 128], "c_iota16": [128, 16],
    "c_zrow": [128, 255],
}


def make_consts():
    c = {}
    half = 64
    inv = 10000.0 ** (-np.arange(half, dtype=np.float32) / half)
    ang = np.arange(S, dtype=np.float32)[:, None] * inv[None, :]
    cos = np.cos(ang).astype(np.float32).T
    sin = np.sin(ang).astype(np.float32).T
    c["c_cos"] = np.concatenate([cos, cos], 0)
    c["c_sin"] = np.concatenate([sin, sin], 0)
    rt = np.zeros((128, 128), np.float32)
    for m in range(128):
        if m < 64:
            rt[m + 64, m] = -1.0
        else:
            rt[m - 64, m] = 1.0
    c["c_rt"] = rt
    kk = np.arange(128)[:, None]
    qq = np.arange(128)[None, :]
    c["c_caus"] = np.where(kk <= qq, 0.0, NEG).astype(np.float32)
    c["c_winb"] = np.where(kk > qq, 0.0, NEG).astype(np.float32)
    n = np.arange(128)[:, None]
    q = np.arange(S)[None, :]
    c["c_cmpb"] = np.where((16 * n + 31 <= q) & (n < 127), 0.0, NEG).astype(np.float32)
    agg = np.array([1, 2, 2, 2, 1], np.float32)
    a = np.zeros((128, 32), np.float32)
    for nn in range(127):
        for j in range(32):
            w = nn + 1 - 4 * j
            if 0 <= w <= 4:
                a[nn, j] = agg[w]
    c["c_aagg"] = a
    pos = (np.arange(16)[None, :, None] * 128 + np.arange(128)[:, None, None])
    j = np.arange(32)[None, None, :]
    allowed = (j * 64) <= pos
    forced = (j == 0) | (j == (pos // 64))
    c["c_am"] = (allowed & ~forced).astype(np.float32)
    c["c_add"] = np.where(forced, 1e30, np.where(allowed, 0.0, -1e30)).astype(np.float32)
    e = np.zeros((32, 16, 128), np.float32)
    for kt in range(16):
        for key in range(128):
            e[2 * kt + key // 64, kt, key] = 1.0
    c["c_esel"] = e
    s_ = np.arange(128)[:, None, None]
    t_ = np.arange(128)[None, None, :]
    c["c_tril"] = np.broadcast_to((s_ <= t_), (128, 16, 128)).astype(np.float32).copy()
    c["c_ident"] = np.eye(128, dtype=np.float32)
    c["c_iota16"] = np.broadcast_to(np.arange(16, dtype=np.float32)[None, :], (128, 16)).copy()
    z = np.zeros((128, 255), np.float32)
    z[:, 127] = 1.0
    c["c_zrow"] = z
    return c


def build_program(stop_after=99, dbg=()):
    nc = bass.Bass("TRN2", target_bir_lowering=False)
    k = K(nc)

    def din(name, shape):
        return nc.dram_tensor(name, list(shape), F32, kind="ExternalInput").ap()

    def dscr(name, shape, dt=F32):
        kind = "ExternalOutput" if name in dbg else "Internal"
        return nc.dram_tensor(name, list(shape), dt, kind=kind).ap()

    x = din("x", [S, D])
    norm_mix_g = din("norm_mix_g", [1, D])
    w_in = din("w_in", [D, 13360])
    w_out = din("w_out", [D, D])
    gm_ln_g = din("gm_ln_g", [1, D])
    gm_ln_b = din("gm_ln_b", [1, D])
    gm_wT = din("gm_wT", [128, 16, 128])
    gm_bT = din("gm_bT", [128, 16])
    posT = {"k": din("cmp_posT_k", [128, 32]), "v": din("cmp_posT_v", [128, 32])}
    cw1 = {"k": din("cmp_w1_k", [4096, 256]), "v": din("cmp_w1_v", [4096, 256])}
    cw2 = {"k": din("cmp_w2_k", [256, 128]), "v": din("cmp_w2_v", [256, 128])}
    norm_ffn_g = din("norm_ffn_g", [1, D])
    peer_w_q = din("peer_w_q", [D, D])
    keysT = [din("keys1T", [128, 128]), din("keys2T", [128, 128])]
    NEXP = 16384 if stop_after >= 5 else 128
    peer_u = din("peer_u", [NEXP, D])
    peer_v = din("peer_v", [NEXP, D])
    norm_final_g = din("norm_final_g", [1, D])
    cin = {n: din(n, s) for n, s in CONST_SHAPES.items()}
    y = nc.dram_tensor("y", [S, D], F32, kind="ExternalOutput").ap()

    SU = dscr("s_u", [S, D]); SV = dscr("s_v", [S, D]); SVS = dscr("s_vs", [S, 512]); SVW = dscr("s_vw", [S, 512])
    SNG = dscr("s_ng", [S, 48]); SMG = dscr("s_mg", [S, 4096])
    SQ = dscr("s_q", [16, 128, S]); SKC = dscr("s_kc", [4, 128, S]); SVC = dscr("s_vc", [4, 128, S])
    SKS = dscr("s_ks", [4, 128, S]); SKW = dscr("s_kw", [4, 128, S])
    SMA = dscr("s_ma", [S, D]); SOB = dscr("s_ob", [S, D]); SX1 = dscr("s_x1", [S, D])
    SH2 = dscr("s_h2", [S, D], BF16); SS = dscr("s_ss", [S, 2048])

    PSALL = nc.alloc_psum_tensor("psall", [128, 4096], F32)

    def PS(b, n=1):
        return PSALL[:, b * 512:(b + n) * 512]

    def PSB(b):
        return PSALL[:, b * 512:(b + 1) * 512].bitcast(BF16)

    def psk(b, n=1):
        return ["ps%d" % i for i in range(b, b + n)]

    ident_f = k.sb([128, 128], F32, "identf")
    ident_b = k.sb([128, 128], BF16, "identb")
    ones_b = k.sb([128, 128], BF16, "onesb")
    nh = k.sb([128, 1], F32, "nh")
    k.dma(ident_f[:], cin["c_ident"], [], ["identf"])
    k.cp("dve", ident_b[:], ident_f[:], ["identf"], ["identb"])
    k.memset("pool", ones_b[:], 1.0, ["onesb"])
    k.memset("pool", nh[:], -0.5, ["nh"])

    def row(t):
        return slice(t * 128, (t + 1) * 128)

    k.mark()
    hT = k.sb([128, 16, S], BF16, "hT")
    k.mark()
    gb = k.sb([128, D], F32, "gb")
    xt = [k.sb([128, D], F32, "xt") for _ in range(2)]
    junk = k.sb([128, D], BF16, "junk")
    hb = [k.sb([128, D], BF16, "hb") for _ in range(2)]
    sm = [k.sb([128, 4], F32, "sm") for _ in range(2)]
    k.dma(gb[:], norm_mix_g.partition_broadcast(128), [], ["gb"])
    for t in range(NT):
        p = t % 2
        kp = "a%d" % p
        k.dma(xt[p][:], x[row(t), :], [], [kp + "xt"])
        k.act(junk[:], xt[p][:], AF.Square, [kp + "xt"], ["junk", kp + "ss"], accum=sm[p][:, 0:1])
        k.rstd_from_ss(sm[p][:, 0:1], sm[p][:, 2:3], sm[p][:, 1:2], nh[:], D, kp)
        k.stt(hb[p][:], xt[p][:], sm[p][:, 2:3], gb[:], ALU.mult, ALU.mult, [kp + "xt", kp + "rstd", "gb"], [kp + "hb"])
        for kc in range(16):
            b = 2 * p + kc // 8
            k.tr(PSB(b)[:, (kc % 8) * 128:(kc % 8 + 1) * 128], hb[p][:, kc * 128:(kc + 1) * 128], ident_b[:],
                 [kp + "hb", "identb"], psk(b))
        for hh in range(2):
            b = 2 * p + hh
            k.cp("dve" if hh == 0 else "act", hT[:, hh * 8:(hh + 1) * 8, row(t)],
                 PSB(b).rearrange("p (a b) -> p a b", b=128), psk(b), [("hT", t)])
    k.release()

    if stop_after >= 1:
        wf = [k.sb([128, 16, 512], F32, "wf") for _ in range(2)]
        wb = [k.sb([128, 16, 512], BF16, "wb") for _ in range(2)]
        ob = [k.sb([128, 512], F32, "ob") for _ in range(4)]
        chunks = []
        for c in range(4):
            chunks.append((C_U + c * 512, 512, "tm", (SU, c * 512)))
        for c in range(4):
            chunks.append((C_V + c * 512, 512, "tm", (SV, c * 512)))
        for c in range(4):
            chunks.append((C_Q + c * 512, 512, "fm", [SQ[4 * c + i] for i in range(4)]))
        chunks.append((C_KV + 0 * 512, 512, "fm", [SKC[i] for i in range(4)]))
        chunks.append((C_KV + 1 * 512, 512, "fm", [SVC[i] for i in range(4)]))
        chunks.append((C_KV + 2 * 512, 512, "fm", [SKS[i] for i in range(4)]))
        chunks.append((C_KV + 3 * 512, 512, "tm", (SVS, 0)))
        chunks.append((C_KV + 4 * 512, 512, "fm", [SKW[i] for i in range(4)]))
        chunks.append((C_KV + 5 * 512, 512, "tm", (SVW, 0)))
        chunks.append((C_NG, 48, "tm", (SNG, 0)))
        for c in range(8):
            chunks.append((C_MG + c * 512, 512, "tm", (SMG, c * 512)))

        def load_chunk(ci):
            c0, n, _, _ = chunks[ci]
            p = ci % 2
            k.dma(wf[p][:, :, 0:n], w_in[:, c0:c0 + n].rearrange("(kc p) n -> p kc n", p=128), [], [("wf", p)])

        cnt = 0
        load_chunk(0)
        for ci, (c0, n, kind, dest) in enumerate(chunks):
            p = ci % 2
            if ci + 1 < len(chunks):
                load_chunk(ci + 1)
            k.cp("dve", wb[p][:, 0:8, 0:n], wf[p][:, 0:8, 0:n], [("wf", p)], [("wb", p)])
            k.cp("act", wb[p][:, 8:16, 0:n], wf[p][:, 8:16, 0:n], [("wf", p)], [("wb", p)])
            for u in range(16):
                b = 2 + cnt % 6
                o = ob[cnt % 4]
                okey = ("ob", cnt % 4)
                if kind == "tm":
                    t = u
                    for kc in range(16):
                        k.mm(PS(b)[:, 0:n], hT[:, kc, row(t)], wb[p][:, kc, 0:n], kc == 0, kc == 15,
                             [("hT", t), ("wb", p)], psk(b))
                    dst = dest[0][row(t), dest[1]:dest[1] + n]
                else:
                    blk, tc = u // 4, u % 4
                    for kc in range(16):
                        k.mm(PS(b)[:, 0:512], wb[p][:, kc, blk * 128:(blk + 1) * 128], hT[:, kc, tc * 512:(tc + 1) * 512],
                             kc == 0, kc == 15, [("hT", 4 * tc + i) for i in range(4)] + [("wb", p)], psk(b))
                    dst = dest[blk][:, tc * 512:(tc + 1) * 512]
                k.cp("act" if cnt % 2 == 0 else "dve", o[:, 0:n], PS(b)[:, 0:n], psk(b), [okey])
                k.dma(dst, o[:, 0:n], [okey], [("scr", ci, u)])
                cnt += 1
    k.release()
    k.barrier()

    if stop_after >= 2:
        k.mark()
        gam = k.sb([128, D], F32, "gam")
        bet = k.sb([128, D], F32, "bet")
        BS = k.sb([128, 16, 128], F32, "BS")
        WTm = k.sb([128, 16, 128], BF16, "WTm")
        bsT = k.sb([128, 16], F32, "bsT")
        k.mark()
        st1 = k.sb([128, 16, 128], F32, "st1")
        st2 = k.sb([128, 16, 128], F32, "st2")
        k.dma(gam[:], gm_ln_g.partition_broadcast(128), [], ["gam"])
        k.dma(bet[:], gm_ln_b.partition_broadcast(128), [], ["bet"])
        k.dma(bsT[:], gm_bT, [], ["bsT"])
        k.dma(st1[:], gm_wT, [], ["st1"])
        k.dma(st2[:], cin["c_tril"], [], ["st2"])
        k.tt("dve", WTm[:], st1[:], st2[:], ALU.mult, ["st1", "st2"], ["WTm"])
        k.cp("dve", BS[:], bsT[:].unsqueeze(2).to_broadcast([128, 16, 128]), ["bsT"], ["BS"])
        k.release()
        ut = [k.sb([128, D], F32, "ut") for _ in range(2)]
        vt = [k.sb([128, D], F32, "vt") for _ in range(2)]
        gt = [k.sb([128, D], F32, "gt") for _ in range(2)]
        tA = k.sb([128, D], F32, "tA")
        tB = k.sb([128, D], F32, "tB")
        Uh = k.sb([128, D], F32, "Uh")
        G2 = k.sb([128, D], F32, "G2")
        vln = k.sb([128, D], BF16, "vln")
        mo = [k.sb([128, D], F32, "mo") for _ in range(2)]
        sm2 = [k.sb([128, 8], F32, "sm2") for _ in range(2)]
        BSf = BS[:].rearrange("p a b -> p (a b)")

        def load2(t):
            p = t % 2
            k.dma(ut[p][:], SU[row(t), :], [], [("ut", p)])
            k.dma(vt[p][:], SV[row(t), :], [], [("vt", p)])
            k.dma(gt[p][:], SMG[row(t), 0:D], [], [("gt", p)])

        def tanh_inner(src, skey):
            k.act(tA[:], src, AF.Square, [skey], ["tA"])
            k.ts("dve", tA[:], tA[:], 0.044715, 1.0, ALU.mult, ALU.add, ["tA"], ["tA"])
            k.tt("dve", tA[:], tA[:], src, ALU.mult, ["tA", skey], ["tA"])
            k.act(tB[:], tA[:], AF.Tanh, ["tA"], ["tB"], scale=GC)

        load2(0)
        for t in range(NT):
            p = t % 2
            if t + 1 < NT:
                load2(t + 1)
            s = sm2[p]
            tanh_inner(ut[p][:], ("ut", p))
            k.ts("dve", tB[:], tB[:], 0.25, 0.25, ALU.mult, ALU.add, ["tB"], ["tB"])
            k.tt("dve", Uh[:], tB[:], ut[p][:], ALU.mult, ["tB", ("ut", p)], ["Uh"])
            tanh_inner(vt[p][:], ("vt", p))
            k.stt(G2[:], tB[:], 1.0, vt[p][:], ALU.add, ALU.mult, ["tB", ("vt", p)], ["G2", ("s1", p)], accum=s[:, 0:1])
            k.act(tA[:], G2[:], AF.Square, ["G2"], ["tA", ("s2", p)], accum=s[:, 1:2])
            k.ts("dve", s[:, 2:3], s[:, 0:1], 1.0 / D, None, ALU.mult, None, [("s1", p)], [("mean2", p)])
            k.tt("dve", s[:, 3:4], s[:, 2:3], s[:, 2:3], ALU.mult, [("mean2", p)], [("msq", p)])
            k.stt(s[:, 4:5], s[:, 1:2], 1.0 / D, s[:, 3:4], ALU.mult, ALU.subtract, [("s2", p), ("msq", p)], [("var4", p)])
            k.ts("dve", s[:, 5:6], s[:, 4:5], 0.25, 1e-6, ALU.mult, ALU.add, [("var4", p)], [("v4", p)])
            k.tt("pool", s[:, 6:7], s[:, 5:6], nh[:], ALU.pow, [("v4", p), "nh"], [("rs", p)])
            k.ts("dve", s[:, 7:8], s[:, 6:7], 0.5, None, ALU.mult, None, [("rs", p)], [("rsh", p)])
            k.ts("dve", tA[:], G2[:], s[:, 2:3], s[:, 7:8], ALU.subtract, ALU.mult, ["G2", ("mean2", p), ("rsh", p)], ["tA"])
            k.tt("dve", tA[:], tA[:], gam[:], ALU.mult, ["tA", "gam"], ["tA"])
            k.tt("dve", vln[:], tA[:], bet[:], ALU.add, ["tA", "bet"], ["vln"])
            b0 = 4 * p
            for g in range(16):
                b = b0 + g // 4
                k.mm(PS(b)[:, (g % 4) * 128:(g % 4 + 1) * 128], WTm[:, g, :], vln[:, g * 128:(g + 1) * 128], True, True,
                     ["WTm", "vln"], psk(b))
            k.tt("dve", tA[:], PS(b0, 4), BSf, ALU.add, psk(b0, 4) + ["BS"], ["tA"])
            k.tt("dve", tA[:], tA[:], Uh[:], ALU.mult, ["tA", "Uh"], ["tA"])
            k.act(tB[:], gt[p][:], AF.Tanh, [("gt", p)], ["tB"], scale=0.5)
            k.stt(mo[p][:], tB[:], 1.0, tA[:], ALU.add, ALU.mult, ["tB", "tA"], [("mo", p)])
            k.dma(SMA[row(t), :], mo[p][:], [("mo", p)], [("sma", t)])
        k.release()
        k.barrier()

    if stop_after >= 3:
        k.mark()
        cosT = k.sb([128, S], F32, "cosT"); sinT = k.sb([128, S], F32, "sinT")
        RT = k.sb([128, 128], F32, "RT")
        CAUS = k.sb([128, 128], BF16, "CAUS"); WINB = k.sb([128, 128], BF16, "WINB")
        CMPB = k.sb([128, S], BF16, "CMPB")
        Aagg = k.sb([128, 32], F32, "Aagg")
        AM = k.sb([128, 16, 32], F32, "AM"); ADDM = k.sb([128, 16, 32], F32, "ADDM")
        ESEL = k.sb([32, 16, 128], BF16, "ESEL")
        w1b = {m: k.sb([128, 32, 256], BF16, "w1b") for m in "kv"}
        w2b = {m: k.sb([128, 2, 128], BF16, "w2b") for m in "kv"}
        posb = {m: k.sb([128, 32], BF16, "posb") for m in "kv"}
        cvec = {m: k.sb([128, 2], F32, "cvec") for m in "kv"}
        stg = k.sb([128, 2 * S], F32, "stg")
        kcb = k.sb([128, S], BF16, "kcb"); vcb = k.sb([128, S], BF16, "vcb")
        ksr = k.sb([128, S], BF16, "ksr"); kwr = k.sb([128, S], BF16, "kwr")
        vsa = k.sb([128, 16, 129], BF16, "vsa"); vwa = k.sb([128, 16, 129], BF16, "vwa")
        qb = [k.sb([128, S], BF16, "qb") for _ in range(4)]
        qr = [k.sb([128, S], BF16, "qr") for _ in range(4)]
        sgr = k.sb([128, 16, 12], F32, "sgr"); sg = k.sb([128, 16, 12], F32, "sg")
        kcT = k.sb([128, 128], BF16, "kcT"); vca = k.sb([128, 130], BF16, "vca")
        hid = k.sb([128, 2, 128], BF16, "hid")
        hx = k.sb([128, 128], F32, "hx"); hy = k.sb([128, 128], F32, "hy"); hz = k.sb([128, 128], F32, "hz")
        biasT = k.sb([32, S], BF16, "biasT")
        PcT = [k.sb([128, 512], BF16, "PcT") for _ in range(2)]
        pn = k.sb([128, 512], F32, "pn"); rZ = [k.sb([128, 512], F32, "rZ") for _ in range(2)]; impS = k.sb([32, 512], F32, "impS")
        sc = k.sb([128, 128], F32, "sc"); selb = k.sb([128, 128], F32, "selb"); wk32 = k.sb([128, 32], F32, "wk32")
        m8 = k.sb([128, 16], F32, "m8")
        accp = [k.sb([128, 4, 4, 128], F32, "acc") for _ in range(2)]
        PT = [k.sb([128, 512], BF16, "PT") for _ in range(3)]
        t1 = [k.sb([128, 512], F32, "t1") for _ in range(2)]
        t2 = [k.sb([128, 512], F32, "t2") for _ in range(2)]
        czs = [k.sb([128, 2], F32, "cz") for _ in range(4)]

        def stgv(b):
            return stg[:, b * S:(b + 1) * S]

        k.dma(cosT[:], cin["c_cos"], [], ["cosT"]); k.dma(sinT[:], cin["c_sin"], [], ["sinT"])
        k.dma(RT[:], cin["c_rt"], [], ["RT"]); k.dma(Aagg[:], cin["c_aagg"], [], ["Aagg"])
        k.dma(AM[:], cin["c_am"], [], ["AM"]); k.dma(ADDM[:], cin["c_add"], [], ["ADDM"])
        k.dma(stgv(0), cin["c_cmpb"], [], [("stg", 0)])
        k.cp("dve", CMPB[:], stgv(0), [("stg", 0)], ["CMPB"])
        k.dma(stg[:, S:S + 128], cin["c_caus"], [], [("stg", 1)])
        k.dma(stg[:, S + 128:S + 256], cin["c_winb"], [], [("stg", 1)])
        k.cp("dve", CAUS[:], stg[:, S:S + 128], [("stg", 1)], ["CAUS"])
        k.cp("dve", WINB[:], stg[:, S + 128:S + 256], [("stg", 1)], ["WINB"])
        k.dma(stg[0:32, 0:S].rearrange("p (a b) -> p a b", b=128), cin["c_esel"], [("stg", 0)], [("stg", 0)])
        k.cp("dve", ESEL[:], stg[0:32, 0:S].rearrange("p (a b) -> p a b", b=128), [("stg", 0)], ["ESEL"])
        for m in "kv":
            for hf in range(2):
                k.dma(stg[:].rearrange("p (l n) -> p l n", n=256),
                      cw1[m][hf * 2048:(hf + 1) * 2048, :].rearrange("(l d) n -> d l n", d=128), [], [("stg", 0), ("stg", 1)])
                k.cp("dve" if hf == 0 else "act", w1b[m][:, hf * 16:(hf + 1) * 16, :], stg[:].rearrange("p (l n) -> p l n", n=256),
                     [("stg", 0), ("stg", 1)], [("w1b", m)])
            k.dma(stg[:, 0:256].rearrange("p (h d) -> p h d", d=128), cw2[m].rearrange("(h p) d -> p h d", p=128), [], [("stg", 0)])
            k.cp("dve", w2b[m][:], stg[:, 0:256].rearrange("p (h d) -> p h d", d=128), [("stg", 0)], [("w2b", m)])
            k.dma(stg[:, S:S + 32], posT[m], [], [("stg", 1)])
            k.cp("dve", posb[m][:], stg[:, S:S + 32], [("stg", 1)], [("posb", m)])
            for hf in range(2):
                for l in range(32):
                    k.mm(PS(7)[:, hf:hf + 1], w1b[m][:, l, hf * 128:(hf + 1) * 128], posb[m][:, l:l + 1], l == 0, l == 31,
                         [("w1b", m), ("posb", m)], psk(7))
            k.cp("dve", cvec[m][:], PS(7)[:, 0:2], psk(7), [("cvec", m)])
        k.memset("pool", vsa[:, :, 128:129], 1.0, ["vsa"])
        k.memset("pool", vwa[:, :, 128:129], 1.0, ["vwa"])
        k.memset("pool", vca[:, 128:129], 1.0, ["vca"])

        rot_i = [0]

        def rotary(src, skey, dst, dkey):
            for tc in range(4):
                i = rot_i[0] % 2
                rot_i[0] += 1
                sl = slice(tc * 512, (tc + 1) * 512)
                b = 4 + i
                k.mm(PS(b), RT[:], src[:, sl], True, True, [skey, "RT"], psk(b))
                k.tt("dve", t1[i][:], PS(b), sinT[:, sl], ALU.mult, psk(b) + ["sinT"], [("t1", i)])
                k.tt("pool", t2[i][:], src[:, sl], cosT[:, sl], ALU.mult, [skey, "cosT"], [("t2", i)])
                k.tt("dve", dst[:, sl], t1[i][:], t2[i][:], ALU.add, [("t1", i), ("t2", i)], [dkey])

        def compress(m, src, skey):
            for hf in range(2):
                b = 4 + hf
                for l in range(32):
                    k.mm(PS(b)[:, 0:127], w1b[m][:, l, hf * 128:(hf + 1) * 128], src[:, l:l + 16 * 126 + 1:16], l == 0, l == 31,
                         [("w1b", m), skey], psk(b))
                k.ts("dve", hx[:, 0:127], PS(b)[:, 0:127], cvec[m][:, hf:hf + 1], None, ALU.add, None, psk(b) + [("cvec", m)], ["hx"])
                k.act(hy[:, 0:127], hx[:, 0:127], AF.Square, ["hx"], ["hy"])
                k.ts("dve", hy[:, 0:127], hy[:, 0:127], 0.044715, 1.0, ALU.mult, ALU.add, ["hy"], ["hy"])
                k.tt("dve", hy[:, 0:127], hy[:, 0:127], hx[:, 0:127], ALU.mult, ["hy", "hx"], ["hy"])
                k.act(hz[:, 0:127], hy[:, 0:127], AF.Tanh, ["hy"], ["hz"], scale=GC)
                k.ts("dve", hz[:, 0:127], hz[:, 0:127], 0.5, 0.5, ALU.mult, ALU.add, ["hz"], ["hz"])
                k.tt("dve", hid[:, hf, 0:127], hz[:, 0:127], hx[:, 0:127], ALU.mult, ["hz", "hx"], ["hid"])
            if m == "k":
                for hf in range(2):
                    k.mm(PS(6)[:, 0:127], w2b[m][:, hf, :], hid[:, hf, 0:127], hf == 0, hf == 1, [("w2b", m), "hid"], psk(6))
                k.cp("act", kcT[:, 0:127], PS(6)[:, 0:127], psk(6), ["kcT"])
            else:
                for hf in range(2):
                    k.mm(PS(6)[0:127, 0:128], hid[:, hf, 0:127], w2b[m][:, hf, :], hf == 0, hf == 1, [("w2b", m), "hid"], psk(6))
                k.cp("act", vca[0:127, 0:128], PS(6)[0:127, 0:128], psk(6), ["vca"])

        sb_i = [0]
        ob_i = [0]
        pt_i = [0]
        cz_i = [0]
        OBANKS = [2, 3, 7]

        def attn(kT, kkey, va, vkey, qrt, qkey, qt, ktl, use_sel):
            obk = OBANKS[ob_i[0] % 3]
            ob_i[0] += 1
            nk = len(ktl)
            done = 0
            for g0 in range(0, nk, 4):
                grp = ktl[g0:g0 + 4]
                sbk = sb_i[0] % 2
                sb_i[0] += 1
                for j, (kt, eb) in enumerate(grp):
                    out = PS(sbk)[:, j * 128:(j + 1) * 128]
                    nmm = 1 + (1 if use_sel else 0) + (1 if eb else 0)
                    k.mm(out, kT[:, kt * 128:(kt + 1) * 128], qrt[:, row(qt)], True, nmm == 1, [kkey, qkey], psk(sbk))
                    i = 1
                    if use_sel:
                        i += 1
                        k.mm(out, ESEL[0:32, kt, :], biasT[0:32, row(qt)], False, i == nmm, ["ESEL", "biasT"], psk(sbk))
                    if eb:
                        k.mm(out, ident_b[:], (CAUS if eb == "caus" else WINB)[:], False, True, ["identb", "CAUS", "WINB"], psk(sbk))
                n = len(grp) * 128
                pi = pt_i[0] % 3
                pt_i[0] += 1
                k.act(PT[pi][:, 0:n], PS(sbk)[:, 0:n], AF.Exp, psk(sbk), [("PT", pi)], scale=SCALE)
                for j, (kt, eb) in enumerate(grp):
                    k.mm(PS(obk)[:, 0:129], PT[pi][:, j * 128:(j + 1) * 128], va[:, kt, :], done == 0, done == nk - 1,
                         [("PT", pi), vkey], psk(obk))
                    done += 1
            return obk

        def combine(obk, qt, qtl, g, br, ap, akey, first, guard):
            cz = czs[cz_i[0] % 4]
            ck = ("cz", cz_i[0] % 4)
            cz_i[0] += 1
            if guard:
                k.ts("dve", cz[:, 0:1], PS(obk)[:, 128:129], 1e-30, None, ALU.max, None, psk(obk), [ck])
                k.recip(cz[:, 0:1], cz[:, 0:1], [ck], [ck])
            else:
                k.recip(cz[:, 0:1], PS(obk)[:, 128:129], psk(obk), [ck])
            gate = sg[:, qt, g * 3 + br:g * 3 + br + 1]
            if first:
                k.ts("dve", ap[:, qtl, g, :], PS(obk)[:, 0:128], cz[:, 0:1], gate, ALU.mult, ALU.mult, psk(obk) + [ck, "sg"], [akey])
            else:
                k.tt("dve", cz[:, 1:2], cz[:, 0:1], gate, ALU.mult, [ck, "sg"], [ck])
                k.stt(ap[:, qtl, g, :], PS(obk)[:, 0:128], cz[:, 1:2], ap[:, qtl, g, :], ALU.mult, ALU.add, psk(obk) + [ck, akey], [akey])

        for hk in range(4 if stop_after >= 3 else 0):
            si = [0]

            def nxt():
                b = si[0] % 2
                si[0] += 1
                return b
            for nm, src, dst in (("kcb", SKC[hk], kcb), ("vcb", SVC[hk], vcb)):
                b = nxt()
                k.dma(stgv(b), src, [], [("stg", b)])
                k.cp("act", dst[:], stgv(b), [("stg", b)], [nm])
            for nm, src, dst in (("ksr", SKS[hk], ksr), ("kwr", SKW[hk], kwr)):
                b = nxt()
                k.dma(stgv(b), src, [], [("stg", b)])
                rotary(stgv(b), ("stg", b), dst, nm)
            for g in range(4):
                b = nxt()
                k.dma(stgv(b), SQ[4 * hk + g], [], [("stg", b)])
                k.cp("act", qb[g][:], stgv(b), [("stg", b)], [("qb", g)])
                rotary(stgv(b), ("stg", b), qr[g], ("qr", g))
            for nm, src, dst in (("vsa", SVS, vsa), ("vwa", SVW, vwa)):
                b = nxt()
                k.dma(stgv(b).rearrange("p (t d) -> p t d", d=128),
                      src[:, hk * 128:(hk + 1) * 128].rearrange("(t p) d -> p t d", p=128), [], [("stg", b)])
                k.cp("dve", dst[:, :, 0:128], stgv(b).rearrange("p (t d) -> p t d", d=128), [("stg", b)], [nm])
            k.dma(sgr[:], SNG[:, hk * 12:(hk + 1) * 12].rearrange("(t p) c -> p t c", p=128), [], ["sgr"])
            k.act(sg[:], sgr[:], AF.Tanh, ["sgr"], ["sg"], scale=0.5)
            k.ts("dve", sg[:], sg[:], 0.5, 0.5, ALU.mult, ALU.add, ["sg"], ["sg"])
            compress("k", kcb, "kcb")
            compress("v", vcb, "vcb")

            for qc in range(4):
                qsl = slice(qc * 512, (qc + 1) * 512)
                par = qc % 2
                ap = accp[par]
                def cmp_a(g):
                    pc = PcT[g % 2]
                    pk = ("pc", g % 2)
                    k.mm(PS(5)[0:127, :], kcT[:, 0:127], qb[g][:, qsl], True, False, ["kcT", ("qb", g)], psk(5))
                    k.mm(PS(5)[0:127, :], ident_b[0:127, 0:127], CMPB[0:127, qsl], False, True, ["identb", "CMPB"], psk(5))
                    k.act(pc[0:127, :], PS(5)[0:127, :], AF.Exp, psk(5), [pk], scale=SCALE)
                    k.mm(PS(5), ones_b[0:127, :], pc[0:127, :], True, True, ["onesb", pk], psk(5))
                    k.ts("dve", rZ[g % 2][:], PS(5), 1e-30, None, ALU.max, None, psk(5), [("rZ", g % 2)])

                def cmp_b(g):
                    pc = PcT[g % 2]
                    pk = ("pc", g % 2)
                    rz = rZ[g % 2]
                    rk = ("rZ", g % 2)
                    k.recip(rz[:], rz[:], [rk], [rk])
                    k.tt("dve", pn[0:127, :], pc[0:127, :], rz[0:127, :], ALU.mult, [pk, rk], ["pn"])
                    k.mm(PS(6)[0:32, :], Aagg[0:127, :], pn[0:127, :], g == 0, g == 3, ["Aagg", "pn"], psk(6))
                    for qtl in range(4):
                        qt = qc * 4 + qtl
                        obk = OBANKS[ob_i[0] % 3]
                        ob_i[0] += 1
                        k.mm(PS(obk)[:, 0:129], pc[0:127, qtl * 128:(qtl + 1) * 128], vca[0:127, 0:129], True, True, [pk, "vca"], psk(obk))
                        combine(obk, qt, qtl, g, 0, ap, ("acc", par, qtl), True, True)

                cmp_a(0)
                for g in range(4):
                    if g + 1 < 4:
                        cmp_a(g + 1)
                    cmp_b(g)
                k.cp("act", impS[0:32, :], PS(6)[0:32, :], psk(6), ["impS"])
                for qtl in range(4):
                    k.tr(PS(5)[:, qtl * 32:(qtl + 1) * 32], impS[0:32, qtl * 128:(qtl + 1) * 128], ident_f[0:32, 0:32],
                         ["impS", "identf"], psk(5))
                sc3 = sc[:].rearrange("p (a b) -> p a b", b=32)
                k.tt("dve", sc3, PS(5)[:, 0:128].rearrange("p (a b) -> p a b", b=32), AM[:, qc * 4:(qc + 1) * 4, :], ALU.mult,
                     psk(5) + ["AM"], ["sc"])
                k.tt("dve", sc3, sc3, ADDM[:, qc * 4:(qc + 1) * 4, :], ALU.add, ["sc", "ADDM"], ["sc"])
                for qtl in range(4):
                    ssl = sc[:, qtl * 32:(qtl + 1) * 32]
                    k.max8(m8[:, 0:8], ssl, ["sc"], ["m8"])
                    k.matchrep(wk32[:], m8[:, 0:8], ssl, -3e38, ["sc", "m8"], ["wk32"])
                    k.max8(m8[:, 8:16], wk32[:], ["wk32"], ["m8"])
                    k.ts("dve", selb[:, qtl * 32:(qtl + 1) * 32], ssl, m8[:, 15:16], -NEG, ALU.is_ge, ALU.mult, ["sc", "m8"], ["selb"])
                k.ts("dve", selb[:], selb[:], NEG, None, ALU.add, None, ["selb"], ["selb"])
                for qtl in range(4):
                    k.tr(PS(6)[0:32, qtl * 128:(qtl + 1) * 128], selb[:, qtl * 32:(qtl + 1) * 32], ident_f[:], ["selb", "identf"], psk(6))
                k.cp("act", biasT[0:32, qsl], PS(6)[0:32, :], psk(6), ["biasT"])
                branches = []
                for qtl in range(4):
                    qt = qc * 4 + qtl
                    for g in range(4):
                        ktl = [(kt, "caus" if kt == qt else None) for kt in range(qt + 1)]
                        branches.append(dict(kT=ksr, kkey="ksr", va=vsa, vkey="vsa", g=g, qt=qt, qtl=qtl, ktl=ktl, sel=True, br=1, last=False))
                        ktl = [(kt, "caus" if kt == qt else ("winb" if kt == qt - 4 else None)) for kt in range(max(0, qt - 4), qt + 1)]
                        branches.append(dict(kT=kwr, kkey="kwr", va=vwa, vkey="vwa", g=g, qt=qt, qtl=qtl, ktl=ktl, sel=False, br=2, last=(g == 3)))
                groups = []
                for bi, B in enumerate(branches):
                    nk = len(B["ktl"])
                    for g0 in range(0, nk, 4):
                        groups.append(dict(b=bi, kts=B["ktl"][g0:g0 + 4], first=(g0 == 0), lastg=(g0 + 4 >= nk), base=g0))

                def emit_scores(G_):
                    B = branches[G_["b"]]
                    sbk = (0, 1, 4)[sb_i[0] % 3]
                    sb_i[0] += 1
                    G_["sbk"] = sbk
                    qt, g = B["qt"], B["g"]
                    for j, (kt, eb) in enumerate(G_["kts"]):
                        out = PS(sbk)[:, j * 128:(j + 1) * 128]
                        nmm = 1 + (1 if B["sel"] else 0) + (1 if eb else 0)
                        k.mm(out, B["kT"][:, kt * 128:(kt + 1) * 128], qr[g][:, row(qt)], True, nmm == 1, [B["kkey"], ("qr", g)], psk(sbk))
                        i = 1
                        if B["sel"]:
                            i += 1
                            k.mm(out, ESEL[0:32, kt, :], biasT[0:32, row(qt)], False, i == nmm, ["ESEL", "biasT"], psk(sbk))
                        if eb:
                            k.mm(out, ident_b[:], (CAUS if eb == "caus" else WINB)[:], False, True, ["identb", "CAUS", "WINB"], psk(sbk))

                def emit_exp_pv(G_):
                    B = branches[G_["b"]]
                    sbk = G_["sbk"]
                    if G_["first"]:
                        B["obk"] = OBANKS[ob_i[0] % 3]
                        ob_i[0] += 1
                    obk = B["obk"]
                    n = len(G_["kts"]) * 128
                    pi = pt_i[0] % 3
                    pt_i[0] += 1
                    k.act(PT[pi][:, 0:n], PS(sbk)[:, 0:n], AF.Exp, psk(sbk), [("PT", pi)], scale=SCALE)
                    nk = len(B["ktl"])
                    for j, (kt, eb) in enumerate(G_["kts"]):
                        idx = G_["base"] + j
                        k.mm(PS(obk)[:, 0:129], PT[pi][:, j * 128:(j + 1) * 128], B["va"][:, kt, :], idx == 0, idx == nk - 1,
                             [("PT", pi), B["vkey"]], psk(obk))
                    if G_["lastg"]:
                        akey = ("acc", par, B["qtl"])
                        combine(obk, B["qt"], B["qtl"], B["g"], B["br"], ap, akey, False, False)
                        if B["last"]:
                            k.dma(SOB[row(B["qt"]), hk * 512:(hk + 1) * 512], ap[:, B["qtl"]].rearrange("p g d -> p (g d)"), [akey],
                                  [("sob", hk, B["qt"])])

                emit_scores(groups[0])
                if len(groups) > 1:
                    emit_scores(groups[1])
                for gi_ in range(len(groups)):
                    if gi_ + 2 < len(groups):
                        emit_scores(groups[gi_ + 2])
                    emit_exp_pv(groups[gi_])
        k.release()
        k.barrier()

    if stop_after >= 4:
        k.mark()
        wo = k.sb([128, 16, D], BF16, "wo")
        k.mark()
        stg4 = k.sb([128, 16, 512], F32, "stg4")
        for c in range(4):
            k.dma(stg4[:], w_out[:, c * 512:(c + 1) * 512].rearrange("(kc p) n -> p kc n", p=128), [], ["stg4"])
            k.cp("dve", wo[:, 0:8, c * 512:(c + 1) * 512], stg4[:, 0:8, :], ["stg4"], ["wo"])
            k.cp("act", wo[:, 8:16, c * 512:(c + 1) * 512], stg4[:, 8:16, :], ["stg4"], ["wo"])
        k.release()
        obt = [k.sb([128, D], F32, "obt") for _ in range(2)]
        mat = [k.sb([128, D], F32, "mat") for _ in range(2)]
        gbt = [k.sb([128, D], F32, "gbt") for _ in range(2)]
        xtt = [k.sb([128, D], F32, "xtt") for _ in range(2)]
        mb = k.sb([128, D], BF16, "mb")
        mT = k.sb([128, 16, 128], BF16, "mT")
        x1o = [k.sb([128, D], F32, "x1o") for _ in range(2)]

        def load4(t):
            p = t % 2
            k.dma(obt[p][:], SOB[row(t), :], [], [("obt", p)])
            k.dma(mat[p][:], SMA[row(t), :], [], [("mat", p)])
            k.dma(gbt[p][:], SMG[row(t), D:2 * D], [], [("gbt", p)])
            k.dma(xtt[p][:], x[row(t), :], [], [("xtt", p)])

        load4(0)
        for t in range(NT):
            p = t % 2
            if t + 1 < NT:
                load4(t + 1)
            k.act(gbt[p][:], gbt[p][:], AF.Tanh, [("gbt", p)], [("gbt", p)], scale=0.5)
            k.ts("dve", gbt[p][:], gbt[p][:], 0.5, 0.5, ALU.mult, ALU.add, [("gbt", p)], [("gbt", p)])
            k.tt("dve", obt[p][:], obt[p][:], gbt[p][:], ALU.mult, [("obt", p), ("gbt", p)], [("obt", p)])
            k.tt("dve", mb[:], obt[p][:], mat[p][:], ALU.add, [("obt", p), ("mat", p)], ["mb"])
            for kc in range(16):
                b = kc // 8
                k.tr(PSB(b)[:, (kc % 8) * 128:(kc % 8 + 1) * 128], mb[:, kc * 128:(kc + 1) * 128], ident_b[:], ["mb", "identb"], psk(b))
            for hh in range(2):
                k.cp("dve" if hh == 0 else "act", mT[:, hh * 8:(hh + 1) * 8, :], PSB(hh).rearrange("p (a b) -> p a b", b=128),
                     psk(hh), ["mT"])
            for c in range(4):
                b = 2 + c
                for kc in range(16):
                    k.mm(PS(b), mT[:, kc, :], wo[:, kc, c * 512:(c + 1) * 512], kc == 0, kc == 15, ["mT", "wo"], psk(b))
                k.tt("dve", x1o[p][:, c * 512:(c + 1) * 512], PS(b), xtt[p][:, c * 512:(c + 1) * 512], ALU.add,
                     psk(b) + [("xtt", p)], [("x1o", p)])
            k.dma(SX1[row(t), :], x1o[p][:], [("x1o", p)], [("sx1", t)])
        k.release()
        k.barrier()

    if stop_after >= 5:
        k.mark()
        eidT_all = k.sb([128, 16, 128], I32, "eidTall")
        gateT_all = k.sb([128, 16, 128], F32, "gateTall")
        iota16 = k.sb([128, 16], F32, "iota16")
        k.dma(iota16[:], cin["c_iota16"], [], ["iota16"])
        k.mark()
        wq = k.sb([128, 16, D], BF16, "wq")
        k.mark()
        stg5 = k.sb([128, 16, 512], F32, "stg5")
        for c in range(4):
            k.dma(stg5[:], peer_w_q[:, c * 512:(c + 1) * 512].rearrange("(kc p) n -> p kc n", p=128), [], ["stg5"])
            k.cp("dve", wq[:, 0:8, c * 512:(c + 1) * 512], stg5[:, 0:8, :], ["stg5"], ["wq"])
            k.cp("act", wq[:, 8:16, c * 512:(c + 1) * 512], stg5[:, 8:16, :], ["stg5"], ["wq"])
        k.release()
        kTf = [k.sb([128, 128], F32, "kTf") for _ in range(2)]
        g2b = k.sb([128, D], F32, "g2b")
        x1a = [k.sb([128, D], F32, "x1a") for _ in range(2)]
        h2a = [k.sb([128, D], BF16, "h2a") for _ in range(2)]
        jk = k.sb([128, D], BF16, "jk")
        h2T = k.sb([128, 16, 128], BF16, "h2T")
        qTs = k.sb([128, 16, 128], F32, "qTs")
        Sxa = [k.sb([128, 16, 128], F32, "Sxa") for _ in range(2)]
        sm5 = [k.sb([128, 4], F32, "sm5") for _ in range(2)]
        cand = k.sb([128, 8, 16, 16], F32, "cand"); eqt = k.sb([128, 8, 16, 16], F32, "eqt")
        V16 = k.sb([128, 16, 16], F32, "V16"); I16u = k.sb([128, 16, 16], U32, "I16u"); I16f = k.sb([128, 16, 16], F32, "I16f")
        wk = k.sb([128, 128], F32, "wk"); wk2 = k.sb([128, 256], F32, "wk2")
        T16 = k.sb([128, 8, 16], F32, "T16"); P16u = k.sb([128, 8, 16], U32, "P16u")
        A16u = k.sb([128, 8, 16], U32, "A16u"); B16u = k.sb([128, 8, 16], U32, "B16u")
        af = k.sb([128, 8, 16], F32, "af"); bf = k.sb([128, 8, 16], F32, "bf")
        ex = k.sb([128, 8, 16], F32, "ex"); gate = k.sb([128, 8, 16], F32, "gate")
        zs = k.sb([128, 8], F32, "zs")
        i1f = k.sb([128, 8, 16], F32, "i1f"); i2f = k.sb([128, 8, 16], F32, "i2f"); eid = k.sb([128, 8, 16], F32, "eid")
        io_b = iota16[:].unsqueeze(1).unsqueeze(1).to_broadcast([128, 8, 16, 16])
        V16v = V16[:].rearrange("p (h two) a -> p h two a", two=2)
        I16v = I16f[:].rearrange("p (h two) a -> p h two a", two=2)
        k.dma(kTf[0][:], keysT[0], [], ["kTf"]); k.dma(kTf[1][:], keysT[1], [], ["kTf"])
        k.dma(g2b[:], norm_ffn_g.partition_broadcast(128), [], ["g2b"])
        def front(t):
            p = t % 2
            kp = "n5%d" % p
            k.dma(x1a[p][:], SX1[row(t), :], [], [("x1a", p)])
            k.act(jk[:], x1a[p][:], AF.Square, [("x1a", p)], ["jk", kp + "ss"], accum=sm5[p][:, 0:1])
            k.rstd_from_ss(sm5[p][:, 0:1], sm5[p][:, 2:3], sm5[p][:, 1:2], nh[:], D, kp)
            k.stt(h2a[p][:], x1a[p][:], sm5[p][:, 2:3], g2b[:], ALU.mult, ALU.mult, [("x1a", p), kp + "rstd", "g2b"], [("h2a", p)])
            k.dma(SH2[row(t), :], h2a[p][:], [("h2a", p)], [("sh2", t)])
            for kc in range(16):
                b = kc // 8
                k.tr(PSB(b)[:, (kc % 8) * 128:(kc % 8 + 1) * 128], h2a[p][:, kc * 128:(kc + 1) * 128], ident_b[:], [("h2a", p), "identb"], psk(b))
            for hh in range(2):
                k.cp("dve" if hh == 0 else "act", h2T[:, hh * 8:(hh + 1) * 8, :], PSB(hh).rearrange("p (a b) -> p a b", b=128),
                     psk(hh), ["h2T"])
            for cb in range(16):
                b = 4 + cb // 4
                for kc in range(16):
                    k.mm(PS(b)[:, (cb % 4) * 128:(cb % 4 + 1) * 128], wq[:, kc, cb * 128:(cb + 1) * 128], h2T[:, kc, :], kc == 0, kc == 15,
                         ["wq", "h2T"], psk(b))
            k.cp("act", qTs[:].rearrange("p a b -> p (a b)"), PS(4, 4), psk(4, 4), ["qTs"])
            for cb in range(16):
                b = 4 + cb // 4
                k.mm(PS(b)[:, (cb % 4) * 128:(cb % 4 + 1) * 128], qTs[:, cb, :], kTf[cb % 2][:], True, True, ["qTs", "kTf"], psk(b))
            k.cp("act", Sxa[p][:].rearrange("p a b -> p (a b)"), PS(4, 4), psk(4, 4), [("Sxa", p)])

        def topk(t):
            eT, gT = eidT_all[:, t, :], gateT_all[:, t, :]
            ek, gk = ("eidT", t), ("gateT", t)
            Sx = Sxa[t % 2]
            sxk = ("Sxa", t % 2)
            for hb_ in range(16):
                k.max8(V16[:, hb_, 0:8], Sx[:, hb_, :], [sxk], ["V16"])
                k.maxidx(I16u[:, hb_, 0:8], V16[:, hb_, 0:8], Sx[:, hb_, :], [sxk, "V16"], ["I16u"])
                k.matchrep(wk[:], V16[:, hb_, 0:8], Sx[:, hb_, :], -3e38, [sxk, "V16"], ["wk"])
                k.max8(V16[:, hb_, 8:16], wk[:], ["wk"], ["V16"])
                k.maxidx(I16u[:, hb_, 8:16], V16[:, hb_, 8:16], wk[:], ["wk", "V16"], ["I16u"])
            k.cp("dve", I16f[:], I16u[:], ["I16u"], ["I16f"])
            k.tt("dve", cand[:], V16v[:, :, 0, :].unsqueeze(3).to_broadcast([128, 8, 16, 16]),
                 V16v[:, :, 1, :].unsqueeze(2).to_broadcast([128, 8, 16, 16]), ALU.add, ["V16"], ["cand"])
            for h in range(8):
                ch = cand[:, h].rearrange("p a b -> p (a b)")
                k.max8(T16[:, h, 0:8], ch, ["cand"], ["T16"])
                k.maxidx(P16u[:, h, 0:8], T16[:, h, 0:8], ch, ["cand", "T16"], ["P16u"])
                k.matchrep(wk2[:], T16[:, h, 0:8], ch, -3e38, ["cand", "T16"], ["wk2"])
                k.max8(T16[:, h, 8:16], wk2[:], ["wk2"], ["T16"])
                k.maxidx(P16u[:, h, 8:16], T16[:, h, 8:16], wk2[:], ["wk2", "T16"], ["P16u"])
            k.op("dve", lambda e: e.tensor_single_scalar(out=A16u[:], in_=P16u[:], scalar=4, op=ALU.logical_shift_right), ["P16u"], ["A16u"])
            k.op("dve", lambda e: e.tensor_single_scalar(out=B16u[:], in_=P16u[:], scalar=15, op=ALU.bitwise_and), ["P16u"], ["B16u"])
            k.cp("dve", af[:], A16u[:], ["A16u"], ["af"])
            k.cp("dve", bf[:], B16u[:], ["B16u"], ["bf"])
            for (pf, col, dst, dk) in ((af, 0, i1f, "i1f"), (bf, 1, i2f, "i2f")):
                k.tt("dve", eqt[:], io_b, pf[:].unsqueeze(3).to_broadcast([128, 8, 16, 16]), ALU.is_equal, ["iota16", "af", "bf"], ["eqt"])
                k.tt("dve", eqt[:], eqt[:], I16v[:, :, col, :].unsqueeze(2).to_broadcast([128, 8, 16, 16]), ALU.mult, ["eqt", "I16f"], ["eqt"])
                k.rsum(dst[:], eqt[:], ["eqt"], [dk])
            k.stt(eid[:], i1f[:], 128.0, i2f[:], ALU.mult, ALU.add, ["i1f", "i2f"], ["eid"])
            k.ts("dve", eid[:], eid[:], 0.0, 16383.0, ALU.max, ALU.min, ["eid"], ["eid"])
            k.tr(PS(2)[:, 0:128], eid[:].rearrange("p a b -> p (a b)"), ident_f[:], ["eid", "identf"], psk(2))
            k.cp("dve", eT, PS(2)[:, 0:128], psk(2), [ek])
            k.tt("dve", ex[:], T16[:], T16[:, :, 0:1].to_broadcast([128, 8, 16]), ALU.subtract, ["T16"], ["ex"])
            k.act(ex[:], ex[:], AF.Exp, ["ex"], ["ex"])
            k.rsum(zs[:], ex[:], ["ex"], ["zs"])
            k.recip(zs[:], zs[:], ["zs"], ["zs"])
            k.tt("dve", gate[:], ex[:], zs[:].unsqueeze(2).to_broadcast([128, 8, 16]), ALU.mult, ["ex", "zs"], ["gate"])
            k.tr(PS(2)[:, 128:256], gate[:].rearrange("p a b -> p (a b)"), ident_f[:], ["gate", "identf"], psk(2))
            k.cp("dve", gT, PS(2)[:, 128:256], psk(2), [gk])


        front(0)
        for t in range(NT):
            if t + 1 < NT:
                front(t + 1)
            topk(t)
        k.release()
        k.barrier()

    if stop_after >= 5:
        k.mark()
        gfb = k.sb([128, D], F32, "gfb")
        SELR = k.sb([128, 128, 128], BF16, "SELR")
        IDm = k.sb([128, 128, 128], BF16, "IDm")
        WSEL = k.sb([128, 128, 128], BF16, "WSEL")
        Ubuf = [k.sb([128, D], F32, "Ubuf") for _ in range(2)]
        Vbuf = [k.sb([128, D], F32, "Vbuf") for _ in range(2)]
        Vbf = [k.sb([128, D], BF16, "Vbf") for _ in range(2)]
        x1t = k.sb([128, D], F32, "x1t")
        yo = k.sb([128, D], F32, "yo")
        h2b = k.sb([128, D], BF16, "h2b")
        jk = k.sb([128, 1024], BF16, "jk")
        A0 = k.sb([128, 128], F32, "A0"); A1 = k.sb([128, 128], F32, "A1")
        At = k.sb([128, 128], F32, "At"); Wt = k.sb([128, 128], F32, "Wt")
        g1 = k.sb([128, 128], F32, "g1"); g2 = k.sb([128, 128], F32, "g2")
        sm6 = k.sb([128, 8], F32, "sm6")

        k.dma(gfb[:], norm_final_g.partition_broadcast(128), [], ["gfb"])
        k.cp("dve", SELR[:], ident_f[:].unsqueeze(2).to_broadcast([128, 128, 128]), ["identf"], ["SELR"])
        idflat = cin["c_ident"].rearrange("a b -> (a b)")
        for c in range(8):
            k.dma(Ubuf[c % 2][:, :], idflat[c * 2048:(c + 1) * 2048].partition_broadcast(128), [], [("G", c % 2)])
            k.cp("dve", IDm[:, c * 16:(c + 1) * 16, :].rearrange("p a b -> p (a b)"), Ubuf[c % 2][:, :], [("G", c % 2)], ["IDm"])
        k.memset("dve", A0[:], 0.0, ["A0"])
        k.memset("dve", A1[:], 0.0, ["A1"])
        gi = [0]

        G = Ubuf + Vbuf + [k.sb([128, D], F32, "Gx") for _ in range(2)]
        NG_ = len(G)
        _unused = None

        def stage1(t):
            eT, ek = eidT_all[:, t, :], ("eidT", t)
            for tk in range(128):
                i = gi[0] % NG_
                gi[0] += 1
                k.op("pool", (lambda e, i=i, tk=tk: e.indirect_dma_start(
                    out=G[i][:, :], out_offset=None, in_=peer_u,
                    in_offset=bass.IndirectOffsetOnAxis(ap=eT[:, tk:tk + 1], axis=0))), [ek, ("gateT", t), "yo", "IDm", "SELR", "A0", "A1"], [("G", i)], dma=True)
                for c in range(4):
                    k.mm(PS(4 + c), SELR[:, tk, :], h2b[:, c * 512:(c + 1) * 512], True, True, ["SELR", "h2b"], psk(4 + c))
                for hf, Ax in ((0, A0), (1, A1)):
                    k.stt(jk[:, 0:1024], G[i][:, hf * 1024:(hf + 1) * 1024], 1.0, PS(4 + 2 * hf, 2), ALU.mult, ALU.mult,
                          [("G", i)] + psk(4 + 2 * hf, 2), ["jk", "A%d" % hf], accum=Ax[:, tk:tk + 1])

        def post1(t):
            gk = ("gateT", t)
            k.tt("dve", At[:], A0[:], A1[:], ALU.add, ["A0", "A1"], ["At"])
            k.act(g1[:], At[:], AF.Square, ["At"], ["g1"])
            k.ts("dve", g1[:], g1[:], 0.044715, 1.0, ALU.mult, ALU.add, ["g1"], ["g1"])
            k.tt("dve", g1[:], g1[:], At[:], ALU.mult, ["g1", "At"], ["g1"])
            k.act(g2[:], g1[:], AF.Tanh, ["g1"], ["g2"], scale=GC)
            k.ts("dve", g2[:], g2[:], 0.5, 0.5, ALU.mult, ALU.add, ["g2"], ["g2"])
            k.tt("dve", g2[:], g2[:], At[:], ALU.mult, ["g2", "At"], ["g2"])
            k.tt("dve", Wt[:], g2[:], gateT_all[:, t, :], ALU.mult, ["g2", gk], ["Wt"])
            k.tt("dve", WSEL[:], IDm[:], Wt[:].unsqueeze(2).to_broadcast([128, 128, 128]), ALU.mult, ["IDm", "Wt"], ["WSEL"])

        def stage2(t):
            eT, ek = eidT_all[:, t, :], ("eidT", t)
            for tk in range(128):
                i = gi[0] % NG_
                gi[0] += 1
                k.op("pool", (lambda e, i=i, tk=tk: e.indirect_dma_start(
                    out=G[i][:, :], out_offset=None, in_=peer_v,
                    in_offset=bass.IndirectOffsetOnAxis(ap=eT[:, tk:tk + 1], axis=0))), [ek, "WSEL"], [("G", i)], dma=True)
                j = tk % 2
                k.cp("act", Vbf[j][:], G[i][:], [("G", i)], [("Vbf", j)])
                for c in range(4):
                    k.mm(PS(c), WSEL[:, tk, :], Vbf[j][:, c * 512:(c + 1) * 512], tk == 0, tk == 127, ["WSEL", ("Vbf", j)], psk(c))

        def final(t):
            k.dma(x1t[:], SX1[row(t), :], [], ["x1t"])
            k.tt("dve", x1t[:], PS(0, 4), x1t[:], ALU.add, psk(0, 4) + ["x1t"], ["x1t"])
            k.act(yo[:], x1t[:], AF.Square, ["x1t"], ["yo", "n6ss"], accum=sm6[:, 4:5])
            k.rstd_from_ss(sm6[:, 4:5], sm6[:, 6:7], sm6[:, 5:6], nh[:], D, "n6")
            k.stt(yo[:], x1t[:], sm6[:, 6:7], gfb[:], ALU.mult, ALU.mult, ["x1t", "n6rstd", "gfb"], ["yo"])
            k.dma(y[row(t), :], yo[:], ["yo"], [("y", t)])

        for t in range(NT):
            k.dma(h2b[:], SH2[row(t), :], [], ["h2b"])
            stage1(t)
            post1(t)
            stage2(t)
            final(t)
        k.release()
        k.release()

    k.emit()
    return nc, k


_CACHE = {}


def make_in_maps(inputs):
    f = lambda a: np.ascontiguousarray(np.asarray(a, dtype=np.float32))
    consts = make_consts()
    shared = {
        "norm_mix_g": f(inputs["norm_mix_g"][0:1]),
        "w_in": f(inputs["w_in"][0]),
        "w_out": f(inputs["w_out"][0]),
        "gm_ln_g": f(inputs["gm_ln_g"][0:1]),
        "gm_ln_b": f(inputs["gm_ln_b"][0:1]),
        "gm_wT": f(np.transpose(np.asarray(inputs["gm_spatial_w"][0]), (2, 0, 1))),
        "gm_bT": f(np.transpose(np.asarray(inputs["gm_spatial_b"][0]), (1, 0))),
        "cmp_posT_k": f(np.asarray(inputs["cmp_pos_k"][0]).T),
        "cmp_posT_v": f(np.asarray(inputs["cmp_pos_v"][0]).T),
        "cmp_w1_k": f(inputs["cmp_w1_k"][0]), "cmp_w1_v": f(inputs["cmp_w1_v"][0]),
        "cmp_w2_k": f(inputs["cmp_w2_k"][0]), "cmp_w2_v": f(inputs["cmp_w2_v"][0]),
        "norm_ffn_g": f(inputs["norm_ffn_g"][0:1]),
        "peer_w_q": f(inputs["peer_w_q"][0]),
        "keys1T": f(np.asarray(inputs["peer_keys1"][0]).T),
        "keys2T": f(np.asarray(inputs["peer_keys2"][0]).T),
        "peer_u": f(inputs["peer_u"][0]),
        "peer_v": f(inputs["peer_v"][0]),
        "norm_final_g": f(np.asarray(inputs["norm_final_g"]).reshape(1, D)),
    }
    shared.update(consts)
    xs = np.asarray(inputs["x"], dtype=np.float32)
    maps = []
    for b in range(8):
        m = dict(shared)
        m["x"] = np.ascontiguousarray(xs[b])
        maps.append(m)
    return maps


def kernel(**inputs):
    if "nc" not in _CACHE:
        _CACHE["nc"] = build_program()[0]
    nc = _CACHE["nc"]
    in_maps = make_in_maps(inputs)
    res = run_bass_kernel_spmd(nc, in_maps, core_ids=list(range(8)))
    out = np.stack([np.asarray(r["y"]) for r in res.results], axis=0)
    return out.astype(np.float32)
```

```python
import numpy as np
import concourse.bass as bass
import concourse.mybir as mybir
from concourse.bass_utils import run_bass_kernel_spmd

F32 = mybir.dt.float32
BF16 = mybir.dt.bfloat16
I32 = mybir.dt.int32
U32 = mybir.dt.uint32
ALU = mybir.AluOpType
AF = mybir.ActivationFunctionType
AX = mybir.AxisListType

ENGS = ("pe", "act", "dve", "pool", "sp")
EPOCH = 30000
NDMASEM = 8
SB_BASE = 16512
SB_TOP = 229344


class Op:
    __slots__ = ("eng", "fn", "deps", "is_dma", "idx", "sig", "semref")

    def __init__(self, eng, fn, is_dma):
        self.eng = eng
        self.fn = fn
        self.deps = []
        self.is_dma = is_dma
        self.sig = False
        self.semref = None


class Prog:
    def __init__(self, nc):
        self.nc = nc
        self.ops = []
        self.last_w = {}
        self.readers = {}
        self.last_on_eng = {e: None for e in ENGS}
        self.barrier_deps = []
        self.need_barrier = {e: False for e in ENGS}
        self.sb_off = SB_BASE
        self.marks = []
        self.names = 0
        self.recent_dma = {}

    def sb(self, shape, dtype, name=None):
        self.names += 1
        name = (name or "t") + "_%d" % self.names
        esz = {F32: 4, BF16: 2, I32: 4, U32: 4}[dtype]
        n = 1
        for s in shape[1:]:
            n *= s
        nbytes = (n * esz + 31) // 32 * 32
        off = self.sb_off
        self.sb_off += nbytes
        assert self.sb_off <= SB_TOP, ("SBUF overflow", name, self.sb_off)
        return self.nc.alloc_sbuf_tensor_at(name, list(shape), dtype, offset=off)

    def mark(self):
        self.marks.append(self.sb_off)

    def release(self):
        self.sb_off = self.marks.pop()
        self.barrier()

    def barrier(self):
        self.barrier_deps = [self.last_on_eng[e] for e in ENGS if self.last_on_eng[e] is not None]
        for q in self.recent_dma.values():
            self.barrier_deps.extend(q)
        for e in ENGS:
            self.need_barrier[e] = True

    def op(self, eng, fn, reads=(), writes=(), dma=False):
        o = Op(eng, fn, dma or eng == "sp")
        o.idx = len(self.ops)
        deps = set()
        if self.need_barrier[eng]:
            deps.update(self.barrier_deps)
            self.need_barrier[eng] = False
        reads = list(reads)
        writes = list(writes)
        for k in list(reads):
            if isinstance(k, str) and k.startswith("ps"):
                reads.remove(k)
                if k not in writes:
                    writes.append(k)
        for k in reads:
            w = self.last_w.get(k)
            if w is not None:
                deps.add(w)
        for k in writes:
            w = self.last_w.get(k)
            if w is not None:
                deps.add(w)
            rd = self.readers.get(k)
            if rd:
                for v in rd.values():
                    if isinstance(v, list):
                        deps.update(v)
                    else:
                        deps.add(v)
        for k in reads:
            rd = self.readers.setdefault(k, {})
            if o.is_dma:
                rd.setdefault("dma_" + eng, []).append(o.idx)
            else:
                rd[eng] = o.idx
        for k in writes:
            self.last_w[k] = o.idx
            self.readers[k] = {}
        deps.discard(o.idx)
        o.deps = sorted(deps)
        self.ops.append(o)
        self.last_on_eng[eng] = o.idx
        if o.is_dma:
            q = self.recent_dma.setdefault(eng, [])
            q.append(o.idx)
            if len(q) > NDMASEM:
                q.pop(0)
        return o

    def emit(self):
        nc = self.nc
        ops = self.ops
        for o in ops:
            latest = {}
            keep = []
            for d in o.deps:
                p = ops[d]
                if p.is_dma:
                    keep.append(d)
                elif p.eng == "pe" and o.eng == "pe":
                    continue
                else:
                    if d > latest.get(p.eng, -1):
                        latest[p.eng] = d
            keep.extend(latest.values())
            o.deps = sorted(keep)
            for d in o.deps:
                ops[d].sig = True
        sems = {}

        def getsem(key):
            if key not in sems:
                sems[key] = nc.alloc_semaphore("s_%s_%s" % key)
            return sems[key]

        cnt = {e: 0 for e in ENGS}
        dcnt = {}
        dper = {}
        prev_on_dsem = {}
        for o in ops:
            if o.is_dma:
                q = o.eng
                i = dcnt.get(q, 0)
                dcnt[q] = i + 1
                j = i % NDMASEM
                key = ("d" + q, j)
                c = dper.get(key, 0) + 1
                dper[key] = c
                o.semref = (key, 16 * c)
                if c > 1:
                    o.deps = sorted(set(o.deps) | {prev_on_dsem[key]})
                prev_on_dsem[key] = o.idx
                o.sig = True
            elif o.sig:
                kk = cnt[o.eng]
                cnt[o.eng] = kk + 1
                o.semref = ((o.eng, kk // EPOCH), (kk % EPOCH) + 1)
        for o in ops:
            if o.semref is not None:
                getsem(o.semref[0])
        self.stats = dict(nops=len(ops), nsig=sum(1 for o in ops if o.sig), cnt=dict(cnt), dcnt=dict(dcnt),
                          nsem=len(sems))
        per_eng = {e: [o for o in ops if o.eng == e] for e in ENGS}
        final_dma = {key: 16 * c for key, c in dper.items()}

        def run(engname, eng):
            waited = {}
            for o in per_eng[engname]:
                for d in o.deps:
                    p = ops[d]
                    if p.semref is None:
                        continue
                    if p.eng == "pe" and o.eng == "pe" and not p.is_dma:
                        continue
                    key, val = p.semref
                    if waited.get(key, 0) >= val:
                        continue
                    waited[key] = val
                    eng.wait_ge(sems[key], val)
                inst = o.fn(eng)
                if o.semref is not None:
                    key, val = o.semref
                    inst.then_inc(sems[key], 16 if o.is_dma else 1)
            if engname == "sp":
                for key, val in final_dma.items():
                    if waited.get(key, 0) < val:
                        eng.wait_ge(sems[key], val)

        with nc.Block() as block:
            @block.tensor
            def _(e):
                run("pe", e)

            @block.scalar
            def _(e):
                run("act", e)

            @block.vector
            def _(e):
                run("dve", e)

            @block.gpsimd
            def _(e):
                run("pool", e)

            @block.sync
            def _(e):
                run("sp", e)


class K(Prog):
    def mm(self, out, lhsT, rhs, start, stop, r, w):
        return self.op("pe", lambda e: e.matmul(out=out, lhsT=lhsT, rhs=rhs, start=start, stop=stop), r, w)

    def tr(self, out, in_, ident, r, w):
        return self.op("pe", lambda e: e.transpose(out=out, in_=in_, identity=ident), r, w)

    def act(self, out, in_, func, r, w, scale=1.0, bias=None, accum=None):
        def f(e):
            kw = dict(out=out, in_=in_, func=func, scale=scale)
            if bias is not None:
                kw["bias"] = bias
            if accum is not None:
                kw["accum_out"] = accum
            return e.activation(**kw)
        return self.op("act", f, r, w)

    def tt(self, eng, out, a, b, op, r, w):
        return self.op(eng, lambda e: e.tensor_tensor(out=out, in0=a, in1=b, op=op), r, w)

    def ts(self, eng, out, a, s1, s2, op0, op1, r, w):
        if s2 is None:
            return self.op(eng, lambda e: e.tensor_scalar(out=out, in0=a, scalar1=s1, scalar2=None, op0=op0), r, w)
        return self.op(eng, lambda e: e.tensor_scalar(out=out, in0=a, scalar1=s1, scalar2=s2, op0=op0, op1=op1), r, w)

    def stt(self, out, a, s, b, op0, op1, r, w, accum=None):
        def f(e):
            kw = dict(out=out, in0=a, scalar=s, in1=b, op0=op0, op1=op1)
            if accum is not None:
                kw["accum_out"] = accum
            return e.scalar_tensor_tensor(**kw)
        return self.op("dve", f, r, w)

    def cp(self, eng, out, in_, r, w):
        if eng == "act":
            return self.op("act", lambda e: e.copy(out=out, in_=in_), r, w)
        return self.op(eng, lambda e: e.tensor_copy(out=out, in_=in_), r, w)

    def recip(self, out, in_, r, w):
        return self.op("dve", lambda e: e.reciprocal(out=out, in_=in_), r, w)

    def rsum(self, out, in_, r, w):
        return self.op("dve", lambda e: e.reduce_sum(out=out, in_=in_, axis=AX.X), r, w)

    def max8(self, out, in_, r, w):
        return self.op("dve", lambda e: e.max(out=out, in_=in_), r, w)

    def maxidx(self, out, in_max, in_values, r, w):
        return self.op("dve", lambda e: e.max_index(out=out, in_max=in_max, in_values=in_values), r, w)

    def matchrep(self, out, rep, vals, imm, r, w):
        return self.op("dve", lambda e: e.match_replace(out=out, in_to_replace=rep, in_values=vals, imm_value=imm), r, w)

    def memset(self, eng, ap, val, w):
        return self.op(eng, lambda e: e.memset(ap, val), (), w)

    def dma(self, out, in_, r, w):
        return self.op("sp", lambda e: e.dma_start(out=out, in_=in_), r, w)

    def rstd_from_ss(self, ss, rstd, tmp, nh, n, key):
        self.ts("dve", tmp, ss, 1.0 / n, 1e-6, ALU.mult, ALU.add, [key + "ss"], [key + "ms"])
        self.tt("pool", rstd, tmp, nh, ALU.pow, [key + "ms", "nh"], [key + "rstd"])

S = 2048
D = 2048
NT = 16
GC = 0.7978845608028654
SCALE = 128 ** -0.5
NEG = -30000.0
C_U, C_V, C_Q, C_KV, C_NG, C_MG = 0, 2048, 4096, 6144, 9216, 9264

CONST_SHAPES = {
    "c_cos": [128, S], "c_sin": [128, S], "c_rt": [128, 128], "c_caus": [128, 128], "c_winb": [128, 128],
    "c_cmpb": [128, S], "c_aagg": [128, 32], "c_am": [128, 16, 32], "c_add": [128, 16, 32],
    "c_esel": [32, 16, 128], "c_tril": [128, 16, 128], "c_ident": [128, 128], "c_iota16": [128, 16],
    "c_zrow": [128, 255],
}


def make_consts():
    c = {}
    half = 64
    inv = 10000.0 ** (-np.arange(half, dtype=np.float32) / half)
    ang = np.arange(S, dtype=np.float32)[:, None] * inv[None, :]
    cos = np.cos(ang).astype(np.float32).T
    sin = np.sin(ang).astype(np.float32).T
    c["c_cos"] = np.concatenate([cos, cos], 0)
    c["c_sin"] = np.concatenate([sin, sin], 0)
    rt = np.zeros((128, 128), np.float32)
    for m in range(128):
        if m < 64:
            rt[m + 64, m] = -1.0
        else:
            rt[m - 64, m] = 1.0
    c["c_rt"] = rt
    kk = np.arange(128)[:, None]
    qq = np.arange(128)[None, :]
    c["c_caus"] = np.where(kk <= qq, 0.0, NEG).astype(np.float32)
    c["c_winb"] = np.where(kk > qq, 0.0, NEG).astype(np.float32)
    n = np.arange(128)[:, None]
    q = np.arange(S)[None, :]
    c["c_cmpb"] = np.where((16 * n + 31 <= q) & (n < 127), 0.0, NEG).astype(np.float32)
    agg = np.array([1, 2, 2, 2, 1], np.float32)
    a = np.zeros((128, 32), np.float32)
    for nn in range(127):
        for j in range(32):
            w = nn + 1 - 4 * j
            if 0 <= w <= 4:
                a[nn, j] = agg[w]
    c["c_aagg"] = a
    pos = (np.arange(16)[None, :, None] * 128 + np.arange(128)[:, None, None])
    j = np.arange(32)[None, None, :]
    allowed = (j * 64) <= pos
    forced = (j == 0) | (j == (pos // 64))
    c["c_am"] = (allowed & ~forced).astype(np.float32)
    c["c_add"] = np.where(forced, 1e30, np.where(allowed, 0.0, -1e30)).astype(np.float32)
    e = np.zeros((32, 16, 128), np.float32)
    for kt in range(16):
        for key in range(128):
            e[2 * kt + key // 64, kt, key] = 1.0
    c["c_esel"] = e
    s_ = np.arange(128)[:, None, None]
    t_ = np.arange(128)[None, None, :]
    c["c_tril"] = np.broadcast_to((s_ <= t_), (128, 16, 128)).astype(np.float32).copy()
    c["c_ident"] = np.eye(128, dtype=np.float32)
    c["c_iota16"] = np.broadcast_to(np.arange(16, dtype=np.float32)[None, :], (128, 16)).copy()
    z = np.zeros((128, 255), np.float32)
    z[:, 127] = 1.0
    c["c_zrow"] = z
    return c


def build_program(stop_after=99, dbg=()):
    nc = bass.Bass("TRN2", target_bir_lowering=False)
    k = K(nc)

    def din(name, shape):
        return nc.dram_tensor(name, list(shape), F32, kind="ExternalInput").ap()

    def dscr(name, shape, dt=F32):
        kind = "ExternalOutput" if name in dbg else "Internal"
        return nc.dram_tensor(name, list(shape), dt, kind=kind).ap()

    x = din("x", [S, D])
    norm_mix_g = din("norm_mix_g", [1, D])
    w_in = din("w_in", [D, 13360])
    w_out = din("w_out", [D, D])
    gm_ln_g = din("gm_ln_g", [1, D])
    gm_ln_b = din("gm_ln_b", [1, D])
    gm_wT = din("gm_wT", [128, 16, 128])
    gm_bT = din("gm_bT", [128, 16])
    posT = {"k": din("cmp_posT_k", [128, 32]), "v": din("cmp_posT_v", [128, 32])}
    cw1 = {"k": din("cmp_w1_k", [4096, 256]), "v": din("cmp_w1_v", [4096, 256])}
    cw2 = {"k": din("cmp_w2_k", [256, 128]), "v": din("cmp_w2_v", [256, 128])}
    norm_ffn_g = din("norm_ffn_g", [1, D])
    peer_w_q = din("peer_w_q", [D, D])
    keysT = [din("keys1T", [128, 128]), din("keys2T", [128, 128])]
    NEXP = 16384 if stop_after >= 5 else 128
    peer_u = din("peer_u", [NEXP, D])
    peer_v = din("peer_v", [NEXP, D])
    norm_final_g = din("norm_final_g", [1, D])
    cin = {n: din(n, s) for n, s in CONST_SHAPES.items()}
    y = nc.dram_tensor("y", [S, D], F32, kind="ExternalOutput").ap()

    SU = dscr("s_u", [S, D]); SV = dscr("s_v", [S, D]); SVS = dscr("s_vs", [S, 512]); SVW = dscr("s_vw", [S, 512])
    SNG = dscr("s_ng", [S, 48]); SMG = dscr("s_mg", [S, 4096])
    SQ = dscr("s_q", [16, 128, S]); SKC = dscr("s_kc", [4, 128, S]); SVC = dscr("s_vc", [4, 128, S])
    SKS = dscr("s_ks", [4, 128, S]); SKW = dscr("s_kw", [4, 128, S])
    SMA = dscr("s_ma", [S, D]); SOB = dscr("s_ob", [S, D]); SX1 = dscr("s_x1", [S, D])
    SH2 = dscr("s_h2", [S, D], BF16); SS = dscr("s_ss", [S, 2048])

    PSALL = nc.alloc_psum_tensor("psall", [128, 4096], F32)

    def PS(b, n=1):
        return PSALL[:, b * 512:(b + n) * 512]

    def PSB(b):
        return PSALL[:, b * 512:(b + 1) * 512].bitcast(BF16)

    def psk(b, n=1):
        return ["ps%d" % i for i in range(b, b + n)]

    ident_f = k.sb([128, 128], F32, "identf")
    ident_b = k.sb([128, 128], BF16, "identb")
    ones_b = k.sb([128, 128], BF16, "onesb")
    nh = k.sb([128, 1], F32, "nh")
    k.dma(ident_f[:], cin["c_ident"], [], ["identf"])
    k.cp("dve", ident_b[:], ident_f[:], ["identf"], ["identb"])
    k.memset("pool", ones_b[:], 1.0, ["onesb"])
    k.memset("pool", nh[:], -0.5, ["nh"])

    def row(t):
        return slice(t * 128, (t + 1) * 128)

    k.mark()
    hT = k.sb([128, 16, S], BF16, "hT")
    k.mark()
    gb = k.sb([128, D], F32, "gb")
    xt = [k.sb([128, D], F32, "xt") for _ in range(2)]
    junk = k.sb([128, D], BF16, "junk")
    hb = [k.sb([128, D], BF16, "hb") for _ in range(2)]
    sm = [k.sb([128, 4], F32, "sm") for _ in range(2)]
    k.dma(gb[:], norm_mix_g.partition_broadcast(128), [], ["gb"])
    for t in range(NT):
        p = t % 2
        kp = "a%d" % p
        k.dma(xt[p][:], x[row(t), :], [], [kp + "xt"])
        k.act(junk[:], xt[p][:], AF.Square, [kp + "xt"], ["junk", kp + "ss"], accum=sm[p][:, 0:1])
        k.rstd_from_ss(sm[p][:, 0:1], sm[p][:, 2:3], sm[p][:, 1:2], nh[:], D, kp)
        k.stt(hb[p][:], xt[p][:], sm[p][:, 2:3], gb[:], ALU.mult, ALU.mult, [kp + "xt", kp + "rstd", "gb"], [kp + "hb"])
        for kc in range(16):
            b = 2 * p + kc // 8
            k.tr(PSB(b)[:, (kc % 8) * 128:(kc % 8 + 1) * 128], hb[p][:, kc * 128:(kc + 1) * 128], ident_b[:],
                 [kp + "hb", "identb"], psk(b))
        for hh in range(2):
            b = 2 * p + hh
            k.cp("dve" if hh == 0 else "act", hT[:, hh * 8:(hh + 1) * 8, row(t)],
                 PSB(b).rearrange("p (a b) -> p a b", b=128), psk(b), [("hT", t)])
    k.release()

    if stop_after >= 1:
        wf = [k.sb([128, 16, 512], F32, "wf") for _ in range(2)]
        wb = [k.sb([128, 16, 512], BF16, "wb") for _ in range(2)]
        ob = [k.sb([128, 512], F32, "ob") for _ in range(4)]
        chunks = []
        for c in range(4):
            chunks.append((C_U + c * 512, 512, "tm", (SU, c * 512)))
        for c in range(4):
            chunks.append((C_V + c * 512, 512, "tm", (SV, c * 512)))
        for c in range(4):
            chunks.append((C_Q + c * 512, 512, "fm", [SQ[4 * c + i] for i in range(4)]))
        chunks.append((C_KV + 0 * 512, 512, "fm", [SKC[i] for i in range(4)]))
        chunks.append((C_KV + 1 * 512, 512, "fm", [SVC[i] for i in range(4)]))
        chunks.append((C_KV + 2 * 512, 512, "fm", [SKS[i] for i in range(4)]))
        chunks.append((C_KV + 3 * 512, 512, "tm", (SVS, 0)))
        chunks.append((C_KV + 4 * 512, 512, "fm", [SKW[i] for i in range(4)]))
        chunks.append((C_KV + 5 * 512, 512, "tm", (SVW, 0)))
        chunks.append((C_NG, 48, "tm", (SNG, 0)))
        for c in range(8):
            chunks.append((C_MG + c * 512, 512, "tm", (SMG, c * 512)))

        def load_chunk(ci):
            c0, n, _, _ = chunks[ci]
            p = ci % 2
            k.dma(wf[p][:, :, 0:n], w_in[:, c0:c0 + n].rearrange("(kc p) n -> p kc n", p=128), [], [("wf", p)])

        cnt = 0
        load_chunk(0)
        for ci, (c0, n, kind, dest) in enumerate(chunks):
            p = ci % 2
            if ci + 1 < len(chunks):
                load_chunk(ci + 1)
            k.cp("dve", wb[p][:, 0:8, 0:n], wf[p][:, 0:8, 0:n], [("wf", p)], [("wb", p)])
            k.cp("act", wb[p][:, 8:16, 0:n], wf[p][:, 8:16, 0:n], [("wf", p)], [("wb", p)])
            for u in range(16):
                b = 2 + cnt % 6
                o = ob[cnt % 4]
                okey = ("ob", cnt % 4)
                if kind == "tm":
                    t = u
                    for kc in range(16):
                        k.mm(PS(b)[:, 0:n], hT[:, kc, row(t)], wb[p][:, kc, 0:n], kc == 0, kc == 15,
                             [("hT", t), ("wb", p)], psk(b))
                    dst = dest[0][row(t), dest[1]:dest[1] + n]
                else:
                    blk, tc = u // 4, u % 4
                    for kc in range(16):
                        k.mm(PS(b)[:, 0:512], wb[p][:, kc, blk * 128:(blk + 1) * 128], hT[:, kc, tc * 512:(tc + 1) * 512],
                             kc == 0, kc == 15, [("hT", 4 * tc + i) for i in range(4)] + [("wb", p)], psk(b))
                    dst = dest[blk][:, tc * 512:(tc + 1) * 512]
                k.cp("act" if cnt % 2 == 0 else "dve", o[:, 0:n], PS(b)[:, 0:n], psk(b), [okey])
                k.dma(dst, o[:, 0:n], [okey], [("scr", ci, u)])
                cnt += 1
    k.release()
    k.barrier()

    if stop_after >= 2:
        k.mark()
        gam = k.sb([128, D], F32, "gam")
        bet = k.sb([128, D], F32, "bet")
        BS = k.sb([128, 16, 128], F32, "BS")
        WTm = k.sb([128, 16, 128], BF16, "WTm")
        bsT = k.sb([128, 16], F32, "bsT")
        k.mark()
        st1 = k.sb([128, 16, 128], F32, "st1")
        st2 = k.sb([128, 16, 128], F32, "st2")
        k.dma(gam[:], gm_ln_g.partition_broadcast(128), [], ["gam"])
        k.dma(bet[:], gm_ln_b.partition_broadcast(128), [], ["bet"])
        k.dma(bsT[:], gm_bT, [], ["bsT"])
        k.dma(st1[:], gm_wT, [], ["st1"])
        k.dma(st2[:], cin["c_tril"], [], ["st2"])
        k.tt("dve", WTm[:], st1[:], st2[:], ALU.mult, ["st1", "st2"], ["WTm"])
        k.cp("dve", BS[:], bsT[:].unsqueeze(2).to_broadcast([128, 16, 128]), ["bsT"], ["BS"])
        k.release()
        ut = [k.sb([128, D], F32, "ut") for _ in range(2)]
        vt = [k.sb([128, D], F32, "vt") for _ in range(2)]
        gt = [k.sb([128, D], F32, "gt") for _ in range(2)]
        tA = k.sb([128, D], F32, "tA")
        tB = k.sb([128, D], F32, "tB")
        Uh = k.sb([128, D], F32, "Uh")
        G2 = k.sb([128, D], F32, "G2")
        vln = k.sb([128, D], BF16, "vln")
        mo = [k.sb([128, D], F32, "mo") for _ in range(2)]
        sm2 = [k.sb([128, 8], F32, "sm2") for _ in range(2)]
        BSf = BS[:].rearrange("p a b -> p (a b)")

        def load2(t):
            p = t % 2
            k.dma(ut[p][:], SU[row(t), :], [], [("ut", p)])
            k.dma(vt[p][:], SV[row(t), :], [], [("vt", p)])
            k.dma(gt[p][:], SMG[row(t), 0:D], [], [("gt", p)])

        def tanh_inner(src, skey):
            k.act(tA[:], src, AF.Square, [skey], ["tA"])
            k.ts("dve", tA[:], tA[:], 0.044715, 1.0, ALU.mult, ALU.add, ["tA"], ["tA"])
            k.tt("dve", tA[:], tA[:], src, ALU.mult, ["tA", skey], ["tA"])
            k.act(tB[:], tA[:], AF.Tanh, ["tA"], ["tB"], scale=GC)

        load2(0)
        for t in range(NT):
            p = t % 2
            if t + 1 < NT:
                load2(t + 1)
            s = sm2[p]
            tanh_inner(ut[p][:], ("ut", p))
            k.ts("dve", tB[:], tB[:], 0.25, 0.25, ALU.mult, ALU.add, ["tB"], ["tB"])
            k.tt("dve", Uh[:], tB[:], ut[p][:], ALU.mult, ["tB", ("ut", p)], ["Uh"])
            tanh_inner(vt[p][:], ("vt", p))
            k.stt(G2[:], tB[:], 1.0, vt[p][:], ALU.add, ALU.mult, ["tB", ("vt", p)], ["G2", ("s1", p)], accum=s[:, 0:1])
            k.act(tA[:], G2[:], AF.Square, ["G2"], ["tA", ("s2", p)], accum=s[:, 1:2])
            k.ts("dve", s[:, 2:3], s[:, 0:1], 1.0 / D, None, ALU.mult, None, [("s1", p)], [("mean2", p)])
            k.tt("dve", s[:, 3:4], s[:, 2:3], s[:, 2:3], ALU.mult, [("mean2", p)], [("msq", p)])
            k.stt(s[:, 4:5], s[:, 1:2], 1.0 / D, s[:, 3:4], ALU.mult, ALU.subtract, [("s2", p), ("msq", p)], [("var4", p)])
            k.ts("dve", s[:, 5:6], s[:, 4:5], 0.25, 1e-6, ALU.mult, ALU.add, [("var4", p)], [("v4", p)])
            k.tt("pool", s[:, 6:7], s[:, 5:6], nh[:], ALU.pow, [("v4", p), "nh"], [("rs", p)])
            k.ts("dve", s[:, 7:8], s[:, 6:7], 0.5, None, ALU.mult, None, [("rs", p)], [("rsh", p)])
            k.ts("dve", tA[:], G2[:], s[:, 2:3], s[:, 7:8], ALU.subtract, ALU.mult, ["G2", ("mean2", p), ("rsh", p)], ["tA"])
            k.tt("dve", tA[:], tA[:], gam[:], ALU.mult, ["tA", "gam"], ["tA"])
            k.tt("dve", vln[:], tA[:], bet[:], ALU.add, ["tA", "bet"], ["vln"])
            b0 = 4 * p
            for g in range(16):
                b = b0 + g // 4
                k.mm(PS(b)[:, (g % 4) * 128:(g % 4 + 1) * 128], WTm[:, g, :], vln[:, g * 128:(g + 1) * 128], True, True,
                     ["WTm", "vln"], psk(b))
            k.tt("dve", tA[:], PS(b0, 4), BSf, ALU.add, psk(b0, 4) + ["BS"], ["tA"])
            k.tt("dve", tA[:], tA[:], Uh[:], ALU.mult, ["tA", "Uh"], ["tA"])
            k.act(tB[:], gt[p][:], AF.Tanh, [("gt", p)], ["tB"], scale=0.5)
            k.stt(mo[p][:], tB[:], 1.0, tA[:], ALU.add, ALU.mult, ["tB", "tA"], [("mo", p)])
            k.dma(SMA[row(t), :], mo[p][:], [("mo", p)], [("sma", t)])
        k.release()
        k.barrier()

    if stop_after >= 3:
        k.mark()
        cosT = k.sb([128, S], F32, "cosT"); sinT = k.sb([128, S], F32, "sinT")
        RT = k.sb([128, 128], F32, "RT")
        CAUS = k.sb([128, 128], BF16, "CAUS"); WINB = k.sb([128, 128], BF16, "WINB")
        CMPB = k.sb([128, S], BF16, "CMPB")
        Aagg = k.sb([128, 32], F32, "Aagg")
        AM = k.sb([128, 16, 32], F32, "AM"); ADDM = k.sb([128, 16, 32], F32, "ADDM")
        ESEL = k.sb([32, 16, 128], BF16, "ESEL")
        w1b = {m: k.sb([128, 32, 256], BF16, "w1b") for m in "kv"}
        w2b = {m: k.sb([128, 2, 128], BF16, "w2b") for m in "kv"}
        posb = {m: k.sb([128, 32], BF16, "posb") for m in "kv"}
        cvec = {m: k.sb([128, 2], F32, "cvec") for m in "kv"}
        stg = k.sb([128, 2 * S], F32, "stg")
        kcb = k.sb([128, S], BF16, "kcb"); vcb = k.sb([128, S], BF16, "vcb")
        ksr = k.sb([128, S], BF16, "ksr"); kwr = k.sb([128, S], BF16, "kwr")
        vsa = k.sb([128, 16, 129], BF16, "vsa"); vwa = k.sb([128, 16, 129], BF16, "vwa")
        qb = [k.sb([128, S], BF16, "qb") for _ in range(4)]
        qr = [k.sb([128, S], BF16, "qr") for _ in range(4)]
        sgr = k.sb([128, 16, 12], F32, "sgr"); sg = k.sb([128, 16, 12], F32, "sg")
        kcT = k.sb([128, 128], BF16, "kcT"); vca = k.sb([128, 130], BF16, "vca")
        hid = k.sb([128, 2, 128], BF16, "hid")
        hx = k.sb([128, 128], F32, "hx"); hy = k.sb([128, 128], F32, "hy"); hz = k.sb([128, 128], F32, "hz")
        biasT = k.sb([32, S], BF16, "biasT")
        PcT = [k.sb([128, 512], BF16, "PcT") for _ in range(2)]
        pn = k.sb([128, 512], F32, "pn"); rZ = [k.sb([128, 512], F32, "rZ") for _ in range(2)]; impS = k.sb([32, 512], F32, "impS")
        sc = k.sb([128, 128], F32, "sc"); selb = k.sb([128, 128], F32, "selb"); wk32 = k.sb([128, 32], F32, "wk32")
        m8 = k.sb([128, 16], F32, "m8")
        accp = [k.sb([128, 4, 4, 128], F32, "acc") for _ in range(2)]
        PT = [k.sb([128, 512], BF16, "PT") for _ in range(3)]
        t1 = [k.sb([128, 512], F32, "t1") for _ in range(2)]
        t2 = [k.sb([128, 512], F32, "t2") for _ in range(2)]
        czs = [k.sb([128, 2], F32, "cz") for _ in range(4)]

        def stgv(b):
            return stg[:, b * S:(b + 1) * S]

        k.dma(cosT[:], cin["c_cos"], [], ["cosT"]); k.dma(sinT[:], cin["c_sin"], [], ["sinT"])
        k.dma(RT[:], cin["c_rt"], [], ["RT"]); k.dma(Aagg[:], cin["c_aagg"], [], ["Aagg"])
        k.dma(AM[:], cin["c_am"], [], ["AM"]); k.dma(ADDM[:], cin["c_add"], [], ["ADDM"])
        k.dma(stgv(0), cin["c_cmpb"], [], [("stg", 0)])
        k.cp("dve", CMPB[:], stgv(0), [("stg", 0)], ["CMPB"])
        k.dma(stg[:, S:S + 128], cin["c_caus"], [], [("stg", 1)])
        k.dma(stg[:, S + 128:S + 256], cin["c_winb"], [], [("stg", 1)])
        k.cp("dve", CAUS[:], stg[:, S:S + 128], [("stg", 1)], ["CAUS"])
        k.cp("dve", WINB[:], stg[:, S + 128:S + 256], [("stg", 1)], ["WINB"])
        k.dma(stg[0:32, 0:S].rearrange("p (a b) -> p a b", b=128), cin["c_esel"], [("stg", 0)], [("stg", 0)])
        k.cp("dve", ESEL[:], stg[0:32, 0:S].rearrange("p (a b) -> p a b", b=128), [("stg", 0)], ["ESEL"])
        for m in "kv":
            for hf in range(2):
                k.dma(stg[:].rearrange("p (l n) -> p l n", n=256),
                      cw1[m][hf * 2048:(hf + 1) * 2048, :].rearrange("(l d) n -> d l n", d=128), [], [("stg", 0), ("stg", 1)])
                k.cp("dve" if hf == 0 else "act", w1b[m][:, hf * 16:(hf + 1) * 16, :], stg[:].rearrange("p (l n) -> p l n", n=256),
                     [("stg", 0), ("stg", 1)], [("w1b", m)])
            k.dma(stg[:, 0:256].rearrange("p (h d) -> p h d", d=128), cw2[m].rearrange("(h p) d -> p h d", p=128), [], [("stg", 0)])
            k.cp("dve", w2b[m][:], stg[:, 0:256].rearrange("p (h d) -> p h d", d=128), [("stg", 0)], [("w2b", m)])
            k.dma(stg[:, S:S + 32], posT[m], [], [("stg", 1)])
            k.cp("dve", posb[m][:], stg[:, S:S + 32], [("stg", 1)], [("posb", m)])
            for hf in range(2):
                for l in range(32):
                    k.mm(PS(7)[:, hf:hf + 1], w1b[m][:, l, hf * 128:(hf + 1) * 128], posb[m][:, l:l + 1], l == 0, l == 31,
                         [("w1b", m), ("posb", m)], psk(7))
            k.cp("dve", cvec[m][:], PS(7)[:, 0:2], psk(7), [("cvec", m)])
        k.memset("pool", vsa[:, :, 128:129], 1.0, ["vsa"])
        k.memset("pool", vwa[:, :, 128:129], 1.0, ["vwa"])
        k.memset("pool", vca[:, 128:129], 1.0, ["vca"])

        rot_i = [0]

        def rotary(src, skey, dst, dkey):
            for tc in range(4):
                i = rot_i[0] % 2
                rot_i[0] += 1
                sl = slice(tc * 512, (tc + 1) * 512)
                b = 4 + i
                k.mm(PS(b), RT[:], src[:, sl], True, True, [skey, "RT"], psk(b))
                k.tt("dve", t1[i][:], PS(b), sinT[:, sl], ALU.mult, psk(b) + ["sinT"], [("t1", i)])
                k.tt("pool", t2[i][:], src[:, sl], cosT[:, sl], ALU.mult, [skey, "cosT"], [("t2", i)])
                k.tt("dve", dst[:, sl], t1[i][:], t2[i][:], ALU.add, [("t1", i), ("t2", i)], [dkey])

        def compress(m, src, skey):
            for hf in range(2):
                b = 4 + hf
                for l in range(32):
                    k.mm(PS(b)[:, 0:127], w1b[m][:, l, hf * 128:(hf + 1) * 128], src[:, l:l + 16 * 126 + 1:16], l == 0, l == 31,
                         [("w1b", m), skey], psk(b))
                k.ts("dve", hx[:, 0:127], PS(b)[:, 0:127], cvec[m][:, hf:hf + 1], None, ALU.add, None, psk(b) + [("cvec", m)], ["hx"])
                k.act(hy[:, 0:127], hx[:, 0:127], AF.Square, ["hx"], ["hy"])
                k.ts("dve", hy[:, 0:127], hy[:, 0:127], 0.044715, 1.0, ALU.mult, ALU.add, ["hy"], ["hy"])
                k.tt("dve", hy[:, 0:127], hy[:, 0:127], hx[:, 0:127], ALU.mult, ["hy", "hx"], ["hy"])
                k.act(hz[:, 0:127], hy[:, 0:127], AF.Tanh, ["hy"], ["hz"], scale=GC)
                k.ts("dve", hz[:, 0:127], hz[:, 0:127], 0.5, 0.5, ALU.mult, ALU.add, ["hz"], ["hz"])
                k.tt("dve", hid[:, hf, 0:127], hz[:, 0:127], hx[:, 0:127], ALU.mult, ["hz", "hx"], ["hid"])
            if m == "k":
                for hf in range(2):
                    k.mm(PS(6)[:, 0:127], w2b[m][:, hf, :], hid[:, hf, 0:127], hf == 0, hf == 1, [("w2b", m), "hid"], psk(6))
                k.cp("act", kcT[:, 0:127], PS(6)[:, 0:127], psk(6), ["kcT"])
            else:
                for hf in range(2):
                    k.mm(PS(6)[0:127, 0:128], hid[:, hf, 0:127], w2b[m][:, hf, :], hf == 0, hf == 1, [("w2b", m), "hid"], psk(6))
                k.cp("act", vca[0:127, 0:128], PS(6)[0:127, 0:128], psk(6), ["vca"])

        sb_i = [0]
        ob_i = [0]
        pt_i = [0]
        cz_i = [0]
        OBANKS = [2, 3, 7]

        def attn(kT, kkey, va, vkey, qrt, qkey, qt, ktl, use_sel):
            obk = OBANKS[ob_i[0] % 3]
            ob_i[0] += 1
            nk = len(ktl)
            done = 0
            for g0 in range(0, nk, 4):
                grp = ktl[g0:g0 + 4]
                sbk = sb_i[0] % 2
                sb_i[0] += 1
                for j, (kt, eb) in enumerate(grp):
                    out = PS(sbk)[:, j * 128:(j + 1) * 128]
                    nmm = 1 + (1 if use_sel else 0) + (1 if eb else 0)
                    k.mm(out, kT[:, kt * 128:(kt + 1) * 128], qrt[:, row(qt)], True, nmm == 1, [kkey, qkey], psk(sbk))
                    i = 1
                    if use_sel:
                        i += 1
                        k.mm(out, ESEL[0:32, kt, :], biasT[0:32, row(qt)], False, i == nmm, ["ESEL", "biasT"], psk(sbk))
                    if eb:
                        k.mm(out, ident_b[:], (CAUS if eb == "caus" else WINB)[:], False, True, ["identb", "CAUS", "WINB"], psk(sbk))
                n = len(grp) * 128
                pi = pt_i[0] % 3
                pt_i[0] += 1
                k.act(PT[pi][:, 0:n], PS(sbk)[:, 0:n], AF.Exp, psk(sbk), [("PT", pi)], scale=SCALE)
                for j, (kt, eb) in enumerate(grp):
                    k.mm(PS(obk)[:, 0:129], PT[pi][:, j * 128:(j + 1) * 128], va[:, kt, :], done == 0, done == nk - 1,
                         [("PT", pi), vkey], psk(obk))
                    done += 1
            return obk

        def combine(obk, qt, qtl, g, br, ap, akey, first, guard):
            cz = czs[cz_i[0] % 4]
            ck = ("cz", cz_i[0] % 4)
            cz_i[0] += 1
            if guard:
                k.ts("dve", cz[:, 0:1], PS(obk)[:, 128:129], 1e-30, None, ALU.max, None, psk(obk), [ck])
                k.recip(cz[:, 0:1], cz[:, 0:1], [ck], [ck])
            else:
                k.recip(cz[:, 0:1], PS(obk)[:, 128:129], psk(obk), [ck])
            gate = sg[:, qt, g * 3 + br:g * 3 + br + 1]
            if first:
                k.ts("dve", ap[:, qtl, g, :], PS(obk)[:, 0:128], cz[:, 0:1], gate, ALU.mult, ALU.mult, psk(obk) + [ck, "sg"], [akey])
            else:
                k.tt("dve", cz[:, 1:2], cz[:, 0:1], gate, ALU.mult, [ck, "sg"], [ck])
                k.stt(ap[:, qtl, g, :], PS(obk)[:, 0:128], cz[:, 1:2], ap[:, qtl, g, :], ALU.mult, ALU.add, psk(obk) + [ck, akey], [akey])

        for hk in range(4 if stop_after >= 3 else 0):
            si = [0]

            def nxt():
                b = si[0] % 2
                si[0] += 1
                return b
            for nm, src, dst in (("kcb", SKC[hk], kcb), ("vcb", SVC[hk], vcb)):
                b = nxt()
                k.dma(stgv(b), src, [], [("stg", b)])
                k.cp("act", dst[:], stgv(b), [("stg", b)], [nm])
            for nm, src, dst in (("ksr", SKS[hk], ksr), ("kwr", SKW[hk], kwr)):
                b = nxt()
                k.dma(stgv(b), src, [], [("stg", b)])
                rotary(stgv(b), ("stg", b), dst, nm)
            for g in range(4):
                b = nxt()
                k.dma(stgv(b), SQ[4 * hk + g], [], [("stg", b)])
                k.cp("act", qb[g][:], stgv(b), [("stg", b)], [("qb", g)])
                rotary(stgv(b), ("stg", b), qr[g], ("qr", g))
            for nm, src, dst in (("vsa", SVS, vsa), ("vwa", SVW, vwa)):
                b = nxt()
                k.dma(stgv(b).rearrange("p (t d) -> p t d", d=128),
                      src[:, hk * 128:(hk + 1) * 128].rearrange("(t p) d -> p t d", p=128), [], [("stg", b)])
                k.cp("dve", dst[:, :, 0:128], stgv(b).rearrange("p (t d) -> p t d", d=128), [("stg", b)], [nm])
            k.dma(sgr[:], SNG[:, hk * 12:(hk + 1) * 12].rearrange("(t p) c -> p t c", p=128), [], ["sgr"])
            k.act(sg[:], sgr[:], AF.Tanh, ["sgr"], ["sg"], scale=0.5)
            k.ts("dve", sg[:], sg[:], 0.5, 0.5, ALU.mult, ALU.add, ["sg"], ["sg"])
            compress("k", kcb, "kcb")
            compress("v", vcb, "vcb")

            for qc in range(4):
                qsl = slice(qc * 512, (qc + 1) * 512)
                par = qc % 2
                ap = accp[par]
                def cmp_a(g):
                    pc = PcT[g % 2]
                    pk = ("pc", g % 2)
                    k.mm(PS(5)[0:127, :], kcT[:, 0:127], qb[g][:, qsl], True, False, ["kcT", ("qb", g)], psk(5))
                    k.mm(PS(5)[0:127, :], ident_b[0:127, 0:127], CMPB[0:127, qsl], False, True, ["identb", "CMPB"], psk(5))
                    k.act(pc[0:127, :], PS(5)[0:127, :], AF.Exp, psk(5), [pk], scale=SCALE)
                    k.mm(PS(5), ones_b[0:127, :], pc[0:127, :], True, True, ["onesb", pk], psk(5))
                    k.ts("dve", rZ[g % 2][:], PS(5), 1e-30, None, ALU.max, None, psk(5), [("rZ", g % 2)])

                def cmp_b(g):
                    pc = PcT[g % 2]
                    pk = ("pc", g % 2)
                    rz = rZ[g % 2]
                    rk = ("rZ", g % 2)
                    k.recip(rz[:], rz[:], [rk], [rk])
                    k.tt("dve", pn[0:127, :], pc[0:127, :], rz[0:127, :], ALU.mult, [pk, rk], ["pn"])
                    k.mm(PS(6)[0:32, :], Aagg[0:127, :], pn[0:127, :], g == 0, g == 3, ["Aagg", "pn"], psk(6))
                    for qtl in range(4):
                        qt = qc * 4 + qtl
                        obk = OBANKS[ob_i[0] % 3]
                        ob_i[0] += 1
                        k.mm(PS(obk)[:, 0:129], pc[0:127, qtl * 128:(qtl + 1) * 128], vca[0:127, 0:129], True, True, [pk, "vca"], psk(obk))
                        combine(obk, qt, qtl, g, 0, ap, ("acc", par, qtl), True, True)

                cmp_a(0)
                for g in range(4):
                    if g + 1 < 4:
                        cmp_a(g + 1)
                    cmp_b(g)
                k.cp("act", impS[0:32, :], PS(6)[0:32, :], psk(6), ["impS"])
                for qtl in range(4):
                    k.tr(PS(5)[:, qtl * 32:(qtl + 1) * 32], impS[0:32, qtl * 128:(qtl + 1) * 128], ident_f[0:32, 0:32],
                         ["impS", "identf"], psk(5))
                sc3 = sc[:].rearrange("p (a b) -> p a b", b=32)
                k.tt("dve", sc3, PS(5)[:, 0:128].rearrange("p (a b) -> p a b", b=32), AM[:, qc * 4:(qc + 1) * 4, :], ALU.mult,
                     psk(5) + ["AM"], ["sc"])
                k.tt("dve", sc3, sc3, ADDM[:, qc * 4:(qc + 1) * 4, :], ALU.add, ["sc", "ADDM"], ["sc"])
                for qtl in range(4):
                    ssl = sc[:, qtl * 32:(qtl + 1) * 32]
                    k.max8(m8[:, 0:8], ssl, ["sc"], ["m8"])
                    k.matchrep(wk32[:], m8[:, 0:8], ssl, -3e38, ["sc", "m8"], ["wk32"])
                    k.max8(m8[:, 8:16], wk32[:], ["wk32"], ["m8"])
                    k.ts("dve", selb[:, qtl * 32:(qtl + 1) * 32], ssl, m8[:, 15:16], -NEG, ALU.is_ge, ALU.mult, ["sc", "m8"], ["selb"])
                k.ts("dve", selb[:], selb[:], NEG, None, ALU.add, None, ["selb"], ["selb"])
                for qtl in range(4):
                    k.tr(PS(6)[0:32, qtl * 128:(qtl + 1) * 128], selb[:, qtl * 32:(qtl + 1) * 32], ident_f[:], ["selb", "identf"], psk(6))
                k.cp("act", biasT[0:32, qsl], PS(6)[0:32, :], psk(6), ["biasT"])
                branches = []
                for qtl in range(4):
                    qt = qc * 4 + qtl
                    for g in range(4):
                        ktl = [(kt, "caus" if kt == qt else None) for kt in range(qt + 1)]
                        branches.append(dict(kT=ksr, kkey="ksr", va=vsa, vkey="vsa", g=g, qt=qt, qtl=qtl, ktl=ktl, sel=True, br=1, last=False))
                        ktl = [(kt, "caus" if kt == qt else ("winb" if kt == qt - 4 else None)) for kt in range(max(0, qt - 4), qt + 1)]
                        branches.append(dict(kT=kwr, kkey="kwr", va=vwa, vkey="vwa", g=g, qt=qt, qtl=qtl, ktl=ktl, sel=False, br=2, last=(g == 3)))
                groups = []
                for bi, B in enumerate(branches):
                    nk = len(B["ktl"])
                    for g0 in range(0, nk, 4):
                        groups.append(dict(b=bi, kts=B["ktl"][g0:g0 + 4], first=(g0 == 0), lastg=(g0 + 4 >= nk), base=g0))

                def emit_scores(G_):
                    B = branches[G_["b"]]
                    sbk = (0, 1, 4)[sb_i[0] % 3]
                    sb_i[0] += 1
                    G_["sbk"] = sbk
                    qt, g = B["qt"], B["g"]
                    for j, (kt, eb) in enumerate(G_["kts"]):
                        out = PS(sbk)[:, j * 128:(j + 1) * 128]
                        nmm = 1 + (1 if B["sel"] else 0) + (1 if eb else 0)
                        k.mm(out, B["kT"][:, kt * 128:(kt + 1) * 128], qr[g][:, row(qt)], True, nmm == 1, [B["kkey"], ("qr", g)], psk(sbk))
                        i = 1
                        if B["sel"]:
                            i += 1
                            k.mm(out, ESEL[0:32, kt, :], biasT[0:32, row(qt)], False, i == nmm, ["ESEL", "biasT"], psk(sbk))
                        if eb:
                            k.mm(out, ident_b[:], (CAUS if eb == "caus" else WINB)[:], False, True, ["identb", "CAUS", "WINB"], psk(sbk))

                def emit_exp_pv(G_):
                    B = branches[G_["b"]]
                    sbk = G_["sbk"]
                    if G_["first"]:
                        B["obk"] = OBANKS[ob_i[0] % 3]
                        ob_i[0] += 1
                    obk = B["obk"]
                    n = len(G_["kts"]) * 128
                    pi = pt_i[0] % 3
                    pt_i[0] += 1
                    k.act(PT[pi][:, 0:n], PS(sbk)[:, 0:n], AF.Exp, psk(sbk), [("PT", pi)], scale=SCALE)
                    nk = len(B["ktl"])
                    for j, (kt, eb) in enumerate(G_["kts"]):
                        idx = G_["base"] + j
                        k.mm(PS(obk)[:, 0:129], PT[pi][:, j * 128:(j + 1) * 128], B["va"][:, kt, :], idx == 0, idx == nk - 1,
                             [("PT", pi), B["vkey"]], psk(obk))
                    if G_["lastg"]:
                        akey = ("acc", par, B["qtl"])
                        combine(obk, B["qt"], B["qtl"], B["g"], B["br"], ap, akey, False, False)
                        if B["last"]:
                            k.dma(SOB[row(B["qt"]), hk * 512:(hk + 1) * 512], ap[:, B["qtl"]].rearrange("p g d -> p (g d)"), [akey],
                                  [("sob", hk, B["qt"])])

                emit_scores(groups[0])
                if len(groups) > 1:
                    emit_scores(groups[1])
                for gi_ in range(len(groups)):
                    if gi_ + 2 < len(groups):
                        emit_scores(groups[gi_ + 2])
                    emit_exp_pv(groups[gi_])
        k.release()
        k.barrier()

    if stop_after >= 4:
        k.mark()
        wo = k.sb([128, 16, D], BF16, "wo")
        k.mark()
        stg4 = k.sb([128, 16, 512], F32, "stg4")
        for c in range(4):
            k.dma(stg4[:], w_out[:, c * 512:(c + 1) * 512].rearrange("(kc p) n -> p kc n", p=128), [], ["stg4"])
            k.cp("dve", wo[:, 0:8, c * 512:(c + 1) * 512], stg4[:, 0:8, :], ["stg4"], ["wo"])
            k.cp("act", wo[:, 8:16, c * 512:(c + 1) * 512], stg4[:, 8:16, :], ["stg4"], ["wo"])
        k.release()
        obt = [k.sb([128, D], F32, "obt") for _ in range(2)]
        mat = [k.sb([128, D], F32, "mat") for _ in range(2)]
        gbt = [k.sb([128, D], F32, "gbt") for _ in range(2)]
        xtt = [k.sb([128, D], F32, "xtt") for _ in range(2)]
        mb = k.sb([128, D], BF16, "mb")
        mT = k.sb([128, 16, 128], BF16, "mT")
        x1o = [k.sb([128, D], F32, "x1o") for _ in range(2)]

        def load4(t):
            p = t % 2
            k.dma(obt[p][:], SOB[row(t), :], [], [("obt", p)])
            k.dma(mat[p][:], SMA[row(t), :], [], [("mat", p)])
            k.dma(gbt[p][:], SMG[row(t), D:2 * D], [], [("gbt", p)])
            k.dma(xtt[p][:], x[row(t), :], [], [("xtt", p)])

        load4(0)
        for t in range(NT):
            p = t % 2
            if t + 1 < NT:
                load4(t + 1)
            k.act(gbt[p][:], gbt[p][:], AF.Tanh, [("gbt", p)], [("gbt", p)], scale=0.5)
            k.ts("dve", gbt[p][:], gbt[p][:], 0.5, 0.5, ALU.mult, ALU.add, [("gbt", p)], [("gbt", p)])
            k.tt("dve", obt[p][:], obt[p][:], gbt[p][:], ALU.mult, [("obt", p), ("gbt", p)], [("obt", p)])
            k.tt("dve", mb[:], obt[p][:], mat[p][:], ALU.add, [("obt", p), ("mat", p)], ["mb"])
            for kc in range(16):
                b = kc // 8
                k.tr(PSB(b)[:, (kc % 8) * 128:(kc % 8 + 1) * 128], mb[:, kc * 128:(kc + 1) * 128], ident_b[:], ["mb", "identb"], psk(b))
            for hh in range(2):
                k.cp("dve" if hh == 0 else "act", mT[:, hh * 8:(hh + 1) * 8, :], PSB(hh).rearrange("p (a b) -> p a b", b=128),
                     psk(hh), ["mT"])
            for c in range(4):
                b = 2 + c
                for kc in range(16):
                    k.mm(PS(b), mT[:, kc, :], wo[:, kc, c * 512:(c + 1) * 512], kc == 0, kc == 15, ["mT", "wo"], psk(b))
                k.tt("dve", x1o[p][:, c * 512:(c + 1) * 512], PS(b), xtt[p][:, c * 512:(c + 1) * 512], ALU.add,
                     psk(b) + [("xtt", p)], [("x1o", p)])
            k.dma(SX1[row(t), :], x1o[p][:], [("x1o", p)], [("sx1", t)])
        k.release()
        k.barrier()

    if stop_after >= 5:
        k.mark()
        eidT_all = k.sb([128, 16, 128], I32, "eidTall")
        gateT_all = k.sb([128, 16, 128], F32, "gateTall")
        iota16 = k.sb([128, 16], F32, "iota16")
        k.dma(iota16[:], cin["c_iota16"], [], ["iota16"])
        k.mark()
        wq = k.sb([128, 16, D], BF16, "wq")
        k.mark()
        stg5 = k.sb([128, 16, 512], F32, "stg5")
        for c in range(4):
            k.dma(stg5[:], peer_w_q[:, c * 512:(c + 1) * 512].rearrange("(kc p) n -> p kc n", p=128), [], ["stg5"])
            k.cp("dve", wq[:, 0:8, c * 512:(c + 1) * 512], stg5[:, 0:8, :], ["stg5"], ["wq"])
            k.cp("act", wq[:, 8:16, c * 512:(c + 1) * 512], stg5[:, 8:16, :], ["stg5"], ["wq"])
        k.release()
        kTf = [k.sb([128, 128], F32, "kTf") for _ in range(2)]
        g2b = k.sb([128, D], F32, "g2b")
        x1a = [k.sb([128, D], F32, "x1a") for _ in range(2)]
        h2a = [k.sb([128, D], BF16, "h2a") for _ in range(2)]
        jk = k.sb([128, D], BF16, "jk")
        h2T = k.sb([128, 16, 128], BF16, "h2T")
        qTs = k.sb([128, 16, 128], F32, "qTs")
        Sxa = [k.sb([128, 16, 128], F32, "Sxa") for _ in range(2)]
        sm5 = [k.sb([128, 4], F32, "sm5") for _ in range(2)]
        cand = k.sb([128, 8, 16, 16], F32, "cand"); eqt = k.sb([128, 8, 16, 16], F32, "eqt")
        V16 = k.sb([128, 16, 16], F32, "V16"); I16u = k.sb([128, 16, 16], U32, "I16u"); I16f = k.sb([128, 16, 16], F32, "I16f")
        wk = k.sb([128, 128], F32, "wk"); wk2 = k.sb([128, 256], F32, "wk2")
        T16 = k.sb([128, 8, 16], F32, "T16"); P16u = k.sb([128, 8, 16], U32, "P16u")
        A16u = k.sb([128, 8, 16], U32, "A16u"); B16u = k.sb([128, 8, 16], U32, "B16u")
        af = k.sb([128, 8, 16], F32, "af"); bf = k.sb([128, 8, 16], F32, "bf")
        ex = k.sb([128, 8, 16], F32, "ex"); gate = k.sb([128, 8, 16], F32, "gate")
        zs = k.sb([128, 8], F32, "zs")
        i1f = k.sb([128, 8, 16], F32, "i1f"); i2f = k.sb([128, 8, 16], F32, "i2f"); eid = k.sb([128, 8, 16], F32, "eid")
        io_b = iota16[:].unsqueeze(1).unsqueeze(1).to_broadcast([128, 8, 16, 16])
        V16v = V16[:].rearrange("p (h two) a -> p h two a", two=2)
        I16v = I16f[:].rearrange("p (h two) a -> p h two a", two=2)
        k.dma(kTf[0][:], keysT[0], [], ["kTf"]); k.dma(kTf[1][:], keysT[1], [], ["kTf"])
        k.dma(g2b[:], norm_ffn_g.partition_broadcast(128), [], ["g2b"])
        def front(t):
            p = t % 2
            kp = "n5%d" % p
            k.dma(x1a[p][:], SX1[row(t), :], [], [("x1a", p)])
            k.act(jk[:], x1a[p][:], AF.Square, [("x1a", p)], ["jk", kp + "ss"], accum=sm5[p][:, 0:1])
            k.rstd_from_ss(sm5[p][:, 0:1], sm5[p][:, 2:3], sm5[p][:, 1:2], nh[:], D, kp)
            k.stt(h2a[p][:], x1a[p][:], sm5[p][:, 2:3], g2b[:], ALU.mult, ALU.mult, [("x1a", p), kp + "rstd", "g2b"], [("h2a", p)])
            k.dma(SH2[row(t), :], h2a[p][:], [("h2a", p)], [("sh2", t)])
            for kc in range(16):
                b = kc // 8
                k.tr(PSB(b)[:, (kc % 8) * 128:(kc % 8 + 1) * 128], h2a[p][:, kc * 128:(kc + 1) * 128], ident_b[:], [("h2a", p), "identb"], psk(b))
            for hh in range(2):
                k.cp("dve" if hh == 0 else "act", h2T[:, hh * 8:(hh + 1) * 8, :], PSB(hh).rearrange("p (a b) -> p a b", b=128),
                     psk(hh), ["h2T"])
            for cb in range(16):
                b = 4 + cb // 4
                for kc in range(16):
                    k.mm(PS(b)[:, (cb % 4) * 128:(cb % 4 + 1) * 128], wq[:, kc, cb * 128:(cb + 1) * 128], h2T[:, kc, :], kc == 0, kc == 15,
                         ["wq", "h2T"], psk(b))
            k.cp("act", qTs[:].rearrange("p a b -> p (a b)"), PS(4, 4), psk(4, 4), ["qTs"])
            for cb in range(16):
                b = 4 + cb // 4
                k.mm(PS(b)[:, (cb % 4) * 128:(cb % 4 + 1) * 128], qTs[:, cb, :], kTf[cb % 2][:], True, True, ["qTs", "kTf"], psk(b))
            k.cp("act", Sxa[p][:].rearrange("p a b -> p (a b)"), PS(4, 4), psk(4, 4), [("Sxa", p)])

        def topk(t):
            eT, gT = eidT_all[:, t, :], gateT_all[:, t, :]
            ek, gk = ("eidT", t), ("gateT", t)
            Sx = Sxa[t % 2]
            sxk = ("Sxa", t % 2)
            for hb_ in range(16):
                k.max8(V16[:, hb_, 0:8], Sx[:, hb_, :], [sxk], ["V16"])
                k.maxidx(I16u[:, hb_, 0:8], V16[:, hb_, 0:8], Sx[:, hb_, :], [sxk, "V16"], ["I16u"])
                k.matchrep(wk[:], V16[:, hb_, 0:8], Sx[:, hb_, :], -3e38, [sxk, "V16"], ["wk"])
                k.max8(V16[:, hb_, 8:16], wk[:], ["wk"], ["V16"])
                k.maxidx(I16u[:, hb_, 8:16], V16[:, hb_, 8:16], wk[:], ["wk", "V16"], ["I16u"])
            k.cp("dve", I16f[:], I16u[:], ["I16u"], ["I16f"])
            k.tt("dve", cand[:], V16v[:, :, 0, :].unsqueeze(3).to_broadcast([128, 8, 16, 16]),
                 V16v[:, :, 1, :].unsqueeze(2).to_broadcast([128, 8, 16, 16]), ALU.add, ["V16"], ["cand"])
            for h in range(8):
                ch = cand[:, h].rearrange("p a b -> p (a b)")
                k.max8(T16[:, h, 0:8], ch, ["cand"], ["T16"])
                k.maxidx(P16u[:, h, 0:8], T16[:, h, 0:8], ch, ["cand", "T16"], ["P16u"])
                k.matchrep(wk2[:], T16[:, h, 0:8], ch, -3e38, ["cand", "T16"], ["wk2"])
                k.max8(T16[:, h, 8:16], wk2[:], ["wk2"], ["T16"])
                k.maxidx(P16u[:, h, 8:16], T16[:, h, 8:16], wk2[:], ["wk2", "T16"], ["P16u"])
            k.op("dve", lambda e: e.tensor_single_scalar(out=A16u[:], in_=P16u[:], scalar=4, op=ALU.logical_shift_right), ["P16u"], ["A16u"])
            k.op("dve", lambda e: e.tensor_single_scalar(out=B16u[:], in_=P16u[:], scalar=15, op=ALU.bitwise_and), ["P16u"], ["B16u"])
            k.cp("dve", af[:], A16u[:], ["A16u"], ["af"])
            k.cp("dve", bf[:], B16u[:], ["B16u"], ["bf"])
            for (pf, col, dst, dk) in ((af, 0, i1f, "i1f"), (bf, 1, i2f, "i2f")):
                k.tt("dve", eqt[:], io_b, pf[:].unsqueeze(3).to_broadcast([128, 8, 16, 16]), ALU.is_equal, ["iota16", "af", "bf"], ["eqt"])
                k.tt("dve", eqt[:], eqt[:], I16v[:, :, col, :].unsqueeze(2).to_broadcast([128, 8, 16, 16]), ALU.mult, ["eqt", "I16f"], ["eqt"])
                k.rsum(dst[:], eqt[:], ["eqt"], [dk])
            k.stt(eid[:], i1f[:], 128.0, i2f[:], ALU.mult, ALU.add, ["i1f", "i2f"], ["eid"])
            k.ts("dve", eid[:], eid[:], 0.0, 16383.0, ALU.max, ALU.min, ["eid"], ["eid"])
            k.tr(PS(2)[:, 0:128], eid[:].rearrange("p a b -> p (a b)"), ident_f[:], ["eid", "identf"], psk(2))
            k.cp("dve", eT, PS(2)[:, 0:128], psk(2), [ek])
            k.tt("dve", ex[:], T16[:], T16[:, :, 0:1].to_broadcast([128, 8, 16]), ALU.subtract, ["T16"], ["ex"])
            k.act(ex[:], ex[:], AF.Exp, ["ex"], ["ex"])
            k.rsum(zs[:], ex[:], ["ex"], ["zs"])
            k.recip(zs[:], zs[:], ["zs"], ["zs"])
            k.tt("dve", gate[:], ex[:], zs[:].unsqueeze(2).to_broadcast([128, 8, 16]), ALU.mult, ["ex", "zs"], ["gate"])
            k.tr(PS(2)[:, 128:256], gate[:].rearrange("p a b -> p (a b)"), ident_f[:], ["gate", "identf"], psk(2))
            k.cp("dve", gT, PS(2)[:, 128:256], psk(2), [gk])


        front(0)
        for t in range(NT):
            if t + 1 < NT:
                front(t + 1)
            topk(t)
        k.release()
        k.barrier()

    if stop_after >= 5:
        k.mark()
        gfb = k.sb([128, D], F32, "gfb")
        SELR = k.sb([128, 128, 128], BF16, "SELR")
        IDm = k.sb([128, 128, 128], BF16, "IDm")
        WSEL = k.sb([128, 128, 128], BF16, "WSEL")
        Ubuf = [k.sb([128, D], F32, "Ubuf") for _ in range(2)]
        Vbuf = [k.sb([128, D], F32, "Vbuf") for _ in range(2)]
        Vbf = [k.sb([128, D], BF16, "Vbf") for _ in range(2)]
        x1t = k.sb([128, D], F32, "x1t")
        yo = k.sb([128, D], F32, "yo")
        h2b = k.sb([128, D], BF16, "h2b")
        jk = k.sb([128, 1024], BF16, "jk")
        A0 = k.sb([128, 128], F32, "A0"); A1 = k.sb([128, 128], F32, "A1")
        At = k.sb([128, 128], F32, "At"); Wt = k.sb([128, 128], F32, "Wt")
        g1 = k.sb([128, 128], F32, "g1"); g2 = k.sb([128, 128], F32, "g2")
        sm6 = k.sb([128, 8], F32, "sm6")

        k.dma(gfb[:], norm_final_g.partition_broadcast(128), [], ["gfb"])
        k.cp("dve", SELR[:], ident_f[:].unsqueeze(2).to_broadcast([128, 128, 128]), ["identf"], ["SELR"])
        idflat = cin["c_ident"].rearrange("a b -> (a b)")
        for c in range(8):
            k.dma(Ubuf[c % 2][:, :], idflat[c * 2048:(c + 1) * 2048].partition_broadcast(128), [], [("G", c % 2)])
            k.cp("dve", IDm[:, c * 16:(c + 1) * 16, :].rearrange("p a b -> p (a b)"), Ubuf[c % 2][:, :], [("G", c % 2)], ["IDm"])
        k.memset("dve", A0[:], 0.0, ["A0", "setup5b"])
        k.memset("dve", A1[:], 0.0, ["A1", "setup5b"])
        gi = [0]

        G = Ubuf + Vbuf + [k.sb([128, D], F32, "Gx") for _ in range(2)]
        NG_ = len(G)
        _unused = None

        def stage1(t):
            eT, ek = eidT_all[:, t, :], ("eidT", t)
            for tk in range(128):
                i = gi[0] % NG_
                gi[0] += 1
                k.op("pool", (lambda e, i=i, tk=tk: e.indirect_dma_start(
                    out=G[i][:, :], out_offset=None, in_=peer_u,
                    in_offset=bass.IndirectOffsetOnAxis(ap=eT[:, tk:tk + 1], axis=0))), [ek, ("gateT", t), "yo", "IDm", "SELR", "setup5b"], [("G", i)], dma=True)
                for c in range(4):
                    k.mm(PS(4 + c), SELR[:, tk, :], h2b[:, c * 512:(c + 1) * 512], True, True, ["SELR", "h2b"], psk(4 + c))
                for hf, Ax in ((0, A0), (1, A1)):
                    k.stt(jk[:, 0:1024], G[i][:, hf * 1024:(hf + 1) * 1024], 1.0, PS(4 + 2 * hf, 2), ALU.mult, ALU.mult,
                          [("G", i)] + psk(4 + 2 * hf, 2), ["jk", "A%d" % hf], accum=Ax[:, tk:tk + 1])

        def post1(t):
            gk = ("gateT", t)
            k.tt("dve", At[:], A0[:], A1[:], ALU.add, ["A0", "A1"], ["At"])
            k.act(g1[:], At[:], AF.Square, ["At"], ["g1"])
            k.ts("dve", g1[:], g1[:], 0.044715, 1.0, ALU.mult, ALU.add, ["g1"], ["g1"])
            k.tt("dve", g1[:], g1[:], At[:], ALU.mult, ["g1", "At"], ["g1"])
            k.act(g2[:], g1[:], AF.Tanh, ["g1"], ["g2"], scale=GC)
            k.ts("dve", g2[:], g2[:], 0.5, 0.5, ALU.mult, ALU.add, ["g2"], ["g2"])
            k.tt("dve", g2[:], g2[:], At[:], ALU.mult, ["g2", "At"], ["g2"])
            k.tt("dve", Wt[:], g2[:], gateT_all[:, t, :], ALU.mult, ["g2", gk], ["Wt"])
            k.tt("dve", WSEL[:], IDm[:], Wt[:].unsqueeze(2).to_broadcast([128, 128, 128]), ALU.mult, ["IDm", "Wt"], ["WSEL"])

        def stage2(t):
            eT, ek = eidT_all[:, t, :], ("eidT", t)
            for tk in range(128):
                i = gi[0] % NG_
                gi[0] += 1
                k.op("pool", (lambda e, i=i, tk=tk: e.indirect_dma_start(
                    out=G[i][:, :], out_offset=None, in_=peer_v,
                    in_offset=bass.IndirectOffsetOnAxis(ap=eT[:, tk:tk + 1], axis=0))), [ek, "WSEL"], [("G", i)], dma=True)
                j = tk % 2
                k.cp("act", Vbf[j][:], G[i][:], [("G", i)], [("Vbf", j)])
                for c in range(4):
                    k.mm(PS(c), WSEL[:, tk, :], Vbf[j][:, c * 512:(c + 1) * 512], tk == 0, tk == 127, ["WSEL", ("Vbf", j)], psk(c))

        def final(t):
            k.dma(x1t[:], SX1[row(t), :], [], ["x1t"])
            k.tt("dve", x1t[:], PS(0, 4), x1t[:], ALU.add, psk(0, 4) + ["x1t"], ["x1t"])
            k.act(yo[:], x1t[:], AF.Square, ["x1t"], ["yo", "n6ss"], accum=sm6[:, 4:5])
            k.rstd_from_ss(sm6[:, 4:5], sm6[:, 6:7], sm6[:, 5:6], nh[:], D, "n6")
            k.stt(yo[:], x1t[:], sm6[:, 6:7], gfb[:], ALU.mult, ALU.mult, ["x1t", "n6rstd", "gfb"], ["yo"])
            k.dma(y[row(t), :], yo[:], ["yo"], [("y", t)])

        for t in range(NT):
            k.dma(h2b[:], SH2[row(t), :], [], ["h2b"])
            stage1(t)
            post1(t)
            stage2(t)
            final(t)
        k.release()
        k.release()

    k.emit()
    return nc, k


_CACHE = {}


def make_in_maps(inputs):
    f = lambda a: np.ascontiguousarray(np.asarray(a, dtype=np.float32))
    consts = make_consts()
    shared = {
        "norm_mix_g": f(inputs["norm_mix_g"][0:1]),
        "w_in": f(inputs["w_in"][0]),
        "w_out": f(inputs["w_out"][0]),
        "gm_ln_g": f(inputs["gm_ln_g"][0:1]),
        "gm_ln_b": f(inputs["gm_ln_b"][0:1]),
        "gm_wT": f(np.transpose(np.asarray(inputs["gm_spatial_w"][0]), (2, 0, 1))),
        "gm_bT": f(np.transpose(np.asarray(inputs["gm_spatial_b"][0]), (1, 0))),
        "cmp_posT_k": f(np.asarray(inputs["cmp_pos_k"][0]).T),
        "cmp_posT_v": f(np.asarray(inputs["cmp_pos_v"][0]).T),
        "cmp_w1_k": f(inputs["cmp_w1_k"][0]), "cmp_w1_v": f(inputs["cmp_w1_v"][0]),
        "cmp_w2_k": f(inputs["cmp_w2_k"][0]), "cmp_w2_v": f(inputs["cmp_w2_v"][0]),
        "norm_ffn_g": f(inputs["norm_ffn_g"][0:1]),
        "peer_w_q": f(inputs["peer_w_q"][0]),
        "keys1T": f(np.asarray(inputs["peer_keys1"][0]).T),
        "keys2T": f(np.asarray(inputs["peer_keys2"][0]).T),
        "peer_u": f(inputs["peer_u"][0]),
        "peer_v": f(inputs["peer_v"][0]),
        "norm_final_g": f(np.asarray(inputs["norm_final_g"]).reshape(1, D)),
    }
    shared.update(consts)
    xs = np.asarray(inputs["x"], dtype=np.float32)
    maps = []
    for b in range(8):
        m = dict(shared)
        m["x"] = np.ascontiguousarray(xs[b])
        maps.append(m)
    return maps


def kernel(**inputs):
    if "nc" not in _CACHE:
        _CACHE["nc"] = build_program()[0]
    nc = _CACHE["nc"]
    in_maps = make_in_maps(inputs)
    res = run_bass_kernel_spmd(nc, in_maps, core_ids=list(range(8)))
    out = np.stack([np.asarray(r["y"]) for r in res.results], axis=0)
    return out.astype(np.float32)
```

```python
import numpy as np
import concourse.bass as bass
import concourse.mybir as mybir
from concourse.bass_utils import run_bass_kernel_spmd

F32 = mybir.dt.float32
BF16 = mybir.dt.bfloat16
I32 = mybir.dt.int32
U32 = mybir.dt.uint32
ALU = mybir.AluOpType
AF = mybir.ActivationFunctionType
AX = mybir.AxisListType

ENGS = ("pe", "act", "dve", "pool", "sp")
EPOCH = 30000
NDMASEM = 8
SB_BASE = 16512
SB_TOP = 229344


class Op:
    __slots__ = ("eng", "fn", "deps", "is_dma", "idx", "sig", "semref")

    def __init__(self, eng, fn, is_dma):
        self.eng = eng
        self.fn = fn
        self.deps = []
        self.is_dma = is_dma
        self.sig = False
        self.semref = None


class Prog:
    def __init__(self, nc):
        self.nc = nc
        self.ops = []
        self.last_w = {}
        self.readers = {}
        self.last_on_eng = {e: None for e in ENGS}
        self.barrier_deps = []
        self.need_barrier = {e: False for e in ENGS}
        self.sb_off = SB_BASE
        self.marks = []
        self.names = 0
        self.recent_dma = {}

    def sb(self, shape, dtype, name=None):
        self.names += 1
        name = (name or "t") + "_%d" % self.names
        esz = {F32: 4, BF16: 2, I32: 4, U32: 4}[dtype]
        n = 1
        for s in shape[1:]:
            n *= s
        nbytes = (n * esz + 31) // 32 * 32
        off = self.sb_off
        self.sb_off += nbytes
        assert self.sb_off <= SB_TOP, ("SBUF overflow", name, self.sb_off)
        return self.nc.alloc_sbuf_tensor_at(name, list(shape), dtype, offset=off)

    def mark(self):
        self.marks.append(self.sb_off)

    def release(self):
        self.sb_off = self.marks.pop()
        self.barrier()

    def barrier(self):
        self.barrier_deps = [self.last_on_eng[e] for e in ENGS if self.last_on_eng[e] is not None]
        for q in self.recent_dma.values():
            self.barrier_deps.extend(q)
        for e in ENGS:
            self.need_barrier[e] = True

    def op(self, eng, fn, reads=(), writes=(), dma=False):
        o = Op(eng, fn, dma or eng == "sp")
        o.idx = len(self.ops)
        deps = set()
        if self.need_barrier[eng]:
            deps.update(self.barrier_deps)
            self.need_barrier[eng] = False
        reads = list(reads)
        writes = list(writes)
        for k in list(reads):
            if isinstance(k, str) and k.startswith("ps"):
                reads.remove(k)
                if k not in writes:
                    writes.append(k)
        for k in reads:
            w = self.last_w.get(k)
            if w is not None:
                deps.add(w)
        for k in writes:
            w = self.last_w.get(k)
            if w is not None:
                deps.add(w)
            rd = self.readers.get(k)
            if rd:
                for v in rd.values():
                    if isinstance(v, list):
                        deps.update(v)
                    else:
                        deps.add(v)
        for k in reads:
            rd = self.readers.setdefault(k, {})
            if o.is_dma:
                rd.setdefault("dma_" + eng, []).append(o.idx)
            else:
                rd[eng] = o.idx
        for k in writes:
            self.last_w[k] = o.idx
            self.readers[k] = {}
        deps.discard(o.idx)
        o.deps = sorted(deps)
        self.ops.append(o)
        self.last_on_eng[eng] = o.idx
        if o.is_dma:
            q = self.recent_dma.setdefault(eng, [])
            q.append(o.idx)
            if len(q) > NDMASEM:
                q.pop(0)
        return o

    def emit(self):
        nc = self.nc
        ops = self.ops
        for o in ops:
            latest = {}
            keep = []
            for d in o.deps:
                p = ops[d]
                if p.is_dma:
                    keep.append(d)
                elif p.eng == "pe" and o.eng == "pe":
                    continue
                else:
                    if d > latest.get(p.eng, -1):
                        latest[p.eng] = d
            keep.extend(latest.values())
            o.deps = sorted(keep)
            for d in o.deps:
                ops[d].sig = True
        sems = {}

        def getsem(key):
            if key not in sems:
                sems[key] = nc.alloc_semaphore("s_%s_%s" % key)
            return sems[key]

        cnt = {e: 0 for e in ENGS}
        dcnt = {}
        dper = {}
        prev_on_dsem = {}
        for o in ops:
            if o.is_dma:
                q = o.eng
                i = dcnt.get(q, 0)
                dcnt[q] = i + 1
                j = i % NDMASEM
                key = ("d" + q, j)
                c = dper.get(key, 0) + 1
                dper[key] = c
                o.semref = (key, 16 * c)
                if c > 1:
                    o.deps = sorted(set(o.deps) | {prev_on_dsem[key]})
                prev_on_dsem[key] = o.idx
                o.sig = True
            elif o.sig:
                kk = cnt[o.eng]
                cnt[o.eng] = kk + 1
                o.semref = ((o.eng, kk // EPOCH), (kk % EPOCH) + 1)
        for o in ops:
            if o.semref is not None:
                getsem(o.semref[0])
        self.stats = dict(nops=len(ops), nsig=sum(1 for o in ops if o.sig), cnt=dict(cnt), dcnt=dict(dcnt),
                          nsem=len(sems))
        per_eng = {e: [o for o in ops if o.eng == e] for e in ENGS}
        final_dma = {key: 16 * c for key, c in dper.items()}

        def run(engname, eng):
            waited = {}
            for o in per_eng[engname]:
                for d in o.deps:
                    p = ops[d]
                    if p.semref is None:
                        continue
                    if p.eng == "pe" and o.eng == "pe" and not p.is_dma:
                        continue
                    key, val = p.semref
                    if waited.get(key, 0) >= val:
                        continue
                    waited[key] = val
                    eng.wait_ge(sems[key], val)
                inst = o.fn(eng)
                if o.semref is not None:
                    key, val = o.semref
                    inst.then_inc(sems[key], 16 if o.is_dma else 1)
            if engname == "sp":
                for key, val in final_dma.items():
                    if waited.get(key, 0) < val:
                        eng.wait_ge(sems[key], val)

        with nc.Block() as block:
            @block.tensor
            def _(e):
                run("pe", e)

            @block.scalar
            def _(e):
                run("act", e)

            @block.vector
            def _(e):
                run("dve", e)

            @block.gpsimd
            def _(e):
                run("pool", e)

            @block.sync
            def _(e):
                run("sp", e)


class K(Prog):
    def mm(self, out, lhsT, rhs, start, stop, r, w):
        return self.op("pe", lambda e: e.matmul(out=out, lhsT=lhsT, rhs=rhs, start=start, stop=stop), r, w)

    def tr(self, out, in_, ident, r, w):
        return self.op("pe", lambda e: e.transpose(out=out, in_=in_, identity=ident), r, w)

    def act(self, out, in_, func, r, w, scale=1.0, bias=None, accum=None):
        def f(e):
            kw = dict(out=out, in_=in_, func=func, scale=scale)
            if bias is not None:
                kw["bias"] = bias
            if accum is not None:
                kw["accum_out"] = accum
            return e.activation(**kw)
        return self.op("act", f, r, w)

    def tt(self, eng, out, a, b, op, r, w):
        return self.op(eng, lambda e: e.tensor_tensor(out=out, in0=a, in1=b, op=op), r, w)

    def ts(self, eng, out, a, s1, s2, op0, op1, r, w):
        if s2 is None:
            return self.op(eng, lambda e: e.tensor_scalar(out=out, in0=a, scalar1=s1, scalar2=None, op0=op0), r, w)
        return self.op(eng, lambda e: e.tensor_scalar(out=out, in0=a, scalar1=s1, scalar2=s2, op0=op0, op1=op1), r, w)

    def stt(self, out, a, s, b, op0, op1, r, w, accum=None):
        def f(e):
            kw = dict(out=out, in0=a, scalar=s, in1=b, op0=op0, op1=op1)
            if accum is not None:
                kw["accum_out"] = accum
            return e.scalar_tensor_tensor(**kw)
        return self.op("dve", f, r, w)

    def cp(self, eng, out, in_, r, w):
        if eng == "act":
            return self.op("act", lambda e: e.copy(out=out, in_=in_), r, w)
        return self.op(eng, lambda e: e.tensor_copy(out=out, in_=in_), r, w)

    def recip(self, out, in_, r, w):
        return self.op("dve", lambda e: e.reciprocal(out=out, in_=in_), r, w)

    def rsum(self, out, in_, r, w):
        return self.op("dve", lambda e: e.reduce_sum(out=out, in_=in_, axis=AX.X), r, w)

    def max8(self, out, in_, r, w):
        return self.op("dve", lambda e: e.max(out=out, in_=in_), r, w)

    def maxidx(self, out, in_max, in_values, r, w):
        return self.op("dve", lambda e: e.max_index(out=out, in_max=in_max, in_values=in_values), r, w)

    def matchrep(self, out, rep, vals, imm, r, w):
        return self.op("dve", lambda e: e.match_replace(out=out, in_to_replace=rep, in_values=vals, imm_value=imm), r, w)

    def memset(self, eng, ap, val, w):
        return self.op(eng, lambda e: e.memset(ap, val), (), w)

    def dma(self, out, in_, r, w):
        return self.op("sp", lambda e: e.dma_start(out=out, in_=in_), r, w)

    def rstd_from_ss(self, ss, rstd, tmp, nh, n, key):
        self.ts("dve", tmp, ss, 1.0 / n, 1e-6, ALU.mult, ALU.add, [key + "ss"], [key + "ms"])
        self.tt("pool", rstd, tmp, nh, ALU.pow, [key + "ms", "nh"], [key + "rstd"])

S = 2048
D = 2048
NT = 16
GC = 0.7978845608028654
SCALE = 128 ** -0.5
NEG = -30000.0
C_U, C_V, C_Q, C_KV, C_NG, C_MG = 0, 2048, 4096, 6144, 9216, 9264

CONST_SHAPES = {
    "c_cos": [128, S], "c_sin": [128, S], "c_rt": [128, 128], "c_caus": [128, 128], "c_winb": [128, 128],
    "c_cmpb": [128, S], "c_aagg": [128, 32], "c_am": [128, 16, 32], "c_add": [128, 16, 32],
    "c_esel": [32, 16, 128], "c_tril": [128, 16, 128], "c_ident": [128, 128], "c_iota16": [128, 16],
    "c_zrow": [128, 255],
}


def make_consts():
    c = {}
    half = 64
    inv = 10000.0 ** (-np.arange(half, dtype=np.float32) / half)
    ang = np.arange(S, dtype=np.float32)[:, None] * inv[None, :]
    cos = np.cos(ang).astype(np.float32).T
    sin = np.sin(ang).astype(np.float32).T
    c["c_cos"] = np.concatenate([cos, cos], 0)
    c["c_sin"] = np.concatenate([sin, sin], 0)
    rt = np.zeros((128, 128), np.float32)
    for m in range(128):
        if m < 64:
            rt[m + 64, m] = -1.0
        else:
            rt[m - 64, m] = 1.0
    c["c_rt"] = rt
    kk = np.arange(128)[:, None]
    qq = np.arange(128)[None, :]
    c["c_caus"] = np.where(kk <= qq, 0.0, NEG).astype(np.float32)
    c["c_winb"] = np.where(kk > qq, 0.0, NEG).astype(np.float32)
    n = np.arange(128)[:, None]
    q = np.arange(S)[None, :]
    c["c_cmpb"] = np.where((16 * n + 31 <= q) & (n < 127), 0.0, NEG).astype(np.float32)
    agg = np.array([1, 2, 2, 2, 1], np.float32)
    a = np.zeros((128, 32), np.float32)
    for nn in range(127):
        for j in range(32):
            w = nn + 1 - 4 * j
            if 0 <= w <= 4:
                a[nn, j] = agg[w]
    c["c_aagg"] = a
    pos = (np.arange(16)[None, :, None] * 128 + np.arange(128)[:, None, None])
    j = np.arange(32)[None, None, :]
    allowed = (j * 64) <= pos
    forced = (j == 0) | (j == (pos // 64))
    c["c_am"] = (allowed & ~forced).astype(np.float32)
    c["c_add"] = np.where(forced, 1e30, np.where(allowed, 0.0, -1e30)).astype(np.float32)
    e = np.zeros((32, 16, 128), np.float32)
    for kt in range(16):
        for key in range(128):
            e[2 * kt + key // 64, kt, key] = 1.0
    c["c_esel"] = e
    s_ = np.arange(128)[:, None, None]
    t_ = np.arange(128)[None, None, :]
    c["c_tril"] = np.broadcast_to((s_ <= t_), (128, 16, 128)).astype(np.float32).copy()
    c["c_ident"] = np.eye(128, dtype=np.float32)
    c["c_iota16"] = np.broadcast_to(np.arange(16, dtype=np.float32)[None, :], (128, 16)).copy()
    z = np.zeros((128, 255), np.float32)
    z[:, 127] = 1.0
    c["c_zrow"] = z
    return c


def build_program(stop_after=99, dbg=()):
    nc = bass.Bass("TRN2", target_bir_lowering=False)
    k = K(nc)

    def din(name, shape):
        return nc.dram_tensor(name, list(shape), F32, kind="ExternalInput").ap()

    def dscr(name, shape, dt=F32):
        kind = "ExternalOutput" if name in dbg else "Internal"
        return nc.dram_tensor(name, list(shape), dt, kind=kind).ap()

    x = din("x", [S, D])
    norm_mix_g = din("norm_mix_g", [1, D])
    w_in = din("w_in", [D, 13360])
    w_out = din("w_out", [D, D])
    gm_ln_g = din("gm_ln_g", [1, D])
    gm_ln_b = din("gm_ln_b", [1, D])
    gm_wT = din("gm_wT", [128, 16, 128])
    gm_bT = din("gm_bT", [128, 16])
    posT = {"k": din("cmp_posT_k", [128, 32]), "v": din("cmp_posT_v", [128, 32])}
    cw1 = {"k": din("cmp_w1_k", [4096, 256]), "v": din("cmp_w1_v", [4096, 256])}
    cw2 = {"k": din("cmp_w2_k", [256, 128]), "v": din("cmp_w2_v", [256, 128])}
    norm_ffn_g = din("norm_ffn_g", [1, D])
    peer_w_q = din("peer_w_q", [D, D])
    keysT = [din("keys1T", [128, 128]), din("keys2T", [128, 128])]
    NEXP = 16384 if stop_after >= 5 else 128
    peer_u = din("peer_u", [NEXP, D])
    peer_v = din("peer_v", [NEXP, D])
    norm_final_g = din("norm_final_g", [1, D])
    cin = {n: din(n, s) for n, s in CONST_SHAPES.items()}
    y = nc.dram_tensor("y", [S, D], F32, kind="ExternalOutput").ap()

    SU = dscr("s_u", [S, D]); SV = dscr("s_v", [S, D]); SVS = dscr("s_vs", [S, 512]); SVW = dscr("s_vw", [S, 512])
    SNG = dscr("s_ng", [S, 48]); SMG = dscr("s_mg", [S, 4096])
    SQ = dscr("s_q", [16, 128, S]); SKC = dscr("s_kc", [4, 128, S]); SVC = dscr("s_vc", [4, 128, S])
    SKS = dscr("s_ks", [4, 128, S]); SKW = dscr("s_kw", [4, 128, S])
    SMA = dscr("s_ma", [S, D]); SOB = dscr("s_ob", [S, D]); SX1 = dscr("s_x1", [S, D])
    SH2 = dscr("s_h2", [S, D], BF16); SS = dscr("s_ss", [S, 2048])

    PSALL = nc.alloc_psum_tensor("psall", [128, 4096], F32)

    def PS(b, n=1):
        return PSALL[:, b * 512:(b + n) * 512]

    def PSB(b):
        return PSALL[:, b * 512:(b + 1) * 512].bitcast(BF16)

    def psk(b, n=1):
        return ["ps%d" % i for i in range(b, b + n)]

    ident_f = k.sb([128, 128], F32, "identf")
    ident_b = k.sb([128, 128], BF16, "identb")
    ones_b = k.sb([128, 128], BF16, "onesb")
    nh = k.sb([128, 1], F32, "nh")
    k.dma(ident_f[:], cin["c_ident"], [], ["identf"])
    k.cp("dve", ident_b[:], ident_f[:], ["identf"], ["identb"])
    k.memset("pool", ones_b[:], 1.0, ["onesb"])
    k.memset("pool", nh[:], -0.5, ["nh"])

    def row(t):
        return slice(t * 128, (t + 1) * 128)

    k.mark()
    hT = k.sb([128, 16, S], BF16, "hT")
    k.mark()
    gb = k.sb([128, D], F32, "gb")
    xt = [k.sb([128, D], F32, "xt") for _ in range(2)]
    junk = k.sb([128, D], BF16, "junk")
    hb = [k.sb([128, D], BF16, "hb") for _ in range(2)]
    sm = [k.sb([128, 4], F32, "sm") for _ in range(2)]
    k.dma(gb[:], norm_mix_g.partition_broadcast(128), [], ["gb"])
    for t in range(NT):
        p = t % 2
        kp = "a%d" % p
        k.dma(xt[p][:], x[row(t), :], [], [kp + "xt"])
        k.act(junk[:], xt[p][:], AF.Square, [kp + "xt"], ["junk", kp + "ss"], accum=sm[p][:, 0:1])
        k.rstd_from_ss(sm[p][:, 0:1], sm[p][:, 2:3], sm[p][:, 1:2], nh[:], D, kp)
        k.stt(hb[p][:], xt[p][:], sm[p][:, 2:3], gb[:], ALU.mult, ALU.mult, [kp + "xt", kp + "rstd", "gb"], [kp + "hb"])
        for kc in range(16):
            b = 2 * p + kc // 8
            k.tr(PSB(b)[:, (kc % 8) * 128:(kc % 8 + 1) * 128], hb[p][:, kc * 128:(kc + 1) * 128], ident_b[:],
                 [kp + "hb", "identb"], psk(b))
        for hh in range(2):
            b = 2 * p + hh
            k.cp("dve" if hh == 0 else "act", hT[:, hh * 8:(hh + 1) * 8, row(t)],
                 PSB(b).rearrange("p (a b) -> p a b", b=128), psk(b), [("hT", t)])
    k.release()

    if stop_after >= 1:
        wf = [k.sb([128, 16, 512], F32, "wf") for _ in range(2)]
        wb = [k.sb([128, 16, 512], BF16, "wb") for _ in range(2)]
        ob = [k.sb([128, 512], F32, "ob") for _ in range(4)]
        chunks = []
        for c in range(4):
            chunks.append((C_U + c * 512, 512, "tm", (SU, c * 512)))
        for c in range(4):
            chunks.append((C_V + c * 512, 512, "tm", (SV, c * 512)))
        for c in range(4):
            chunks.append((C_Q + c * 512, 512, "fm", [SQ[4 * c + i] for i in range(4)]))
        chunks.append((C_KV + 0 * 512, 512, "fm", [SKC[i] for i in range(4)]))
        chunks.append((C_KV + 1 * 512, 512, "fm", [SVC[i] for i in range(4)]))
        chunks.append((C_KV + 2 * 512, 512, "fm", [SKS[i] for i in range(4)]))
        chunks.append((C_KV + 3 * 512, 512, "tm", (SVS, 0)))
        chunks.append((C_KV + 4 * 512, 512, "fm", [SKW[i] for i in range(4)]))
        chunks.append((C_KV + 5 * 512, 512, "tm", (SVW, 0)))
        chunks.append((C_NG, 48, "tm", (SNG, 0)))
        for c in range(8):
            chunks.append((C_MG + c * 512, 512, "tm", (SMG, c * 512)))

        def load_chunk(ci):
            c0, n, _, _ = chunks[ci]
            p = ci % 2
            k.dma(wf[p][:, :, 0:n], w_in[:, c0:c0 + n].rearrange("(kc p) n -> p kc n", p=128), [], [("wf", p)])

        cnt = 0
        load_chunk(0)
        for ci, (c0, n, kind, dest) in enumerate(chunks):
            p = ci % 2
            if ci + 1 < len(chunks):
                load_chunk(ci + 1)
            k.cp("dve", wb[p][:, 0:8, 0:n], wf[p][:, 0:8, 0:n], [("wf", p)], [("wb", p)])
            k.cp("act", wb[p][:, 8:16, 0:n], wf[p][:, 8:16, 0:n], [("wf", p)], [("wb", p)])
            for u in range(16):
                b = 2 + cnt % 6
                o = ob[cnt % 4]
                okey = ("ob", cnt % 4)
                if kind == "tm":
                    t = u
                    for kc in range(16):
                        k.mm(PS(b)[:, 0:n], hT[:, kc, row(t)], wb[p][:, kc, 0:n], kc == 0, kc == 15,
                             [("hT", t), ("wb", p)], psk(b))
                    dst = dest[0][row(t), dest[1]:dest[1] + n]
                else:
                    blk, tc = u // 4, u % 4
                    for kc in range(16):
                        k.mm(PS(b)[:, 0:512], wb[p][:, kc, blk * 128:(blk + 1) * 128], hT[:, kc, tc * 512:(tc + 1) * 512],
                             kc == 0, kc == 15, [("hT", 4 * tc + i) for i in range(4)] + [("wb", p)], psk(b))
                    dst = dest[blk][:, tc * 512:(tc + 1) * 512]
                k.cp("act" if cnt % 2 == 0 else "dve", o[:, 0:n], PS(b)[:, 0:n], psk(b), [okey])
                k.dma(dst, o[:, 0:n], [okey], [("scr", ci, u)])
                cnt += 1
    k.release()
    k.barrier()

    if stop_after >= 2:
        k.mark()
        gam = k.sb([128, D], F32, "gam")
        bet = k.sb([128, D], F32, "bet")
        BS = k.sb([128, 16, 128], F32, "BS")
        WTm = k.sb([128, 16, 128], BF16, "WTm")
        bsT = k.sb([128, 16], F32, "bsT")
        k.mark()
        st1 = k.sb([128, 16, 128], F32, "st1")
        st2 = k.sb([128, 16, 128], F32, "st2")
        k.dma(gam[:], gm_ln_g.partition_broadcast(128), [], ["gam"])
        k.dma(bet[:], gm_ln_b.partition_broadcast(128), [], ["bet"])
        k.dma(bsT[:], gm_bT, [], ["bsT"])
        k.dma(st1[:], gm_wT, [], ["st1"])
        k.dma(st2[:], cin["c_tril"], [], ["st2"])
        k.tt("dve", WTm[:], st1[:], st2[:], ALU.mult, ["st1", "st2"], ["WTm"])
        k.cp("dve", BS[:], bsT[:].unsqueeze(2).to_broadcast([128, 16, 128]), ["bsT"], ["BS"])
        k.release()
        ut = [k.sb([128, D], F32, "ut") for _ in range(2)]
        vt = [k.sb([128, D], F32, "vt") for _ in range(2)]
        gt = [k.sb([128, D], F32, "gt") for _ in range(2)]
        tA = k.sb([128, D], F32, "tA")
        tB = k.sb([128, D], F32, "tB")
        Uh = k.sb([128, D], F32, "Uh")
        G2 = k.sb([128, D], F32, "G2")
        vln = k.sb([128, D], BF16, "vln")
        mo = [k.sb([128, D], F32, "mo") for _ in range(2)]
        sm2 = [k.sb([128, 8], F32, "sm2") for _ in range(2)]
        BSf = BS[:].rearrange("p a b -> p (a b)")

        def load2(t):
            p = t % 2
            k.dma(ut[p][:], SU[row(t), :], [], [("ut", p)])
            k.dma(vt[p][:], SV[row(t), :], [], [("vt", p)])
            k.dma(gt[p][:], SMG[row(t), 0:D], [], [("gt", p)])

        def tanh_inner(src, skey):
            k.act(tA[:], src, AF.Square, [skey], ["tA"])
            k.ts("dve", tA[:], tA[:], 0.044715, 1.0, ALU.mult, ALU.add, ["tA"], ["tA"])
            k.tt("dve", tA[:], tA[:], src, ALU.mult, ["tA", skey], ["tA"])
            k.act(tB[:], tA[:], AF.Tanh, ["tA"], ["tB"], scale=GC)

        load2(0)
        for t in range(NT):
            p = t % 2
            if t + 1 < NT:
                load2(t + 1)
            s = sm2[p]
            tanh_inner(ut[p][:], ("ut", p))
            k.ts("dve", tB[:], tB[:], 0.25, 0.25, ALU.mult, ALU.add, ["tB"], ["tB"])
            k.tt("dve", Uh[:], tB[:], ut[p][:], ALU.mult, ["tB", ("ut", p)], ["Uh"])
            tanh_inner(vt[p][:], ("vt", p))
            k.stt(G2[:], tB[:], 1.0, vt[p][:], ALU.add, ALU.mult, ["tB", ("vt", p)], ["G2", ("s1", p)], accum=s[:, 0:1])
            k.act(tA[:], G2[:], AF.Square, ["G2"], ["tA", ("s2", p)], accum=s[:, 1:2])
            k.ts("dve", s[:, 2:3], s[:, 0:1], 1.0 / D, None, ALU.mult, None, [("s1", p)], [("mean2", p)])
            k.tt("dve", s[:, 3:4], s[:, 2:3], s[:, 2:3], ALU.mult, [("mean2", p)], [("msq", p)])
            k.stt(s[:, 4:5], s[:, 1:2], 1.0 / D, s[:, 3:4], ALU.mult, ALU.subtract, [("s2", p), ("msq", p)], [("var4", p)])
            k.ts("dve", s[:, 5:6], s[:, 4:5], 0.25, 1e-6, ALU.mult, ALU.add, [("var4", p)], [("v4", p)])
            k.tt("pool", s[:, 6:7], s[:, 5:6], nh[:], ALU.pow, [("v4", p), "nh"], [("rs", p)])
            k.ts("dve", s[:, 7:8], s[:, 6:7], 0.5, None, ALU.mult, None, [("rs", p)], [("rsh", p)])
            k.ts("dve", tA[:], G2[:], s[:, 2:3], s[:, 7:8], ALU.subtract, ALU.mult, ["G2", ("mean2", p), ("rsh", p)], ["tA"])
            k.tt("dve", tA[:], tA[:], gam[:], ALU.mult, ["tA", "gam"], ["tA"])
            k.tt("dve", vln[:], tA[:], bet[:], ALU.add, ["tA", "bet"], ["vln"])
            b0 = 4 * p
            for g in range(16):
                b = b0 + g // 4
                k.mm(PS(b)[:, (g % 4) * 128:(g % 4 + 1) * 128], WTm[:, g, :], vln[:, g * 128:(g + 1) * 128], True, True,
                     ["WTm", "vln"], psk(b))
            k.tt("dve", tA[:], PS(b0, 4), BSf, ALU.add, psk(b0, 4) + ["BS"], ["tA"])
            k.tt("dve", tA[:], tA[:], Uh[:], ALU.mult, ["tA", "Uh"], ["tA"])
            k.act(tB[:], gt[p][:], AF.Tanh, [("gt", p)], ["tB"], scale=0.5)
            k.stt(mo[p][:], tB[:], 1.0, tA[:], ALU.add, ALU.mult, ["tB", "tA"], [("mo", p)])
            k.dma(SMA[row(t), :], mo[p][:], [("mo", p)], [("sma", t)])
        k.release()
        k.barrier()

    if stop_after >= 3:
        k.mark()
        cosT = k.sb([128, S], F32, "cosT"); sinT = k.sb([128, S], F32, "sinT")
        RT = k.sb([128, 128], F32, "RT")
        CAUS = k.sb([128, 128], BF16, "CAUS"); WINB = k.sb([128, 128], BF16, "WINB")
        CMPB = k.sb([128, S], BF16, "CMPB")
        Aagg = k.sb([128, 32], F32, "Aagg")
        AM = k.sb([128, 16, 32], F32, "AM"); ADDM = k.sb([128, 16, 32], F32, "ADDM")
        ESEL = k.sb([32, 16, 128], BF16, "ESEL")
        w1b = {m: k.sb([128, 32, 256], BF16, "w1b") for m in "kv"}
        w2b = {m: k.sb([128, 2, 128], BF16, "w2b") for m in "kv"}
        posb = {m: k.sb([128, 32], BF16, "posb") for m in "kv"}
        cvec = {m: k.sb([128, 2], F32, "cvec") for m in "kv"}
        stg = k.sb([128, 2 * S], F32, "stg")
        kcb = k.sb([128, S], BF16, "kcb"); vcb = k.sb([128, S], BF16, "vcb")
        ksr = k.sb([128, S], BF16, "ksr"); kwr = k.sb([128, S], BF16, "kwr")
        vsa = k.sb([128, 16, 129], BF16, "vsa"); vwa = k.sb([128, 16, 129], BF16, "vwa")
        qb = [k.sb([128, S], BF16, "qb") for _ in range(4)]
        qr = [k.sb([128, S], BF16, "qr") for _ in range(4)]
        sgr = k.sb([128, 16, 12], F32, "sgr"); sg = k.sb([128, 16, 12], F32, "sg")
        kcT = k.sb([128, 128], BF16, "kcT"); vca = k.sb([128, 130], BF16, "vca")
        hid = k.sb([128, 2, 128], BF16, "hid")
        hx = k.sb([128, 128], F32, "hx"); hy = k.sb([128, 128], F32, "hy"); hz = k.sb([128, 128], F32, "hz")
        biasT = k.sb([32, S], BF16, "biasT")
        PcT = [k.sb([128, 512], BF16, "PcT") for _ in range(2)]
        pn = k.sb([128, 512], F32, "pn"); rZ = [k.sb([128, 512], F32, "rZ") for _ in range(2)]; impS = k.sb([32, 512], F32, "impS")
        sc = k.sb([128, 128], F32, "sc"); selb = k.sb([128, 128], F32, "selb"); wk32 = k.sb([128, 32], F32, "wk32")
        m8 = k.sb([128, 16], F32, "m8")
        accp = [k.sb([128, 4, 4, 128], F32, "acc") for _ in range(2)]
        PT = [k.sb([128, 512], BF16, "PT") for _ in range(3)]
        t1 = [k.sb([128, 512], F32, "t1") for _ in range(2)]
        t2 = [k.sb([128, 512], F32, "t2") for _ in range(2)]
        czs = [k.sb([128, 2], F32, "cz") for _ in range(4)]

        def stgv(b):
            return stg[:, b * S:(b + 1) * S]

        k.dma(cosT[:], cin["c_cos"], [], ["cosT"]); k.dma(sinT[:], cin["c_sin"], [], ["sinT"])
        k.dma(RT[:], cin["c_rt"], [], ["RT"]); k.dma(Aagg[:], cin["c_aagg"], [], ["Aagg"])
        k.dma(AM[:], cin["c_am"], [], ["AM"]); k.dma(ADDM[:], cin["c_add"], [], ["ADDM"])
        k.dma(stgv(0), cin["c_cmpb"], [], [("stg", 0)])
        k.cp("dve", CMPB[:], stgv(0), [("stg", 0)], ["CMPB"])
        k.dma(stg[:, S:S + 128], cin["c_caus"], [], [("stg", 1)])
        k.dma(stg[:, S + 128:S + 256], cin["c_winb"], [], [("stg", 1)])
        k.cp("dve", CAUS[:], stg[:, S:S + 128], [("stg", 1)], ["CAUS"])
        k.cp("dve", WINB[:], stg[:, S + 128:S + 256], [("stg", 1)], ["WINB"])
        k.dma(stg[0:32, 0:S].rearrange("p (a b) -> p a b", b=128), cin["c_esel"], [("stg", 0)], [("stg", 0)])
        k.cp("dve", ESEL[:], stg[0:32, 0:S].rearrange("p (a b) -> p a b", b=128), [("stg", 0)], ["ESEL"])
        for m in "kv":
            for hf in range(2):
                k.dma(stg[:].rearrange("p (l n) -> p l n", n=256),
                      cw1[m][hf * 2048:(hf + 1) * 2048, :].rearrange("(l d) n -> d l n", d=128), [], [("stg", 0), ("stg", 1)])
                k.cp("dve" if hf == 0 else "act", w1b[m][:, hf * 16:(hf + 1) * 16, :], stg[:].rearrange("p (l n) -> p l n", n=256),
                     [("stg", 0), ("stg", 1)], [("w1b", m)])
            k.dma(stg[:, 0:256].rearrange("p (h d) -> p h d", d=128), cw2[m].rearrange("(h p) d -> p h d", p=128), [], [("stg", 0)])
            k.cp("dve", w2b[m][:], stg[:, 0:256].rearrange("p (h d) -> p h d", d=128), [("stg", 0)], [("w2b", m)])
            k.dma(stg[:, S:S + 32], posT[m], [], [("stg", 1)])
            k.cp("dve", posb[m][:], stg[:, S:S + 32], [("stg", 1)], [("posb", m)])
            for hf in range(2):
                for l in range(32):
                    k.mm(PS(7)[:, hf:hf + 1], w1b[m][:, l, hf * 128:(hf + 1) * 128], posb[m][:, l:l + 1], l == 0, l == 31,
                         [("w1b", m), ("posb", m)], psk(7))
            k.cp("dve", cvec[m][:], PS(7)[:, 0:2], psk(7), [("cvec", m)])
        k.memset("pool", vsa[:, :, 128:129], 1.0, ["vsa"])
        k.memset("pool", vwa[:, :, 128:129], 1.0, ["vwa"])
        k.memset("pool", vca[:, 128:129], 1.0, ["vca"])

        rot_i = [0]

        def rotary(src, skey, dst, dkey):
            for tc in range(4):
                i = rot_i[0] % 2
                rot_i[0] += 1
                sl = slice(tc * 512, (tc + 1) * 512)
                b = 4 + i
                k.mm(PS(b), RT[:], src[:, sl], True, True, [skey, "RT"], psk(b))
                k.tt("dve", t1[i][:], PS(b), sinT[:, sl], ALU.mult, psk(b) + ["sinT"], [("t1", i)])
                k.tt("pool", t2[i][:], src[:, sl], cosT[:, sl], ALU.mult, [skey, "cosT"], [("t2", i)])
                k.tt("dve", dst[:, sl], t1[i][:], t2[i][:], ALU.add, [("t1", i), ("t2", i)], [dkey])

        def compress(m, src, skey):
            for hf in range(2):
                b = 4 + hf
                for l in range(32):
                    k.mm(PS(b)[:, 0:127], w1b[m][:, l, hf * 128:(hf + 1) * 128], src[:, l:l + 16 * 126 + 1:16], l == 0, l == 31,
                         [("w1b", m), skey], psk(b))
                k.ts("dve", hx[:, 0:127], PS(b)[:, 0:127], cvec[m][:, hf:hf + 1], None, ALU.add, None, psk(b) + [("cvec", m)], ["hx"])
                k.act(hy[:, 0:127], hx[:, 0:127], AF.Square, ["hx"], ["hy"])
                k.ts("dve", hy[:, 0:127], hy[:, 0:127], 0.044715, 1.0, ALU.mult, ALU.add, ["hy"], ["hy"])
                k.tt("dve", hy[:, 0:127], hy[:, 0:127], hx[:, 0:127], ALU.mult, ["hy", "hx"], ["hy"])
                k.act(hz[:, 0:127], hy[:, 0:127], AF.Tanh, ["hy"], ["hz"], scale=GC)
                k.ts("dve", hz[:, 0:127], hz[:, 0:127], 0.5, 0.5, ALU.mult, ALU.add, ["hz"], ["hz"])
                k.tt("dve", hid[:, hf, 0:127], hz[:, 0:127], hx[:, 0:127], ALU.mult, ["hz", "hx"], ["hid"])
            if m == "k":
                for hf in range(2):
                    k.mm(PS(6)[:, 0:127], w2b[m][:, hf, :], hid[:, hf, 0:127], hf == 0, hf == 1, [("w2b", m), "hid"], psk(6))
                k.cp("act", kcT[:, 0:127], PS(6)[:, 0:127], psk(6), ["kcT"])
            else:
                for hf in range(2):
                    k.mm(PS(6)[0:127, 0:128], hid[:, hf, 0:127], w2b[m][:, hf, :], hf == 0, hf == 1, [("w2b", m), "hid"], psk(6))
                k.cp("act", vca[0:127, 0:128], PS(6)[0:127, 0:128], psk(6), ["vca"])

        sb_i = [0]
        ob_i = [0]
        pt_i = [0]
        cz_i = [0]
        OBANKS = [2, 3, 7]

        def attn(kT, kkey, va, vkey, qrt, qkey, qt, ktl, use_sel):
            obk = OBANKS[ob_i[0] % 3]
            ob_i[0] += 1
            nk = len(ktl)
            done = 0
            for g0 in range(0, nk, 4):
                grp = ktl[g0:g0 + 4]
                sbk = sb_i[0] % 2
                sb_i[0] += 1
                for j, (kt, eb) in enumerate(grp):
                    out = PS(sbk)[:, j * 128:(j + 1) * 128]
                    nmm = 1 + (1 if use_sel else 0) + (1 if eb else 0)
                    k.mm(out, kT[:, kt * 128:(kt + 1) * 128], qrt[:, row(qt)], True, nmm == 1, [kkey, qkey], psk(sbk))
                    i = 1
                    if use_sel:
                        i += 1
                        k.mm(out, ESEL[0:32, kt, :], biasT[0:32, row(qt)], False, i == nmm, ["ESEL", "biasT"], psk(sbk))
                    if eb:
                        k.mm(out, ident_b[:], (CAUS if eb == "caus" else WINB)[:], False, True, ["identb", "CAUS", "WINB"], psk(sbk))
                n = len(grp) * 128
                pi = pt_i[0] % 3
                pt_i[0] += 1
                k.act(PT[pi][:, 0:n], PS(sbk)[:, 0:n], AF.Exp, psk(sbk), [("PT", pi)], scale=SCALE)
                for j, (kt, eb) in enumerate(grp):
                    k.mm(PS(obk)[:, 0:129], PT[pi][:, j * 128:(j + 1) * 128], va[:, kt, :], done == 0, done == nk - 1,
                         [("PT", pi), vkey], psk(obk))
                    done += 1
            return obk

        def combine(obk, qt, qtl, g, br, ap, akey, first, guard):
            cz = czs[cz_i[0] % 4]
            ck = ("cz", cz_i[0] % 4)
            cz_i[0] += 1
            if guard:
                k.ts("dve", cz[:, 0:1], PS(obk)[:, 128:129], 1e-30, None, ALU.max, None, psk(obk), [ck])
                k.recip(cz[:, 0:1], cz[:, 0:1], [ck], [ck])
            else:
                k.recip(cz[:, 0:1], PS(obk)[:, 128:129], psk(obk), [ck])
            gate = sg[:, qt, g * 3 + br:g * 3 + br + 1]
            if first:
                k.ts("dve", ap[:, qtl, g, :], PS(obk)[:, 0:128], cz[:, 0:1], gate, ALU.mult, ALU.mult, psk(obk) + [ck, "sg"], [akey])
            else:
                k.tt("dve", cz[:, 1:2], cz[:, 0:1], gate, ALU.mult, [ck, "sg"], [ck])
                k.stt(ap[:, qtl, g, :], PS(obk)[:, 0:128], cz[:, 1:2], ap[:, qtl, g, :], ALU.mult, ALU.add, psk(obk) + [ck, akey], [akey])

        for hk in range(4 if stop_after >= 3 else 0):
            si = [0]

            def nxt():
                b = si[0] % 2
                si[0] += 1
                return b
            for nm, src, dst in (("kcb", SKC[hk], kcb), ("vcb", SVC[hk], vcb)):
                b = nxt()
                k.dma(stgv(b), src, [], [("stg", b)])
                k.cp("act", dst[:], stgv(b), [("stg", b)], [nm])
            for nm, src, dst in (("ksr", SKS[hk], ksr), ("kwr", SKW[hk], kwr)):
                b = nxt()
                k.dma(stgv(b), src, [], [("stg", b)])
                rotary(stgv(b), ("stg", b), dst, nm)
            for g in range(4):
                b = nxt()
                k.dma(stgv(b), SQ[4 * hk + g], [], [("stg", b)])
                k.cp("act", qb[g][:], stgv(b), [("stg", b)], [("qb", g)])
                rotary(stgv(b), ("stg", b), qr[g], ("qr", g))
            for nm, src, dst in (("vsa", SVS, vsa), ("vwa", SVW, vwa)):
                b = nxt()
                k.dma(stgv(b).rearrange("p (t d) -> p t d", d=128),
                      src[:, hk * 128:(hk + 1) * 128].rearrange("(t p) d -> p t d", p=128), [], [("stg", b)])
                k.cp("dve", dst[:, :, 0:128], stgv(b).rearrange("p (t d) -> p t d", d=128), [("stg", b)], [nm])
            k.dma(sgr[:], SNG[:, hk * 12:(hk + 1) * 12].rearrange("(t p) c -> p t c", p=128), [], ["sgr"])
            k.act(sg[:], sgr[:], AF.Tanh, ["sgr"], ["sg"], scale=0.5)
            k.ts("dve", sg[:], sg[:], 0.5, 0.5, ALU.mult, ALU.add, ["sg"], ["sg"])
            compress("k", kcb, "kcb")
            compress("v", vcb, "vcb")

            def cmpsel_gen(qc):
                qsl = slice(qc * 512, (qc + 1) * 512)
                par = qc % 2
                ap = accp[par]
                def cmp_a(g):
                    pc = PcT[g % 2]
                    pk = ("pc", g % 2)
                    k.mm(PS(5)[0:127, :], kcT[:, 0:127], qb[g][:, qsl], True, False, ["kcT", ("qb", g)], psk(5))
                    k.mm(PS(5)[0:127, :], ident_b[0:127, 0:127], CMPB[0:127, qsl], False, True, ["identb", "CMPB"], psk(5))
                    k.act(pc[0:127, :], PS(5)[0:127, :], AF.Exp, psk(5), [pk], scale=SCALE)
                    k.mm(PS(5), ones_b[0:127, :], pc[0:127, :], True, True, ["onesb", pk], psk(5))
                    k.ts("dve", rZ[g % 2][:], PS(5), 1e-30, None, ALU.max, None, psk(5), [("rZ", g % 2)])

                def cmp_b(g):
                    pc = PcT[g % 2]
                    pk = ("pc", g % 2)
                    rz = rZ[g % 2]
                    rk = ("rZ", g % 2)
                    k.recip(rz[:], rz[:], [rk], [rk])
                    k.tt("dve", pn[0:127, :], pc[0:127, :], rz[0:127, :], ALU.mult, [pk, rk], ["pn"])
                    k.mm(PS(6)[0:32, :], Aagg[0:127, :], pn[0:127, :], g == 0, g == 3, ["Aagg", "pn"], psk(6))
                    for qtl in range(4):
                        qt = qc * 4 + qtl
                        obk = 7
                        k.mm(PS(obk)[:, 0:129], pc[0:127, qtl * 128:(qtl + 1) * 128], vca[0:127, 0:129], True, True, [pk, "vca"], psk(obk))
                        combine(obk, qt, qtl, g, 0, ap, ("acc", par, qtl), True, True)

                cmp_a(0)
                yield
                for g in range(4):
                    if g + 1 < 4:
                        cmp_a(g + 1)
                        yield
                    cmp_b(g)
                    yield
                k.cp("act", impS[0:32, :], PS(6)[0:32, :], psk(6), ["impS"])
                yield
                for qtl in range(4):
                    k.tr(PS(5)[:, qtl * 32:(qtl + 1) * 32], impS[0:32, qtl * 128:(qtl + 1) * 128], ident_f[0:32, 0:32],
                         ["impS", "identf"], psk(5))
                sc3 = sc[:].rearrange("p (a b) -> p a b", b=32)
                k.tt("dve", sc3, PS(5)[:, 0:128].rearrange("p (a b) -> p a b", b=32), AM[:, qc * 4:(qc + 1) * 4, :], ALU.mult,
                     psk(5) + ["AM"], ["sc"])
                k.tt("dve", sc3, sc3, ADDM[:, qc * 4:(qc + 1) * 4, :], ALU.add, ["sc", "ADDM"], ["sc"])
                yield
                for qtl in range(4):
                    ssl = sc[:, qtl * 32:(qtl + 1) * 32]
                    k.max8(m8[:, 0:8], ssl, ["sc"], ["m8"])
                    k.matchrep(wk32[:], m8[:, 0:8], ssl, -3e38, ["sc", "m8"], ["wk32"])
                    k.max8(m8[:, 8:16], wk32[:], ["wk32"], ["m8"])
                    k.ts("dve", selb[:, qtl * 32:(qtl + 1) * 32], ssl, m8[:, 15:16], -NEG, ALU.is_ge, ALU.mult, ["sc", "m8"], ["selb"])
                    yield
                k.ts("dve", selb[:], selb[:], NEG, None, ALU.add, None, ["selb"], ["selb"])
                for qtl in range(4):
                    k.tr(PS(6)[0:32, qtl * 128:(qtl + 1) * 128], selb[:, qtl * 32:(qtl + 1) * 32], ident_f[:], ["selb", "identf"], psk(6))
                k.cp("act", biasT[0:32, qsl], PS(6)[0:32, :], psk(6), [("biasT", qc)])
                yield
            def slcwin(qc, other):
                par = qc % 2
                ap = accp[par]
                branches = []
                for qtl in range(4):
                    qt = qc * 4 + qtl
                    for g in range(4):
                        ktl = [(kt, "caus" if kt == qt else None) for kt in range(qt + 1)]
                        branches.append(dict(kT=ksr, kkey="ksr", va=vsa, vkey="vsa", g=g, qt=qt, qtl=qtl, ktl=ktl, sel=True, br=1, last=False))
                        ktl = [(kt, "caus" if kt == qt else ("winb" if kt == qt - 4 else None)) for kt in range(max(0, qt - 4), qt + 1)]
                        branches.append(dict(kT=kwr, kkey="kwr", va=vwa, vkey="vwa", g=g, qt=qt, qtl=qtl, ktl=ktl, sel=False, br=2, last=(g == 3)))
                groups = []
                for bi, B in enumerate(branches):
                    nk = len(B["ktl"])
                    for g0 in range(0, nk, 4):
                        groups.append(dict(b=bi, kts=B["ktl"][g0:g0 + 4], first=(g0 == 0), lastg=(g0 + 4 >= nk), base=g0))

                def emit_scores(G_):
                    B = branches[G_["b"]]
                    sbk = (0, 1, 4)[sb_i[0] % 3]
                    sb_i[0] += 1
                    G_["sbk"] = sbk
                    qt, g = B["qt"], B["g"]
                    for j, (kt, eb) in enumerate(G_["kts"]):
                        out = PS(sbk)[:, j * 128:(j + 1) * 128]
                        nmm = 1 + (1 if B["sel"] else 0) + (1 if eb else 0)
                        k.mm(out, B["kT"][:, kt * 128:(kt + 1) * 128], qr[g][:, row(qt)], True, nmm == 1, [B["kkey"], ("qr", g)], psk(sbk))
                        i = 1
                        if B["sel"]:
                            i += 1
                            k.mm(out, ESEL[0:32, kt, :], biasT[0:32, row(qt)], False, i == nmm, ["ESEL", ("biasT", qc)], psk(sbk))
                        if eb:
                            k.mm(out, ident_b[:], (CAUS if eb == "caus" else WINB)[:], False, True, ["identb", "CAUS", "WINB"], psk(sbk))

                def emit_exp_pv(G_):
                    B = branches[G_["b"]]
                    sbk = G_["sbk"]
                    if G_["first"]:
                        B["obk"] = (2, 3)[ob_i[0] % 2]
                        ob_i[0] += 1
                    obk = B["obk"]
                    n = len(G_["kts"]) * 128
                    pi = pt_i[0] % 3
                    pt_i[0] += 1
                    k.act(PT[pi][:, 0:n], PS(sbk)[:, 0:n], AF.Exp, psk(sbk), [("PT", pi)], scale=SCALE)
                    nk = len(B["ktl"])
                    for j, (kt, eb) in enumerate(G_["kts"]):
                        idx = G_["base"] + j
                        k.mm(PS(obk)[:, 0:129], PT[pi][:, j * 128:(j + 1) * 128], B["va"][:, kt, :], idx == 0, idx == nk - 1,
                             [("PT", pi), B["vkey"]], psk(obk))
                    if G_["lastg"]:
                        akey = ("acc", par, B["qtl"])
                        combine(obk, B["qt"], B["qtl"], B["g"], B["br"], ap, akey, False, False)
                        if B["last"]:
                            k.dma(SOB[row(B["qt"]), hk * 512:(hk + 1) * 512], ap[:, B["qtl"]].rearrange("p g d -> p (g d)"), [akey],
                                  [("sob", hk, B["qt"])])

                emit_scores(groups[0])
                if len(groups) > 1:
                    emit_scores(groups[1])
                for gi_ in range(len(groups)):
                    if gi_ + 2 < len(groups):
                        emit_scores(groups[gi_ + 2])
                    emit_exp_pv(groups[gi_])
                    if other is not None:
                        next(other, None)

            def drain(gen):
                for _ in gen:
                    pass

            drain(cmpsel_gen(0))
            for qc in range(4):
                nxt = cmpsel_gen(qc + 1) if qc + 1 < 4 else None
                slcwin(qc, nxt)
                if nxt is not None:
                    drain(nxt)
        k.release()
        k.barrier()

    if stop_after >= 4:
        k.mark()
        wo = k.sb([128, 16, D], BF16, "wo")
        k.mark()
        stg4 = k.sb([128, 16, 512], F32, "stg4")
        for c in range(4):
            k.dma(stg4[:], w_out[:, c * 512:(c + 1) * 512].rearrange("(kc p) n -> p kc n", p=128), [], ["stg4"])
            k.cp("dve", wo[:, 0:8, c * 512:(c + 1) * 512], stg4[:, 0:8, :], ["stg4"], ["wo"])
            k.cp("act", wo[:, 8:16, c * 512:(c + 1) * 512], stg4[:, 8:16, :], ["stg4"], ["wo"])
        k.release()
        obt = [k.sb([128, D], F32, "obt") for _ in range(2)]
        mat = [k.sb([128, D], F32, "mat") for _ in range(2)]
        gbt = [k.sb([128, D], F32, "gbt") for _ in range(2)]
        xtt = [k.sb([128, D], F32, "xtt") for _ in range(2)]
        mb = k.sb([128, D], BF16, "mb")
        mT = k.sb([128, 16, 128], BF16, "mT")
        x1o = [k.sb([128, D], F32, "x1o") for _ in range(2)]

        def load4(t):
            p = t % 2
            k.dma(obt[p][:], SOB[row(t), :], [], [("obt", p)])
            k.dma(mat[p][:], SMA[row(t), :], [], [("mat", p)])
            k.dma(gbt[p][:], SMG[row(t), D:2 * D], [], [("gbt", p)])
            k.dma(xtt[p][:], x[row(t), :], [], [("xtt", p)])

        load4(0)
        for t in range(NT):
            p = t % 2
            if t + 1 < NT:
                load4(t + 1)
            k.act(gbt[p][:], gbt[p][:], AF.Tanh, [("gbt", p)], [("gbt", p)], scale=0.5)
            k.ts("dve", gbt[p][:], gbt[p][:], 0.5, 0.5, ALU.mult, ALU.add, [("gbt", p)], [("gbt", p)])
            k.tt("dve", obt[p][:], obt[p][:], gbt[p][:], ALU.mult, [("obt", p), ("gbt", p)], [("obt", p)])
            k.tt("dve", mb[:], obt[p][:], mat[p][:], ALU.add, [("obt", p), ("mat", p)], ["mb"])
            for kc in range(16):
                b = kc // 8
                k.tr(PSB(b)[:, (kc % 8) * 128:(kc % 8 + 1) * 128], mb[:, kc * 128:(kc + 1) * 128], ident_b[:], ["mb", "identb"], psk(b))
            for hh in range(2):
                k.cp("dve" if hh == 0 else "act", mT[:, hh * 8:(hh + 1) * 8, :], PSB(hh).rearrange("p (a b) -> p a b", b=128),
                     psk(hh), ["mT"])
            for c in range(4):
                b = 2 + c
                for kc in range(16):
                    k.mm(PS(b), mT[:, kc, :], wo[:, kc, c * 512:(c + 1) * 512], kc == 0, kc == 15, ["mT", "wo"], psk(b))
                k.tt("dve", x1o[p][:, c * 512:(c + 1) * 512], PS(b), xtt[p][:, c * 512:(c + 1) * 512], ALU.add,
                     psk(b) + [("xtt", p)], [("x1o", p)])
            k.dma(SX1[row(t), :], x1o[p][:], [("x1o", p)], [("sx1", t)])
        k.release()
        k.barrier()

    if stop_after >= 5:
        k.mark()
        eidT_all = k.sb([128, 16, 128], I32, "eidTall")
        gateT_all = k.sb([128, 16, 128], F32, "gateTall")
        iota16 = k.sb([128, 16], F32, "iota16")
        k.dma(iota16[:], cin["c_iota16"], [], ["iota16"])
        k.mark()
        wq = k.sb([128, 16, D], BF16, "wq")
        k.mark()
        stg5 = k.sb([128, 16, 512], F32, "stg5")
        for c in range(4):
            k.dma(stg5[:], peer_w_q[:, c * 512:(c + 1) * 512].rearrange("(kc p) n -> p kc n", p=128), [], ["stg5"])
            k.cp("dve", wq[:, 0:8, c * 512:(c + 1) * 512], stg5[:, 0:8, :], ["stg5"], ["wq"])
            k.cp("act", wq[:, 8:16, c * 512:(c + 1) * 512], stg5[:, 8:16, :], ["stg5"], ["wq"])
        k.release()
        kTf = [k.sb([128, 128], F32, "kTf") for _ in range(2)]
        g2b = k.sb([128, D], F32, "g2b")
        x1a = [k.sb([128, D], F32, "x1a") for _ in range(2)]
        h2a = [k.sb([128, D], BF16, "h2a") for _ in range(2)]
        jk = k.sb([128, D], BF16, "jk")
        h2T = k.sb([128, 16, 128], BF16, "h2T")
        qTs = k.sb([128, 16, 128], F32, "qTs")
        Sxa = [k.sb([128, 16, 128], F32, "Sxa") for _ in range(2)]
        sm5 = [k.sb([128, 4], F32, "sm5") for _ in range(2)]
        cand = k.sb([128, 8, 16, 16], F32, "cand"); eqt = k.sb([128, 8, 16, 16], F32, "eqt")
        V16 = k.sb([128, 16, 16], F32, "V16"); I16u = k.sb([128, 16, 16], U32, "I16u"); I16f = k.sb([128, 16, 16], F32, "I16f")
        wk = k.sb([128, 128], F32, "wk"); wk2 = k.sb([128, 256], F32, "wk2")
        T16 = k.sb([128, 8, 16], F32, "T16"); P16u = k.sb([128, 8, 16], U32, "P16u")
        A16u = k.sb([128, 8, 16], U32, "A16u"); B16u = k.sb([128, 8, 16], U32, "B16u")
        af = k.sb([128, 8, 16], F32, "af"); bf = k.sb([128, 8, 16], F32, "bf")
        ex = k.sb([128, 8, 16], F32, "ex"); gate = k.sb([128, 8, 16], F32, "gate")
        zs = k.sb([128, 8], F32, "zs")
        i1f = k.sb([128, 8, 16], F32, "i1f"); i2f = k.sb([128, 8, 16], F32, "i2f"); eid = k.sb([128, 8, 16], F32, "eid")
        io_b = iota16[:].unsqueeze(1).unsqueeze(1).to_broadcast([128, 8, 16, 16])
        V16v = V16[:].rearrange("p (h two) a -> p h two a", two=2)
        I16v = I16f[:].rearrange("p (h two) a -> p h two a", two=2)
        k.dma(kTf[0][:], keysT[0], [], ["kTf"]); k.dma(kTf[1][:], keysT[1], [], ["kTf"])
        k.dma(g2b[:], norm_ffn_g.partition_broadcast(128), [], ["g2b"])
        def front(t):
            p = t % 2
            kp = "n5%d" % p
            k.dma(x1a[p][:], SX1[row(t), :], [], [("x1a", p)])
            k.act(jk[:], x1a[p][:], AF.Square, [("x1a", p)], ["jk", kp + "ss"], accum=sm5[p][:, 0:1])
            k.rstd_from_ss(sm5[p][:, 0:1], sm5[p][:, 2:3], sm5[p][:, 1:2], nh[:], D, kp)
            k.stt(h2a[p][:], x1a[p][:], sm5[p][:, 2:3], g2b[:], ALU.mult, ALU.mult, [("x1a", p), kp + "rstd", "g2b"], [("h2a", p)])
            k.dma(SH2[row(t), :], h2a[p][:], [("h2a", p)], [("sh2", t)])
            for kc in range(16):
                b = kc // 8
                k.tr(PSB(b)[:, (kc % 8) * 128:(kc % 8 + 1) * 128], h2a[p][:, kc * 128:(kc + 1) * 128], ident_b[:], [("h2a", p), "identb"], psk(b))
            for hh in range(2):
                k.cp("dve" if hh == 0 else "act", h2T[:, hh * 8:(hh + 1) * 8, :], PSB(hh).rearrange("p (a b) -> p a b", b=128),
                     psk(hh), ["h2T"])
            for cb in range(16):
                b = 4 + cb // 4
                for kc in range(16):
                    k.mm(PS(b)[:, (cb % 4) * 128:(cb % 4 + 1) * 128], wq[:, kc, cb * 128:(cb + 1) * 128], h2T[:, kc, :], kc == 0, kc == 15,
                         ["wq", "h2T"], psk(b))
            k.cp("act", qTs[:].rearrange("p a b -> p (a b)"), PS(4, 4), psk(4, 4), ["qTs"])
            for cb in range(16):
                b = 4 + cb // 4
                k.mm(PS(b)[:, (cb % 4) * 128:(cb % 4 + 1) * 128], qTs[:, cb, :], kTf[cb % 2][:], True, True, ["qTs", "kTf"], psk(b))
            k.cp("act", Sxa[p][:].rearrange("p a b -> p (a b)"), PS(4, 4), psk(4, 4), [("Sxa", p)])

        def topk(t):
            eT, gT = eidT_all[:, t, :], gateT_all[:, t, :]
            ek, gk = ("eidT", t), ("gateT", t)
            Sx = Sxa[t % 2]
            sxk = ("Sxa", t % 2)
            for hb_ in range(16):
                k.max8(V16[:, hb_, 0:8], Sx[:, hb_, :], [sxk], ["V16"])
                k.maxidx(I16u[:, hb_, 0:8], V16[:, hb_, 0:8], Sx[:, hb_, :], [sxk, "V16"], ["I16u"])
                k.matchrep(wk[:], V16[:, hb_, 0:8], Sx[:, hb_, :], -3e38, [sxk, "V16"], ["wk"])
                k.max8(V16[:, hb_, 8:16], wk[:], ["wk"], ["V16"])
                k.maxidx(I16u[:, hb_, 8:16], V16[:, hb_, 8:16], wk[:], ["wk", "V16"], ["I16u"])
            k.cp("dve", I16f[:], I16u[:], ["I16u"], ["I16f"])
            k.tt("dve", cand[:], V16v[:, :, 0, :].unsqueeze(3).to_broadcast([128, 8, 16, 16]),
                 V16v[:, :, 1, :].unsqueeze(2).to_broadcast([128, 8, 16, 16]), ALU.add, ["V16"], ["cand"])
            for h in range(8):
                ch = cand[:, h].rearrange("p a b -> p (a b)")
                k.max8(T16[:, h, 0:8], ch, ["cand"], ["T16"])
                k.maxidx(P16u[:, h, 0:8], T16[:, h, 0:8], ch, ["cand", "T16"], ["P16u"])
                k.matchrep(wk2[:], T16[:, h, 0:8], ch, -3e38, ["cand", "T16"], ["wk2"])
                k.max8(T16[:, h, 8:16], wk2[:], ["wk2"], ["T16"])
                k.maxidx(P16u[:, h, 8:16], T16[:, h, 8:16], wk2[:], ["wk2", "T16"], ["P16u"])
            k.op("dve", lambda e: e.tensor_single_scalar(out=A16u[:], in_=P16u[:], scalar=4, op=ALU.logical_shift_right), ["P16u"], ["A16u"])
            k.op("dve", lambda e: e.tensor_single_scalar(out=B16u[:], in_=P16u[:], scalar=15, op=ALU.bitwise_and), ["P16u"], ["B16u"])
            k.cp("dve", af[:], A16u[:], ["A16u"], ["af"])
            k.cp("dve", bf[:], B16u[:], ["B16u"], ["bf"])
            for (pf, col, dst, dk) in ((af, 0, i1f, "i1f"), (bf, 1, i2f, "i2f")):
                k.tt("dve", eqt[:], io_b, pf[:].unsqueeze(3).to_broadcast([128, 8, 16, 16]), ALU.is_equal, ["iota16", "af", "bf"], ["eqt"])
                k.tt("dve", eqt[:], eqt[:], I16v[:, :, col, :].unsqueeze(2).to_broadcast([128, 8, 16, 16]), ALU.mult, ["eqt", "I16f"], ["eqt"])
                k.rsum(dst[:], eqt[:], ["eqt"], [dk])
            k.stt(eid[:], i1f[:], 128.0, i2f[:], ALU.mult, ALU.add, ["i1f", "i2f"], ["eid"])
            k.ts("dve", eid[:], eid[:], 0.0, 16383.0, ALU.max, ALU.min, ["eid"], ["eid"])
            k.tr(PS(2)[:, 0:128], eid[:].rearrange("p a b -> p (a b)"), ident_f[:], ["eid", "identf"], psk(2))
            k.cp("dve", eT, PS(2)[:, 0:128], psk(2), [ek])
            k.tt("dve", ex[:], T16[:], T16[:, :, 0:1].to_broadcast([128, 8, 16]), ALU.subtract, ["T16"], ["ex"])
            k.act(ex[:], ex[:], AF.Exp, ["ex"], ["ex"])
            k.rsum(zs[:], ex[:], ["ex"], ["zs"])
            k.recip(zs[:], zs[:], ["zs"], ["zs"])
            k.tt("dve", gate[:], ex[:], zs[:].unsqueeze(2).to_broadcast([128, 8, 16]), ALU.mult, ["ex", "zs"], ["gate"])
            k.tr(PS(2)[:, 128:256], gate[:].rearrange("p a b -> p (a b)"), ident_f[:], ["gate", "identf"], psk(2))
            k.cp("dve", gT, PS(2)[:, 128:256], psk(2), [gk])


        front(0)
        for t in range(NT):
            if t + 1 < NT:
                front(t + 1)
            topk(t)
        k.release()
        k.barrier()

    if stop_after >= 5:
        k.mark()
        gfb = k.sb([128, D], F32, "gfb")
        SELR = k.sb([128, 128, 128], BF16, "SELR")
        IDm = k.sb([128, 128, 128], BF16, "IDm")
        WSEL = k.sb([128, 128, 128], BF16, "WSEL")
        Ubuf = [k.sb([128, D], F32, "Ubuf") for _ in range(2)]
        Vbuf = [k.sb([128, D], F32, "Vbuf") for _ in range(2)]
        Vbf = [k.sb([128, D], BF16, "Vbf") for _ in range(2)]
        x1t = k.sb([128, D], F32, "x1t")
        yo = k.sb([128, D], F32, "yo")
        h2b = k.sb([128, D], BF16, "h2b")
        jk = k.sb([128, 1024], BF16, "jk")
        A0 = k.sb([128, 128], F32, "A0"); A1 = k.sb([128, 128], F32, "A1")
        At = k.sb([128, 128], F32, "At"); Wt = k.sb([128, 128], F32, "Wt")
        g1 = k.sb([128, 128], F32, "g1"); g2 = k.sb([128, 128], F32, "g2")
        sm6 = k.sb([128, 8], F32, "sm6")

        k.dma(gfb[:], norm_final_g.partition_broadcast(128), [], ["gfb"])
        k.cp("dve", SELR[:], ident_f[:].unsqueeze(2).to_broadcast([128, 128, 128]), ["identf"], ["SELR"])
        idflat = cin["c_ident"].rearrange("a b -> (a b)")
        for c in range(8):
            k.dma(Ubuf[c % 2][:, :], idflat[c * 2048:(c + 1) * 2048].partition_broadcast(128), [], [("G", c % 2)])
            k.cp("dve", IDm[:, c * 16:(c + 1) * 16, :].rearrange("p a b -> p (a b)"), Ubuf[c % 2][:, :], [("G", c % 2)], ["IDm"])
        k.memset("dve", A0[:], 0.0, ["A0", "setup5b"])
        k.memset("dve", A1[:], 0.0, ["A1", "setup5b"])
        gi = [0]

        G = Ubuf + Vbuf + [k.sb([128, D], F32, "Gx") for _ in range(2)]
        NG_ = len(G)
        _unused = None

        def stage1(t):
            eT, ek = eidT_all[:, t, :], ("eidT", t)
            for tk in range(128):
                i = gi[0] % NG_
                gi[0] += 1
                k.op("pool", (lambda e, i=i, tk=tk: e.indirect_dma_start(
                    out=G[i][:, :], out_offset=None, in_=peer_u,
                    in_offset=bass.IndirectOffsetOnAxis(ap=eT[:, tk:tk + 1], axis=0))), [ek, ("gateT", t), "yo", "IDm", "SELR", "setup5b"], [("G", i)], dma=True)
                for c in range(4):
                    k.mm(PS(4 + c), SELR[:, tk, :], h2b[:, c * 512:(c + 1) * 512], True, True, ["SELR", "h2b"], psk(4 + c))
                for hf, Ax in ((0, A0), (1, A1)):
                    k.stt(jk[:, 0:1024], G[i][:, hf * 1024:(hf + 1) * 1024], 1.0, PS(4 + 2 * hf, 2), ALU.mult, ALU.mult,
                          [("G", i)] + psk(4 + 2 * hf, 2), ["jk", "A%d" % hf], accum=Ax[:, tk:tk + 1])

        def post1(t):
            gk = ("gateT", t)
            k.tt("dve", At[:], A0[:], A1[:], ALU.add, ["A0", "A1"], ["At"])
            k.act(g1[:], At[:], AF.Square, ["At"], ["g1"])
            k.ts("dve", g1[:], g1[:], 0.044715, 1.0, ALU.mult, ALU.add, ["g1"], ["g1"])
            k.tt("dve", g1[:], g1[:], At[:], ALU.mult, ["g1", "At"], ["g1"])
            k.act(g2[:], g1[:], AF.Tanh, ["g1"], ["g2"], scale=GC)
            k.ts("dve", g2[:], g2[:], 0.5, 0.5, ALU.mult, ALU.add, ["g2"], ["g2"])
            k.tt("dve", g2[:], g2[:], At[:], ALU.mult, ["g2", "At"], ["g2"])
            k.tt("dve", Wt[:], g2[:], gateT_all[:, t, :], ALU.mult, ["g2", gk], ["Wt"])
            k.tt("dve", WSEL[:], IDm[:], Wt[:].unsqueeze(2).to_broadcast([128, 128, 128]), ALU.mult, ["IDm", "Wt"], ["WSEL"])

        def stage2(t):
            eT, ek = eidT_all[:, t, :], ("eidT", t)
            for tk in range(128):
                i = gi[0] % NG_
                gi[0] += 1
                k.op("pool", (lambda e, i=i, tk=tk: e.indirect_dma_start(
                    out=G[i][:, :], out_offset=None, in_=peer_v,
                    in_offset=bass.IndirectOffsetOnAxis(ap=eT[:, tk:tk + 1], axis=0))), [ek, "WSEL"], [("G", i)], dma=True)
                j = tk % 2
                k.cp("act", Vbf[j][:], G[i][:], [("G", i)], [("Vbf", j)])
                for c in range(4):
                    k.mm(PS(c), WSEL[:, tk, :], Vbf[j][:, c * 512:(c + 1) * 512], tk == 0, tk == 127, ["WSEL", ("Vbf", j)], psk(c))

        def final(t):
            k.dma(x1t[:], SX1[row(t), :], [], ["x1t"])
            k.tt("dve", x1t[:], PS(0, 4), x1t[:], ALU.add, psk(0, 4) + ["x1t"], ["x1t"])
            k.act(yo[:], x1t[:], AF.Square, ["x1t"], ["yo", "n6ss"], accum=sm6[:, 4:5])
            k.rstd_from_ss(sm6[:, 4:5], sm6[:, 6:7], sm6[:, 5:6], nh[:], D, "n6")
            k.stt(yo[:], x1t[:], sm6[:, 6:7], gfb[:], ALU.mult, ALU.mult, ["x1t", "n6rstd", "gfb"], ["yo"])
            k.dma(y[row(t), :], yo[:], ["yo"], [("y", t)])

        for t in range(NT):
            k.dma(h2b[:], SH2[row(t), :], [], ["h2b"])
            stage1(t)
            post1(t)
            stage2(t)
            final(t)
        k.release()
        k.release()

    k.emit()
    return nc, k


_CACHE = {}


def make_in_maps(inputs):
    f = lambda a: np.ascontiguousarray(np.asarray(a, dtype=np.float32))
    consts = make_consts()
    shared = {
        "norm_mix_g": f(inputs["norm_mix_g"][0:1]),
        "w_in": f(inputs["w_in"][0]),
        "w_out": f(inputs["w_out"][0]),
        "gm_ln_g": f(inputs["gm_ln_g"][0:1]),
        "gm_ln_b": f(inputs["gm_ln_b"][0:1]),
        "gm_wT": f(np.transpose(np.asarray(inputs["gm_spatial_w"][0]), (2, 0, 1))),
        "gm_bT": f(np.transpose(np.asarray(inputs["gm_spatial_b"][0]), (1, 0))),
        "cmp_posT_k": f(np.asarray(inputs["cmp_pos_k"][0]).T),
        "cmp_posT_v": f(np.asarray(inputs["cmp_pos_v"][0]).T),
        "cmp_w1_k": f(inputs["cmp_w1_k"][0]), "cmp_w1_v": f(inputs["cmp_w1_v"][0]),
        "cmp_w2_k": f(inputs["cmp_w2_k"][0]), "cmp_w2_v": f(inputs["cmp_w2_v"][0]),
        "norm_ffn_g": f(inputs["norm_ffn_g"][0:1]),
        "peer_w_q": f(inputs["peer_w_q"][0]),
        "keys1T": f(np.asarray(inputs["peer_keys1"][0]).T),
        "keys2T": f(np.asarray(inputs["peer_keys2"][0]).T),
        "peer_u": f(inputs["peer_u"][0]),
        "peer_v": f(inputs["peer_v"][0]),
        "norm_final_g": f(np.asarray(inputs["norm_final_g"]).reshape(1, D)),
    }
    shared.update(consts)
    xs = np.asarray(inputs["x"], dtype=np.float32)
    maps = []
    for b in range(8):
        m = dict(shared)
        m["x"] = np.ascontiguousarray(xs[b])
        maps.append(m)
    return maps


def kernel(**inputs):
    if "nc" not in _CACHE:
        _CACHE["nc"] = build_program()[0]
    nc = _CACHE["nc"]
    in_maps = make_in_maps(inputs)
    res = run_bass_kernel_spmd(nc, in_maps, core_ids=list(range(8)))
    out = np.stack([np.asarray(r["y"]) for r in res.results], axis=0)
    return out.astype(np.float32)
```

```python
import numpy as np
import concourse.bass as bass
import concourse.mybir as mybir
from concourse.bass_utils import run_bass_kernel_spmd

F32 = mybir.dt.float32
BF16 = mybir.dt.bfloat16
I32 = mybir.dt.int32
U32 = mybir.dt.uint32
ALU = mybir.AluOpType
AF = mybir.ActivationFunctionType
AX = mybir.AxisListType

ENGS = ("pe", "act", "dve", "pool", "sp")
EPOCH = 30000
NDMASEM = 8
SB_BASE = 16512
SB_TOP = 229344


class Op:
    __slots__ = ("eng", "fn", "deps", "is_dma", "idx", "sig", "semref")

    def __init__(self, eng, fn, is_dma):
        self.eng = eng
        self.fn = fn
        self.deps = []
        self.is_dma = is_dma
        self.sig = False
        self.semref = None


class Prog:
    def __init__(self, nc):
        self.nc = nc
        self.ops = []
        self.last_w = {}
        self.readers = {}
        self.last_on_eng = {e: None for e in ENGS}
        self.barrier_deps = []
        self.need_barrier = {e: False for e in ENGS}
        self.sb_off = SB_BASE
        self.marks = []
        self.names = 0
        self.recent_dma = {}

    def sb(self, shape, dtype, name=None):
        self.names += 1
        name = (name or "t") + "_%d" % self.names
        esz = {F32: 4, BF16: 2, I32: 4, U32: 4}[dtype]
        n = 1
        for s in shape[1:]:
            n *= s
        nbytes = (n * esz + 31) // 32 * 32
        off = self.sb_off
        self.sb_off += nbytes
        assert self.sb_off <= SB_TOP, ("SBUF overflow", name, self.sb_off)
        return self.nc.alloc_sbuf_tensor_at(name, list(shape), dtype, offset=off)

    def mark(self):
        self.marks.append(self.sb_off)

    def release(self):
        self.sb_off = self.marks.pop()
        self.barrier()

    def barrier(self):
        self.barrier_deps = [self.last_on_eng[e] for e in ENGS if self.last_on_eng[e] is not None]
        for q in self.recent_dma.values():
            self.barrier_deps.extend(q)
        for e in ENGS:
            self.need_barrier[e] = True

    def op(self, eng, fn, reads=(), writes=(), dma=False):
        o = Op(eng, fn, dma or eng == "sp")
        o.idx = len(self.ops)
        deps = set()
        if self.need_barrier[eng]:
            deps.update(self.barrier_deps)
            self.need_barrier[eng] = False
        reads = list(reads)
        writes = list(writes)
        for k in list(reads):
            if isinstance(k, str) and k.startswith("ps"):
                reads.remove(k)
                if k not in writes:
                    writes.append(k)
        for k in reads:
            w = self.last_w.get(k)
            if w is not None:
                deps.add(w)
        for k in writes:
            w = self.last_w.get(k)
            if w is not None:
                deps.add(w)
            rd = self.readers.get(k)
            if rd:
                for v in rd.values():
                    if isinstance(v, list):
                        deps.update(v)
                    else:
                        deps.add(v)
        for k in reads:
            rd = self.readers.setdefault(k, {})
            if o.is_dma:
                rd.setdefault("dma_" + eng, []).append(o.idx)
            else:
                rd[eng] = o.idx
        for k in writes:
            self.last_w[k] = o.idx
            self.readers[k] = {}
        deps.discard(o.idx)
        o.deps = sorted(deps)
        self.ops.append(o)
        self.last_on_eng[eng] = o.idx
        if o.is_dma:
            q = self.recent_dma.setdefault(eng, [])
            q.append(o.idx)
            if len(q) > NDMASEM:
                q.pop(0)
        return o

    def emit(self):
        nc = self.nc
        ops = self.ops
        for o in ops:
            latest = {}
            keep = []
            for d in o.deps:
                p = ops[d]
                if p.is_dma:
                    keep.append(d)
                elif p.eng == "pe" and o.eng == "pe":
                    continue
                else:
                    if d > latest.get(p.eng, -1):
                        latest[p.eng] = d
            keep.extend(latest.values())
            o.deps = sorted(keep)
            for d in o.deps:
                ops[d].sig = True
        sems = {}

        def getsem(key):
            if key not in sems:
                sems[key] = nc.alloc_semaphore("s_%s_%s" % key)
            return sems[key]

        cnt = {e: 0 for e in ENGS}
        dcnt = {}
        dper = {}
        prev_on_dsem = {}
        for o in ops:
            if o.is_dma:
                q = o.eng
                i = dcnt.get(q, 0)
                dcnt[q] = i + 1
                j = i % NDMASEM
                key = ("d" + q, j)
                c = dper.get(key, 0) + 1
                dper[key] = c
                o.semref = (key, 16 * c)
                if c > 1:
                    o.deps = sorted(set(o.deps) | {prev_on_dsem[key]})
                prev_on_dsem[key] = o.idx
                o.sig = True
            elif o.sig:
                kk = cnt[o.eng]
                cnt[o.eng] = kk + 1
                o.semref = ((o.eng, kk // EPOCH), (kk % EPOCH) + 1)
        for o in ops:
            if o.semref is not None:
                getsem(o.semref[0])
        self.stats = dict(nops=len(ops), nsig=sum(1 for o in ops if o.sig), cnt=dict(cnt), dcnt=dict(dcnt),
                          nsem=len(sems))
        per_eng = {e: [o for o in ops if o.eng == e] for e in ENGS}
        final_dma = {key: 16 * c for key, c in dper.items()}

        def run(engname, eng):
            waited = {}
            for o in per_eng[engname]:
                for d in o.deps:
                    p = ops[d]
                    if p.semref is None:
                        continue
                    if p.eng == "pe" and o.eng == "pe" and not p.is_dma:
                        continue
                    key, val = p.semref
                    if waited.get(key, 0) >= val:
                        continue
                    waited[key] = val
                    eng.wait_ge(sems[key], val)
                inst = o.fn(eng)
                if o.semref is not None:
                    key, val = o.semref
                    inst.then_inc(sems[key], 16 if o.is_dma else 1)
            if engname == "sp":
                for key, val in final_dma.items():
                    if waited.get(key, 0) < val:
                        eng.wait_ge(sems[key], val)

        with nc.Block() as block:
            @block.tensor
            def _(e):
                run("pe", e)

            @block.scalar
            def _(e):
                run("act", e)

            @block.vector
            def _(e):
                run("dve", e)

            @block.gpsimd
            def _(e):
                run("pool", e)

            @block.sync
            def _(e):
                run("sp", e)


class K(Prog):
    def mm(self, out, lhsT, rhs, start, stop, r, w):
        return self.op("pe", lambda e: e.matmul(out=out, lhsT=lhsT, rhs=rhs, start=start, stop=stop), r, w)

    def tr(self, out, in_, ident, r, w):
        return self.op("pe", lambda e: e.transpose(out=out, in_=in_, identity=ident), r, w)

    def act(self, out, in_, func, r, w, scale=1.0, bias=None, accum=None):
        def f(e):
            kw = dict(out=out, in_=in_, func=func, scale=scale)
            if bias is not None:
                kw["bias"] = bias
            if accum is not None:
                kw["accum_out"] = accum
            return e.activation(**kw)
        return self.op("act", f, r, w)

    def tt(self, eng, out, a, b, op, r, w):
        return self.op(eng, lambda e: e.tensor_tensor(out=out, in0=a, in1=b, op=op), r, w)

    def ts(self, eng, out, a, s1, s2, op0, op1, r, w):
        if s2 is None:
            return self.op(eng, lambda e: e.tensor_scalar(out=out, in0=a, scalar1=s1, scalar2=None, op0=op0), r, w)
        return self.op(eng, lambda e: e.tensor_scalar(out=out, in0=a, scalar1=s1, scalar2=s2, op0=op0, op1=op1), r, w)

    def stt(self, out, a, s, b, op0, op1, r, w, accum=None):
        def f(e):
            kw = dict(out=out, in0=a, scalar=s, in1=b, op0=op0, op1=op1)
            if accum is not None:
                kw["accum_out"] = accum
            return e.scalar_tensor_tensor(**kw)
        return self.op("dve", f, r, w)

    def cp(self, eng, out, in_, r, w):
        if eng == "act":
            return self.op("act", lambda e: e.copy(out=out, in_=in_), r, w)
        return self.op(eng, lambda e: e.tensor_copy(out=out, in_=in_), r, w)

    def recip(self, out, in_, r, w):
        return self.op("dve", lambda e: e.reciprocal(out=out, in_=in_), r, w)

    def rsum(self, out, in_, r, w):
        return self.op("dve", lambda e: e.reduce_sum(out=out, in_=in_, axis=AX.X), r, w)

    def max8(self, out, in_, r, w):
        return self.op("dve", lambda e: e.max(out=out, in_=in_), r, w)

    def maxidx(self, out, in_max, in_values, r, w):
        return self.op("dve", lambda e: e.max_index(out=out, in_max=in_max, in_values=in_values), r, w)

    def matchrep(self, out, rep, vals, imm, r, w):
        return self.op("dve", lambda e: e.match_replace(out=out, in_to_replace=rep, in_values=vals, imm_value=imm), r, w)

    def memset(self, eng, ap, val, w):
        return self.op(eng, lambda e: e.memset(ap, val), (), w)

    def dma(self, out, in_, r, w):
        return self.op("sp", lambda e: e.dma_start(out=out, in_=in_), r, w)

    def rstd_from_ss(self, ss, rstd, tmp, nh, n, key):
        self.ts("dve", tmp, ss, 1.0 / n, 1e-6, ALU.mult, ALU.add, [key + "ss"], [key + "ms"])
        self.tt("pool", rstd, tmp, nh, ALU.pow, [key + "ms", "nh"], [key + "rstd"])

S = 2048
D = 2048
NT = 16
GC = 0.7978845608028654
SCALE = 128 ** -0.5
NEG = -30000.0
C_U, C_V, C_Q, C_KV, C_NG, C_MG = 0, 2048, 4096, 6144, 9216, 9264

CONST_SHAPES = {
    "c_cos": [128, S], "c_sin": [128, S], "c_rt": [128, 128], "c_caus": [128, 128], "c_winb": [128, 128],
    "c_cmpb": [128, S], "c_aagg": [128, 32], "c_am": [128, 16, 32], "c_add": [128, 16, 32],
    "c_esel": [32, 16, 128], "c_tril": [128, 16, 128], "c_ident": [128, 128], "c_iota16": [128, 16],
    "c_zrow": [128, 255],
}


def make_consts():
    c = {}
    half = 64
    inv = 10000.0 ** (-np.arange(half, dtype=np.float32) / half)
    ang = np.arange(S, dtype=np.float32)[:, None] * inv[None, :]
    cos = np.cos(ang).astype(np.float32).T
    sin = np.sin(ang).astype(np.float32).T
    c["c_cos"] = np.concatenate([cos, cos], 0)
    c["c_sin"] = np.concatenate([sin, sin], 0)
    rt = np.zeros((128, 128), np.float32)
    for m in range(128):
        if m < 64:
            rt[m + 64, m] = -1.0
        else:
            rt[m - 64, m] = 1.0
    c["c_rt"] = rt
    kk = np.arange(128)[:, None]
    qq = np.arange(128)[None, :]
    c["c_caus"] = np.where(kk <= qq, 0.0, NEG).astype(np.float32)
    c["c_winb"] = np.where(kk > qq, 0.0, NEG).astype(np.float32)
    n = np.arange(128)[:, None]
    q = np.arange(S)[None, :]
    c["c_cmpb"] = np.where((16 * n + 31 <= q) & (n < 127), 0.0, NEG).astype(np.float32)
    agg = np.array([1, 2, 2, 2, 1], np.float32)
    a = np.zeros((128, 32), np.float32)
    for nn in range(127):
        for j in range(32):
            w = nn + 1 - 4 * j
            if 0 <= w <= 4:
                a[nn, j] = agg[w]
    c["c_aagg"] = a
    pos = (np.arange(16)[None, :, None] * 128 + np.arange(128)[:, None, None])
    j = np.arange(32)[None, None, :]
    allowed = (j * 64) <= pos
    forced = (j == 0) | (j == (pos // 64))
    c["c_am"] = (allowed & ~forced).astype(np.float32)
    c["c_add"] = np.where(forced, 1e30, np.where(allowed, 0.0, -1e30)).astype(np.float32)
    e = np.zeros((32, 16, 128), np.float32)
    for kt in range(16):
        for key in range(128):
            e[2 * kt + key // 64, kt, key] = 1.0
    c["c_esel"] = e
    s_ = np.arange(128)[:, None, None]
    t_ = np.arange(128)[None, None, :]
    c["c_tril"] = np.broadcast_to((s_ <= t_), (128, 16, 128)).astype(np.float32).copy()
    c["c_ident"] = np.eye(128, dtype=np.float32)
    c["c_iota16"] = np.broadcast_to(np.arange(16, dtype=np.float32)[None, :], (128, 16)).copy()
    z = np.zeros((128, 255), np.float32)
    z[:, 127] = 1.0
    c["c_zrow"] = z
    return c


def build_program(stop_after=99, dbg=()):
    nc = bass.Bass("TRN2", target_bir_lowering=False)
    k = K(nc)

    def din(name, shape):
        return nc.dram_tensor(name, list(shape), F32, kind="ExternalInput").ap()

    def dscr(name, shape, dt=F32):
        kind = "ExternalOutput" if name in dbg else "Internal"
        return nc.dram_tensor(name, list(shape), dt, kind=kind).ap()

    x = din("x", [S, D])
    norm_mix_g = din("norm_mix_g", [1, D])
    w_in = din("w_in", [D, 13360])
    w_out = din("w_out", [D, D])
    gm_ln_g = din("gm_ln_g", [1, D])
    gm_ln_b = din("gm_ln_b", [1, D])
    gm_wT = din("gm_wT", [128, 16, 128])
    gm_bT = din("gm_bT", [128, 16])
    posT = {"k": din("cmp_posT_k", [128, 32]), "v": din("cmp_posT_v", [128, 32])}
    cw1 = {"k": din("cmp_w1_k", [4096, 256]), "v": din("cmp_w1_v", [4096, 256])}
    cw2 = {"k": din("cmp_w2_k", [256, 128]), "v": din("cmp_w2_v", [256, 128])}
    norm_ffn_g = din("norm_ffn_g", [1, D])
    peer_w_q = din("peer_w_q", [D, D])
    keysT = [din("keys1T", [128, 128]), din("keys2T", [128, 128])]
    NEXP = 16384 if stop_after >= 5 else 128
    peer_u = din("peer_u", [NEXP, D])
    peer_v = din("peer_v", [NEXP, D])
    norm_final_g = din("norm_final_g", [1, D])
    cin = {n: din(n, s) for n, s in CONST_SHAPES.items()}
    y = nc.dram_tensor("y", [S, D], F32, kind="ExternalOutput").ap()

    SU = dscr("s_u", [S, D]); SV = dscr("s_v", [S, D]); SVS = dscr("s_vs", [S, 512]); SVW = dscr("s_vw", [S, 512])
    SNG = dscr("s_ng", [S, 48]); SMG = dscr("s_mg", [S, 4096])
    SQ = dscr("s_q", [16, 128, S]); SKC = dscr("s_kc", [4, 128, S]); SVC = dscr("s_vc", [4, 128, S])
    SKS = dscr("s_ks", [4, 128, S]); SKW = dscr("s_kw", [4, 128, S])
    SMA = dscr("s_ma", [S, D]); SOB = dscr("s_ob", [S, D]); SX1 = dscr("s_x1", [S, D])
    SH2 = dscr("s_h2", [S, D], BF16); SS = dscr("s_ss", [S, 2048])

    PSALL = nc.alloc_psum_tensor("psall", [128, 4096], F32)

    def PS(b, n=1):
        return PSALL[:, b * 512:(b + n) * 512]

    def PSB(b):
        return PSALL[:, b * 512:(b + 1) * 512].bitcast(BF16)

    def psk(b, n=1):
        return ["ps%d" % i for i in range(b, b + n)]

    ident_f = k.sb([128, 128], F32, "identf")
    ident_b = k.sb([128, 128], BF16, "identb")
    ones_b = k.sb([128, 128], BF16, "onesb")
    nh = k.sb([128, 1], F32, "nh")
    k.dma(ident_f[:], cin["c_ident"], [], ["identf"])
    k.cp("dve", ident_b[:], ident_f[:], ["identf"], ["identb"])
    k.memset("pool", ones_b[:], 1.0, ["onesb"])
    k.memset("pool", nh[:], -0.5, ["nh"])

    def row(t):
        return slice(t * 128, (t + 1) * 128)

    k.mark()
    hT = k.sb([128, 16, S], BF16, "hT")
    k.mark()
    gb = k.sb([128, D], F32, "gb")
    xt = [k.sb([128, D], F32, "xt") for _ in range(2)]
    junk = k.sb([128, D], BF16, "junk")
    hb = [k.sb([128, D], BF16, "hb") for _ in range(2)]
    sm = [k.sb([128, 4], F32, "sm") for _ in range(2)]
    k.dma(gb[:], norm_mix_g.partition_broadcast(128), [], ["gb"])
    for t in range(NT):
        p = t % 2
        kp = "a%d" % p
        k.dma(xt[p][:], x[row(t), :], [], [kp + "xt"])
        k.act(junk[:], xt[p][:], AF.Square, [kp + "xt"], ["junk", kp + "ss"], accum=sm[p][:, 0:1])
        k.rstd_from_ss(sm[p][:, 0:1], sm[p][:, 2:3], sm[p][:, 1:2], nh[:], D, kp)
        k.stt(hb[p][:], xt[p][:], sm[p][:, 2:3], gb[:], ALU.mult, ALU.mult, [kp + "xt", kp + "rstd", "gb"], [kp + "hb"])
        for kc in range(16):
            b = 2 * p + kc // 8
            k.tr(PSB(b)[:, (kc % 8) * 128:(kc % 8 + 1) * 128], hb[p][:, kc * 128:(kc + 1) * 128], ident_b[:],
                 [kp + "hb", "identb"], psk(b))
        for hh in range(2):
            b = 2 * p + hh
            k.cp("dve" if hh == 0 else "act", hT[:, hh * 8:(hh + 1) * 8, row(t)],
                 PSB(b).rearrange("p (a b) -> p a b", b=128), psk(b), [("hT", t)])
    k.release()

    if stop_after >= 1:
        wf = [k.sb([128, 16, 512], F32, "wf") for _ in range(2)]
        wb = [k.sb([128, 16, 512], BF16, "wb") for _ in range(2)]
        ob = [k.sb([128, 512], F32, "ob") for _ in range(4)]
        chunks = []
        for c in range(4):
            chunks.append((C_U + c * 512, 512, "tm", (SU, c * 512)))
        for c in range(4):
            chunks.append((C_V + c * 512, 512, "tm", (SV, c * 512)))
        for c in range(4):
            chunks.append((C_Q + c * 512, 512, "fm", [SQ[4 * c + i] for i in range(4)]))
        chunks.append((C_KV + 0 * 512, 512, "fm", [SKC[i] for i in range(4)]))
        chunks.append((C_KV + 1 * 512, 512, "fm", [SVC[i] for i in range(4)]))
        chunks.append((C_KV + 2 * 512, 512, "fm", [SKS[i] for i in range(4)]))
        chunks.append((C_KV + 3 * 512, 512, "tm", (SVS, 0)))
        chunks.append((C_KV + 4 * 512, 512, "fm", [SKW[i] for i in range(4)]))
        chunks.append((C_KV + 5 * 512, 512, "tm", (SVW, 0)))
        chunks.append((C_NG, 48, "tm", (SNG, 0)))
        for c in range(8):
            chunks.append((C_MG + c * 512, 512, "tm", (SMG, c * 512)))

        def load_chunk(ci):
            c0, n, _, _ = chunks[ci]
            p = ci % 2
            k.dma(wf[p][:, :, 0:n], w_in[:, c0:c0 + n].rearrange("(kc p) n -> p kc n", p=128), [], [("wf", p)])

        cnt = 0
        load_chunk(0)
        for ci, (c0, n, kind, dest) in enumerate(chunks):
            p = ci % 2
            if ci + 1 < len(chunks):
                load_chunk(ci + 1)
            k.cp("dve", wb[p][:, 0:8, 0:n], wf[p][:, 0:8, 0:n], [("wf", p)], [("wb", p)])
            k.cp("act", wb[p][:, 8:16, 0:n], wf[p][:, 8:16, 0:n], [("wf", p)], [("wb", p)])
            for u in range(16):
                b = 2 + cnt % 6
                o = ob[cnt % 4]
                okey = ("ob", cnt % 4)
                if kind == "tm":
                    t = u
                    for kc in range(16):
                        k.mm(PS(b)[:, 0:n], hT[:, kc, row(t)], wb[p][:, kc, 0:n], kc == 0, kc == 15,
                             [("hT", t), ("wb", p)], psk(b))
                    dst = dest[0][row(t), dest[1]:dest[1] + n]
                else:
                    blk, tc = u // 4, u % 4
                    for kc in range(16):
                        k.mm(PS(b)[:, 0:512], wb[p][:, kc, blk * 128:(blk + 1) * 128], hT[:, kc, tc * 512:(tc + 1) * 512],
                             kc == 0, kc == 15, [("hT", 4 * tc + i) for i in range(4)] + [("wb", p)], psk(b))
                    dst = dest[blk][:, tc * 512:(tc + 1) * 512]
                k.cp("act" if cnt % 2 == 0 else "dve", o[:, 0:n], PS(b)[:, 0:n], psk(b), [okey])
                k.dma(dst, o[:, 0:n], [okey], [("scr", ci, u)])
                cnt += 1
    k.release()
    k.barrier()

    if stop_after >= 2:
        k.mark()
        gam = k.sb([128, D], F32, "gam")
        bet = k.sb([128, D], F32, "bet")
        BS = k.sb([128, 16, 128], F32, "BS")
        WTm = k.sb([128, 16, 128], BF16, "WTm")
        bsT = k.sb([128, 16], F32, "bsT")
        k.mark()
        st1 = k.sb([128, 16, 128], F32, "st1")
        st2 = k.sb([128, 16, 128], F32, "st2")
        k.dma(gam[:], gm_ln_g.partition_broadcast(128), [], ["gam"])
        k.dma(bet[:], gm_ln_b.partition_broadcast(128), [], ["bet"])
        k.dma(bsT[:], gm_bT, [], ["bsT"])
        k.dma(st1[:], gm_wT, [], ["st1"])
        k.dma(st2[:], cin["c_tril"], [], ["st2"])
        k.tt("dve", WTm[:], st1[:], st2[:], ALU.mult, ["st1", "st2"], ["WTm"])
        k.cp("dve", BS[:], bsT[:].unsqueeze(2).to_broadcast([128, 16, 128]), ["bsT"], ["BS"])
        k.release()
        ut = [k.sb([128, D], F32, "ut") for _ in range(2)]
        vt = [k.sb([128, D], F32, "vt") for _ in range(2)]
        gt = [k.sb([128, D], F32, "gt") for _ in range(2)]
        tAu = k.sb([128, D], F32, "tAu"); tBu = k.sb([128, D], F32, "tBu")
        tAv = k.sb([128, D], F32, "tAv"); tBv = k.sb([128, D], F32, "tBv")
        tG = k.sb([128, D], F32, "tG")
        Uh = k.sb([128, D], F32, "Uh")
        G2 = k.sb([128, D], F32, "G2")
        vln = k.sb([128, D], BF16, "vln")
        mo = [k.sb([128, D], F32, "mo") for _ in range(2)]
        sm2 = [k.sb([128, 8], F32, "sm2") for _ in range(2)]
        BSf = BS[:].rearrange("p a b -> p (a b)")

        def load2(t):
            p = t % 2
            k.dma(ut[p][:], SU[row(t), :], [], [("ut", p)])
            k.dma(vt[p][:], SV[row(t), :], [], [("vt", p)])
            k.dma(gt[p][:], SMG[row(t), 0:D], [], [("gt", p)])

        load2(0)
        for t in range(NT):
            p = t % 2
            if t + 1 < NT:
                load2(t + 1)
            s = sm2[p]
            uk, vk, gk = ("ut", p), ("vt", p), ("gt", p)
            k.act(tAu[:], ut[p][:], AF.Square, [uk], ["tAu"])
            k.act(tAv[:], vt[p][:], AF.Square, [vk], ["tAv"])
            k.act(tG[:], gt[p][:], AF.Tanh, [gk], ["tG"], scale=0.5)
            k.ts("dve", tAu[:], tAu[:], 0.044715, 1.0, ALU.mult, ALU.add, ["tAu"], ["tAu"])
            k.tt("dve", tAu[:], tAu[:], ut[p][:], ALU.mult, ["tAu", uk], ["tAu"])
            k.act(tBu[:], tAu[:], AF.Tanh, ["tAu"], ["tBu"], scale=GC)
            k.ts("dve", tAv[:], tAv[:], 0.044715, 1.0, ALU.mult, ALU.add, ["tAv"], ["tAv"])
            k.tt("dve", tAv[:], tAv[:], vt[p][:], ALU.mult, ["tAv", vk], ["tAv"])
            k.act(tBv[:], tAv[:], AF.Tanh, ["tAv"], ["tBv"], scale=GC)
            k.ts("dve", tBu[:], tBu[:], 0.25, 0.25, ALU.mult, ALU.add, ["tBu"], ["tBu"])
            k.tt("dve", Uh[:], tBu[:], ut[p][:], ALU.mult, ["tBu", uk], ["Uh"])
            k.stt(G2[:], tBv[:], 1.0, vt[p][:], ALU.add, ALU.mult, ["tBv", vk], ["G2", ("s1", p)], accum=s[:, 0:1])
            k.act(tAv[:], G2[:], AF.Square, ["G2"], ["tAv", ("s2", p)], accum=s[:, 1:2])
            k.ts("dve", s[:, 2:3], s[:, 0:1], 1.0 / D, None, ALU.mult, None, [("s1", p)], [("mean2", p)])
            k.tt("dve", s[:, 3:4], s[:, 2:3], s[:, 2:3], ALU.mult, [("mean2", p)], [("msq", p)])
            k.stt(s[:, 4:5], s[:, 1:2], 1.0 / D, s[:, 3:4], ALU.mult, ALU.subtract, [("s2", p), ("msq", p)], [("var4", p)])
            k.ts("dve", s[:, 5:6], s[:, 4:5], 0.25, 1e-6, ALU.mult, ALU.add, [("var4", p)], [("v4", p)])
            k.tt("pool", s[:, 6:7], s[:, 5:6], nh[:], ALU.pow, [("v4", p), "nh"], [("rs", p)])
            k.ts("dve", s[:, 7:8], s[:, 6:7], 0.5, None, ALU.mult, None, [("rs", p)], [("rsh", p)])
            k.ts("dve", tBv[:], G2[:], s[:, 2:3], s[:, 7:8], ALU.subtract, ALU.mult, ["G2", ("mean2", p), ("rsh", p)], ["tBv"])
            k.tt("dve", tBv[:], tBv[:], gam[:], ALU.mult, ["tBv", "gam"], ["tBv"])
            k.tt("dve", vln[:], tBv[:], bet[:], ALU.add, ["tBv", "bet"], ["vln"])
            b0 = 4 * p
            for g in range(16):
                b = b0 + g // 4
                k.mm(PS(b)[:, (g % 4) * 128:(g % 4 + 1) * 128], WTm[:, g, :], vln[:, g * 128:(g + 1) * 128], True, True,
                     ["WTm", "vln"], psk(b))
            k.tt("dve", tBu[:], PS(b0, 4), BSf, ALU.add, psk(b0, 4) + ["BS"], ["tBu"])
            k.tt("dve", tBu[:], tBu[:], Uh[:], ALU.mult, ["tBu", "Uh"], ["tBu"])
            k.stt(mo[p][:], tG[:], 1.0, tBu[:], ALU.add, ALU.mult, ["tG", "tBu"], [("mo", p)])
            k.dma(SMA[row(t), :], mo[p][:], [("mo", p)], [("sma", t)])
        k.release()
        k.barrier()

    if stop_after >= 3:
        k.mark()
        cosT = k.sb([128, S], F32, "cosT"); sinT = k.sb([128, S], F32, "sinT")
        RT = k.sb([128, 128], F32, "RT")
        CAUS = k.sb([128, 128], BF16, "CAUS"); WINB = k.sb([128, 128], BF16, "WINB")
        CMPB = k.sb([128, S], BF16, "CMPB")
        Aagg = k.sb([128, 32], F32, "Aagg")
        AM = k.sb([128, 16, 32], F32, "AM"); ADDM = k.sb([128, 16, 32], F32, "ADDM")
        ESEL = k.sb([32, 16, 128], BF16, "ESEL")
        w1b = {m: k.sb([128, 32, 256], BF16, "w1b") for m in "kv"}
        w2b = {m: k.sb([128, 2, 128], BF16, "w2b") for m in "kv"}
        posb = {m: k.sb([128, 32], BF16, "posb") for m in "kv"}
        cvec = {m: k.sb([128, 2], F32, "cvec") for m in "kv"}
        stg = k.sb([128, 2 * S], F32, "stg")
        kcb = k.sb([128, S], BF16, "kcb"); vcb = k.sb([128, S], BF16, "vcb")
        ksr = k.sb([128, S], BF16, "ksr"); kwr = k.sb([128, S], BF16, "kwr")
        vsa = k.sb([128, 16, 129], BF16, "vsa"); vwa = k.sb([128, 16, 129], BF16, "vwa")
        qb = [k.sb([128, S], BF16, "qb") for _ in range(4)]
        qr = [k.sb([128, S], BF16, "qr") for _ in range(4)]
        sgr = k.sb([128, 16, 12], F32, "sgr"); sg = k.sb([128, 16, 12], F32, "sg")
        kcT = k.sb([128, 128], BF16, "kcT"); vca = k.sb([128, 130], BF16, "vca")
        hid = k.sb([128, 2, 128], BF16, "hid")
        hx = k.sb([128, 128], F32, "hx"); hy = k.sb([128, 128], F32, "hy"); hz = k.sb([128, 128], F32, "hz")
        biasT = k.sb([32, S], BF16, "biasT")
        PcT = [k.sb([128, 512], BF16, "PcT") for _ in range(2)]
        pn = k.sb([128, 512], F32, "pn"); rZ = [k.sb([128, 512], F32, "rZ") for _ in range(2)]; impS = k.sb([32, 512], F32, "impS")
        sc = k.sb([128, 128], F32, "sc"); selb = k.sb([128, 128], F32, "selb"); wk32 = k.sb([128, 32], F32, "wk32")
        m8 = k.sb([128, 16], F32, "m8")
        accp = [k.sb([128, 4, 4, 128], F32, "acc") for _ in range(2)]
        PT = [k.sb([128, 512], BF16, "PT") for _ in range(3)]
        t1 = [k.sb([128, 512], F32, "t1") for _ in range(2)]
        t2 = [k.sb([128, 512], F32, "t2") for _ in range(2)]
        czs = [k.sb([128, 2], F32, "cz") for _ in range(4)]

        def stgv(b):
            return stg[:, b * S:(b + 1) * S]

        k.dma(cosT[:], cin["c_cos"], [], ["cosT"]); k.dma(sinT[:], cin["c_sin"], [], ["sinT"])
        k.dma(RT[:], cin["c_rt"], [], ["RT"]); k.dma(Aagg[:], cin["c_aagg"], [], ["Aagg"])
        k.dma(AM[:], cin["c_am"], [], ["AM"]); k.dma(ADDM[:], cin["c_add"], [], ["ADDM"])
        k.dma(stgv(0), cin["c_cmpb"], [], [("stg", 0)])
        k.cp("dve", CMPB[:], stgv(0), [("stg", 0)], ["CMPB"])
        k.dma(stg[:, S:S + 128], cin["c_caus"], [], [("stg", 1)])
        k.dma(stg[:, S + 128:S + 256], cin["c_winb"], [], [("stg", 1)])
        k.cp("dve", CAUS[:], stg[:, S:S + 128], [("stg", 1)], ["CAUS"])
        k.cp("dve", WINB[:], stg[:, S + 128:S + 256], [("stg", 1)], ["WINB"])
        k.dma(stg[0:32, 0:S].rearrange("p (a b) -> p a b", b=128), cin["c_esel"], [("stg", 0)], [("stg", 0)])
        k.cp("dve", ESEL[:], stg[0:32, 0:S].rearrange("p (a b) -> p a b", b=128), [("stg", 0)], ["ESEL"])
        for m in "kv":
            for hf in range(2):
                k.dma(stg[:].rearrange("p (l n) -> p l n", n=256),
                      cw1[m][hf * 2048:(hf + 1) * 2048, :].rearrange("(l d) n -> d l n", d=128), [], [("stg", 0), ("stg", 1)])
                k.cp("dve" if hf == 0 else "act", w1b[m][:, hf * 16:(hf + 1) * 16, :], stg[:].rearrange("p (l n) -> p l n", n=256),
                     [("stg", 0), ("stg", 1)], [("w1b", m)])
            k.dma(stg[:, 0:256].rearrange("p (h d) -> p h d", d=128), cw2[m].rearrange("(h p) d -> p h d", p=128), [], [("stg", 0)])
            k.cp("dve", w2b[m][:], stg[:, 0:256].rearrange("p (h d) -> p h d", d=128), [("stg", 0)], [("w2b", m)])
            k.dma(stg[:, S:S + 32], posT[m], [], [("stg", 1)])
            k.cp("dve", posb[m][:], stg[:, S:S + 32], [("stg", 1)], [("posb", m)])
            for hf in range(2):
                for l in range(32):
                    k.mm(PS(7)[:, hf:hf + 1], w1b[m][:, l, hf * 128:(hf + 1) * 128], posb[m][:, l:l + 1], l == 0, l == 31,
                         [("w1b", m), ("posb", m)], psk(7))
            k.cp("dve", cvec[m][:], PS(7)[:, 0:2], psk(7), [("cvec", m)])
        k.memset("pool", vsa[:, :, 128:129], 1.0, ["vsa"])
        k.memset("pool", vwa[:, :, 128:129], 1.0, ["vwa"])
        k.memset("pool", vca[:, 128:129], 1.0, ["vca"])

        rot_i = [0]

        def rotary(src, skey, dst, dkey):
            for tc in range(4):
                i = rot_i[0] % 2
                rot_i[0] += 1
                sl = slice(tc * 512, (tc + 1) * 512)
                b = 4 + i
                k.mm(PS(b), RT[:], src[:, sl], True, True, [skey, "RT"], psk(b))
                k.tt("dve", t1[i][:], PS(b), sinT[:, sl], ALU.mult, psk(b) + ["sinT"], [("t1", i)])
                k.tt("pool", t2[i][:], src[:, sl], cosT[:, sl], ALU.mult, [skey, "cosT"], [("t2", i)])
                k.tt("dve", dst[:, sl], t1[i][:], t2[i][:], ALU.add, [("t1", i), ("t2", i)], [dkey])

        def compress(m, src, skey):
            for hf in range(2):
                b = 4 + hf
                for l in range(32):
                    k.mm(PS(b)[:, 0:127], w1b[m][:, l, hf * 128:(hf + 1) * 128], src[:, l:l + 16 * 126 + 1:16], l == 0, l == 31,
                         [("w1b", m), skey], psk(b))
                k.ts("dve", hx[:, 0:127], PS(b)[:, 0:127], cvec[m][:, hf:hf + 1], None, ALU.add, None, psk(b) + [("cvec", m)], ["hx"])
                k.act(hy[:, 0:127], hx[:, 0:127], AF.Square, ["hx"], ["hy"])
                k.ts("dve", hy[:, 0:127], hy[:, 0:127], 0.044715, 1.0, ALU.mult, ALU.add, ["hy"], ["hy"])
                k.tt("dve", hy[:, 0:127], hy[:, 0:127], hx[:, 0:127], ALU.mult, ["hy", "hx"], ["hy"])
                k.act(hz[:, 0:127], hy[:, 0:127], AF.Tanh, ["hy"], ["hz"], scale=GC)
                k.ts("dve", hz[:, 0:127], hz[:, 0:127], 0.5, 0.5, ALU.mult, ALU.add, ["hz"], ["hz"])
                k.tt("dve", hid[:, hf, 0:127], hz[:, 0:127], hx[:, 0:127], ALU.mult, ["hz", "hx"], ["hid"])
            if m == "k":
                for hf in range(2):
                    k.mm(PS(6)[:, 0:127], w2b[m][:, hf, :], hid[:, hf, 0:127], hf == 0, hf == 1, [("w2b", m), "hid"], psk(6))
                k.cp("act", kcT[:, 0:127], PS(6)[:, 0:127], psk(6), ["kcT"])
            else:
                for hf in range(2):
                    k.mm(PS(6)[0:127, 0:128], hid[:, hf, 0:127], w2b[m][:, hf, :], hf == 0, hf == 1, [("w2b", m), "hid"], psk(6))
                k.cp("act", vca[0:127, 0:128], PS(6)[0:127, 0:128], psk(6), ["vca"])

        sb_i = [0]
        ob_i = [0]
        pt_i = [0]
        cz_i = [0]
        OBANKS = [2, 3, 7]

        def attn(kT, kkey, va, vkey, qrt, qkey, qt, ktl, use_sel):
            obk = OBANKS[ob_i[0] % 3]
            ob_i[0] += 1
            nk = len(ktl)
            done = 0
            for g0 in range(0, nk, 4):
                grp = ktl[g0:g0 + 4]
                sbk = sb_i[0] % 2
                sb_i[0] += 1
                for j, (kt, eb) in enumerate(grp):
                    out = PS(sbk)[:, j * 128:(j + 1) * 128]
                    nmm = 1 + (1 if use_sel else 0) + (1 if eb else 0)
                    k.mm(out, kT[:, kt * 128:(kt + 1) * 128], qrt[:, row(qt)], True, nmm == 1, [kkey, qkey], psk(sbk))
                    i = 1
                    if use_sel:
                        i += 1
                        k.mm(out, ESEL[0:32, kt, :], biasT[0:32, row(qt)], False, i == nmm, ["ESEL", "biasT"], psk(sbk))
                    if eb:
                        k.mm(out, ident_b[:], (CAUS if eb == "caus" else WINB)[:], False, True, ["identb", "CAUS", "WINB"], psk(sbk))
                n = len(grp) * 128
                pi = pt_i[0] % 3
                pt_i[0] += 1
                k.act(PT[pi][:, 0:n], PS(sbk)[:, 0:n], AF.Exp, psk(sbk), [("PT", pi)], scale=SCALE)
                for j, (kt, eb) in enumerate(grp):
                    k.mm(PS(obk)[:, 0:129], PT[pi][:, j * 128:(j + 1) * 128], va[:, kt, :], done == 0, done == nk - 1,
                         [("PT", pi), vkey], psk(obk))
                    done += 1
            return obk

        def combine(obk, qt, qtl, g, br, ap, akey, first, guard):
            cz = czs[cz_i[0] % 4]
            ck = ("cz", cz_i[0] % 4)
            cz_i[0] += 1
            if guard:
                k.ts("dve", cz[:, 0:1], PS(obk)[:, 128:129], 1e-30, None, ALU.max, None, psk(obk), [ck])
                k.recip(cz[:, 0:1], cz[:, 0:1], [ck], [ck])
            else:
                k.recip(cz[:, 0:1], PS(obk)[:, 128:129], psk(obk), [ck])
            gate = sg[:, qt, g * 3 + br:g * 3 + br + 1]
            if first:
                k.ts("dve", ap[:, qtl, g, :], PS(obk)[:, 0:128], cz[:, 0:1], gate, ALU.mult, ALU.mult, psk(obk) + [ck, "sg"], [akey])
            else:
                k.tt("dve", cz[:, 1:2], cz[:, 0:1], gate, ALU.mult, [ck, "sg"], [ck])
                k.stt(ap[:, qtl, g, :], PS(obk)[:, 0:128], cz[:, 1:2], ap[:, qtl, g, :], ALU.mult, ALU.add, psk(obk) + [ck, akey], [akey])

        for hk in range(4 if stop_after >= 3 else 0):
            si = [0]

            def nxt():
                b = si[0] % 2
                si[0] += 1
                return b
            for nm, src, dst in (("kcb", SKC[hk], kcb), ("vcb", SVC[hk], vcb)):
                b = nxt()
                k.dma(stgv(b), src, [], [("stg", b)])
                k.cp("act", dst[:], stgv(b), [("stg", b)], [nm])
            for nm, src, dst in (("ksr", SKS[hk], ksr), ("kwr", SKW[hk], kwr)):
                b = nxt()
                k.dma(stgv(b), src, [], [("stg", b)])
                rotary(stgv(b), ("stg", b), dst, nm)
            for g in range(4):
                b = nxt()
                k.dma(stgv(b), SQ[4 * hk + g], [], [("stg", b)])
                k.cp("act", qb[g][:], stgv(b), [("stg", b)], [("qb", g)])
                rotary(stgv(b), ("stg", b), qr[g], ("qr", g))
            for nm, src, dst in (("vsa", SVS, vsa), ("vwa", SVW, vwa)):
                b = nxt()
                k.dma(stgv(b).rearrange("p (t d) -> p t d", d=128),
                      src[:, hk * 128:(hk + 1) * 128].rearrange("(t p) d -> p t d", p=128), [], [("stg", b)])
                k.cp("dve", dst[:, :, 0:128], stgv(b).rearrange("p (t d) -> p t d", d=128), [("stg", b)], [nm])
            k.dma(sgr[:], SNG[:, hk * 12:(hk + 1) * 12].rearrange("(t p) c -> p t c", p=128), [], ["sgr"])
            k.act(sg[:], sgr[:], AF.Tanh, ["sgr"], ["sg"], scale=0.5)
            k.ts("dve", sg[:], sg[:], 0.5, 0.5, ALU.mult, ALU.add, ["sg"], ["sg"])
            compress("k", kcb, "kcb")
            compress("v", vcb, "vcb")

            for qc in range(4):
                qsl = slice(qc * 512, (qc + 1) * 512)
                par = qc % 2
                ap = accp[par]
                def cmp_a(g):
                    pc = PcT[g % 2]
                    pk = ("pc", g % 2)
                    k.mm(PS(5)[0:127, :], kcT[:, 0:127], qb[g][:, qsl], True, False, ["kcT", ("qb", g)], psk(5))
                    k.mm(PS(5)[0:127, :], ident_b[0:127, 0:127], CMPB[0:127, qsl], False, True, ["identb", "CMPB"], psk(5))
                    k.act(pc[0:127, :], PS(5)[0:127, :], AF.Exp, psk(5), [pk], scale=SCALE)
                    k.mm(PS(5), ones_b[0:127, :], pc[0:127, :], True, True, ["onesb", pk], psk(5))
                    k.ts("dve", rZ[g % 2][:], PS(5), 1e-30, None, ALU.max, None, psk(5), [("rZ", g % 2)])

                def cmp_b(g):
                    pc = PcT[g % 2]
                    pk = ("pc", g % 2)
                    rz = rZ[g % 2]
                    rk = ("rZ", g % 2)
                    k.recip(rz[:], rz[:], [rk], [rk])
                    k.tt("dve", pn[0:127, :], pc[0:127, :], rz[0:127, :], ALU.mult, [pk, rk], ["pn"])
                    k.mm(PS(6)[0:32, :], Aagg[0:127, :], pn[0:127, :], g == 0, g == 3, ["Aagg", "pn"], psk(6))
                    for qtl in range(4):
                        qt = qc * 4 + qtl
                        obk = OBANKS[ob_i[0] % 3]
                        ob_i[0] += 1
                        k.mm(PS(obk)[:, 0:129], pc[0:127, qtl * 128:(qtl + 1) * 128], vca[0:127, 0:129], True, True, [pk, "vca"], psk(obk))
                        combine(obk, qt, qtl, g, 0, ap, ("acc", par, qtl), True, True)

                cmp_a(0)
                for g in range(4):
                    if g + 1 < 4:
                        cmp_a(g + 1)
                    cmp_b(g)
                k.cp("act", impS[0:32, :], PS(6)[0:32, :], psk(6), ["impS"])
                for qtl in range(4):
                    k.tr(PS(5)[:, qtl * 32:(qtl + 1) * 32], impS[0:32, qtl * 128:(qtl + 1) * 128], ident_f[0:32, 0:32],
                         ["impS", "identf"], psk(5))
                sc3 = sc[:].rearrange("p (a b) -> p a b", b=32)
                k.tt("dve", sc3, PS(5)[:, 0:128].rearrange("p (a b) -> p a b", b=32), AM[:, qc * 4:(qc + 1) * 4, :], ALU.mult,
                     psk(5) + ["AM"], ["sc"])
                k.tt("dve", sc3, sc3, ADDM[:, qc * 4:(qc + 1) * 4, :], ALU.add, ["sc", "ADDM"], ["sc"])
                for qtl in range(4):
                    ssl = sc[:, qtl * 32:(qtl + 1) * 32]
                    k.max8(m8[:, 0:8], ssl, ["sc"], ["m8"])
                    k.matchrep(wk32[:], m8[:, 0:8], ssl, -3e38, ["sc", "m8"], ["wk32"])
                    k.max8(m8[:, 8:16], wk32[:], ["wk32"], ["m8"])
                    k.ts("dve", selb[:, qtl * 32:(qtl + 1) * 32], ssl, m8[:, 15:16], -NEG, ALU.is_ge, ALU.mult, ["sc", "m8"], ["selb"])
                k.ts("dve", selb[:], selb[:], NEG, None, ALU.add, None, ["selb"], ["selb"])
                for qtl in range(4):
                    k.tr(PS(6)[0:32, qtl * 128:(qtl + 1) * 128], selb[:, qtl * 32:(qtl + 1) * 32], ident_f[:], ["selb", "identf"], psk(6))
                k.cp("act", biasT[0:32, qsl], PS(6)[0:32, :], psk(6), ["biasT"])
                branches = []
                for qtl in range(4):
                    qt = qc * 4 + qtl
                    for g in range(4):
                        ktl = [(kt, "caus" if kt == qt else None) for kt in range(qt + 1)]
                        branches.append(dict(kT=ksr, kkey="ksr", va=vsa, vkey="vsa", g=g, qt=qt, qtl=qtl, ktl=ktl, sel=True, br=1, last=False))
                        ktl = [(kt, "caus" if kt == qt else ("winb" if kt == qt - 4 else None)) for kt in range(max(0, qt - 4), qt + 1)]
                        branches.append(dict(kT=kwr, kkey="kwr", va=vwa, vkey="vwa", g=g, qt=qt, qtl=qtl, ktl=ktl, sel=False, br=2, last=(g == 3)))
                groups = []
                for bi, B in enumerate(branches):
                    nk = len(B["ktl"])
                    for g0 in range(0, nk, 4):
                        groups.append(dict(b=bi, kts=B["ktl"][g0:g0 + 4], first=(g0 == 0), lastg=(g0 + 4 >= nk), base=g0))

                def emit_scores(G_):
                    B = branches[G_["b"]]
                    sbk = (0, 1, 4)[sb_i[0] % 3]
                    sb_i[0] += 1
                    G_["sbk"] = sbk
                    qt, g = B["qt"], B["g"]
                    for j, (kt, eb) in enumerate(G_["kts"]):
                        out = PS(sbk)[:, j * 128:(j + 1) * 128]
                        nmm = 1 + (1 if B["sel"] else 0) + (1 if eb else 0)
                        k.mm(out, B["kT"][:, kt * 128:(kt + 1) * 128], qr[g][:, row(qt)], True, nmm == 1, [B["kkey"], ("qr", g)], psk(sbk))
                        i = 1
                        if B["sel"]:
                            i += 1
                            k.mm(out, ESEL[0:32, kt, :], biasT[0:32, row(qt)], False, i == nmm, ["ESEL", "biasT"], psk(sbk))
                        if eb:
                            k.mm(out, ident_b[:], (CAUS if eb == "caus" else WINB)[:], False, True, ["identb", "CAUS", "WINB"], psk(sbk))

                def emit_exp_pv(G_):
                    B = branches[G_["b"]]
                    sbk = G_["sbk"]
                    if G_["first"]:
                        B["obk"] = OBANKS[ob_i[0] % 3]
                        ob_i[0] += 1
                    obk = B["obk"]
                    n = len(G_["kts"]) * 128
                    pi = pt_i[0] % 3
                    pt_i[0] += 1
                    k.act(PT[pi][:, 0:n], PS(sbk)[:, 0:n], AF.Exp, psk(sbk), [("PT", pi)], scale=SCALE)
                    nk = len(B["ktl"])
                    for j, (kt, eb) in enumerate(G_["kts"]):
                        idx = G_["base"] + j
                        k.mm(PS(obk)[:, 0:129], PT[pi][:, j * 128:(j + 1) * 128], B["va"][:, kt, :], idx == 0, idx == nk - 1,
                             [("PT", pi), B["vkey"]], psk(obk))
                    if G_["lastg"]:
                        akey = ("acc", par, B["qtl"])
                        combine(obk, B["qt"], B["qtl"], B["g"], B["br"], ap, akey, False, False)
                        if B["last"]:
                            k.dma(SOB[row(B["qt"]), hk * 512:(hk + 1) * 512], ap[:, B["qtl"]].rearrange("p g d -> p (g d)"), [akey],
                                  [("sob", hk, B["qt"])])

                emit_scores(groups[0])
                if len(groups) > 1:
                    emit_scores(groups[1])
                for gi_ in range(len(groups)):
                    if gi_ + 2 < len(groups):
                        emit_scores(groups[gi_ + 2])
                    emit_exp_pv(groups[gi_])
        k.release()
        k.barrier()

    if stop_after >= 4:
        k.mark()
        wo = k.sb([128, 16, D], BF16, "wo")
        k.mark()
        stg4 = k.sb([128, 16, 512], F32, "stg4")
        for c in range(4):
            k.dma(stg4[:], w_out[:, c * 512:(c + 1) * 512].rearrange("(kc p) n -> p kc n", p=128), [], ["stg4"])
            k.cp("dve", wo[:, 0:8, c * 512:(c + 1) * 512], stg4[:, 0:8, :], ["stg4"], ["wo"])
            k.cp("act", wo[:, 8:16, c * 512:(c + 1) * 512], stg4[:, 8:16, :], ["stg4"], ["wo"])
        k.release()
        obt = [k.sb([128, D], F32, "obt") for _ in range(2)]
        mat = [k.sb([128, D], F32, "mat") for _ in range(2)]
        gbt = [k.sb([128, D], F32, "gbt") for _ in range(2)]
        xtt = [k.sb([128, D], F32, "xtt") for _ in range(2)]
        mb = k.sb([128, D], BF16, "mb")
        mT = k.sb([128, 16, 128], BF16, "mT")
        x1o = [k.sb([128, D], F32, "x1o") for _ in range(2)]

        def load4(t):
            p = t % 2
            k.dma(obt[p][:], SOB[row(t), :], [], [("obt", p)])
            k.dma(mat[p][:], SMA[row(t), :], [], [("mat", p)])
            k.dma(gbt[p][:], SMG[row(t), D:2 * D], [], [("gbt", p)])
            k.dma(xtt[p][:], x[row(t), :], [], [("xtt", p)])

        load4(0)
        for t in range(NT):
            p = t % 2
            if t + 1 < NT:
                load4(t + 1)
            k.act(gbt[p][:], gbt[p][:], AF.Tanh, [("gbt", p)], [("gbt", p)], scale=0.5)
            k.ts("dve", gbt[p][:], gbt[p][:], 0.5, 0.5, ALU.mult, ALU.add, [("gbt", p)], [("gbt", p)])
            k.tt("dve", obt[p][:], obt[p][:], gbt[p][:], ALU.mult, [("obt", p), ("gbt", p)], [("obt", p)])
            k.tt("dve", mb[:], obt[p][:], mat[p][:], ALU.add, [("obt", p), ("mat", p)], ["mb"])
            for kc in range(16):
                b = kc // 8
                k.tr(PSB(b)[:, (kc % 8) * 128:(kc % 8 + 1) * 128], mb[:, kc * 128:(kc + 1) * 128], ident_b[:], ["mb", "identb"], psk(b))
            for hh in range(2):
                k.cp("dve" if hh == 0 else "act", mT[:, hh * 8:(hh + 1) * 8, :], PSB(hh).rearrange("p (a b) -> p a b", b=128),
                     psk(hh), ["mT"])
            for c in range(4):
                b = 2 + c
                for kc in range(16):
                    k.mm(PS(b), mT[:, kc, :], wo[:, kc, c * 512:(c + 1) * 512], kc == 0, kc == 15, ["mT", "wo"], psk(b))
                k.tt("dve", x1o[p][:, c * 512:(c + 1) * 512], PS(b), xtt[p][:, c * 512:(c + 1) * 512], ALU.add,
                     psk(b) + [("xtt", p)], [("x1o", p)])
            k.dma(SX1[row(t), :], x1o[p][:], [("x1o", p)], [("sx1", t)])
        k.release()
        k.barrier()

    if stop_after >= 5:
        k.mark()
        eidT_all = k.sb([128, 16, 128], I32, "eidTall")
        gateT_all = k.sb([128, 16, 128], F32, "gateTall")
        iota16 = k.sb([128, 16], F32, "iota16")
        k.dma(iota16[:], cin["c_iota16"], [], ["iota16"])
        k.mark()
        wq = k.sb([128, 16, D], BF16, "wq")
        k.mark()
        stg5 = k.sb([128, 16, 512], F32, "stg5")
        for c in range(4):
            k.dma(stg5[:], peer_w_q[:, c * 512:(c + 1) * 512].rearrange("(kc p) n -> p kc n", p=128), [], ["stg5"])
            k.cp("dve", wq[:, 0:8, c * 512:(c + 1) * 512], stg5[:, 0:8, :], ["stg5"], ["wq"])
            k.cp("act", wq[:, 8:16, c * 512:(c + 1) * 512], stg5[:, 8:16, :], ["stg5"], ["wq"])
        k.release()
        kTf = [k.sb([128, 128], F32, "kTf") for _ in range(2)]
        g2b = k.sb([128, D], F32, "g2b")
        x1a = [k.sb([128, D], F32, "x1a") for _ in range(2)]
        h2a = [k.sb([128, D], BF16, "h2a") for _ in range(2)]
        jk = k.sb([128, D], BF16, "jk")
        h2T = k.sb([128, 16, 128], BF16, "h2T")
        qTs = k.sb([128, 16, 128], F32, "qTs")
        Sxa = [k.sb([128, 16, 128], F32, "Sxa") for _ in range(2)]
        sm5 = [k.sb([128, 4], F32, "sm5") for _ in range(2)]
        cand = k.sb([128, 8, 16, 16], F32, "cand"); eqt = k.sb([128, 8, 16, 16], F32, "eqt")
        V16 = k.sb([128, 16, 16], F32, "V16"); I16u = k.sb([128, 16, 16], U32, "I16u"); I16f = k.sb([128, 16, 16], F32, "I16f")
        wk = k.sb([128, 128], F32, "wk"); wk2 = k.sb([128, 256], F32, "wk2")
        T16 = k.sb([128, 8, 16], F32, "T16"); P16u = k.sb([128, 8, 16], U32, "P16u")
        A16u = k.sb([128, 8, 16], U32, "A16u"); B16u = k.sb([128, 8, 16], U32, "B16u")
        af = k.sb([128, 8, 16], F32, "af"); bf = k.sb([128, 8, 16], F32, "bf")
        ex = k.sb([128, 8, 16], F32, "ex"); gate = k.sb([128, 8, 16], F32, "gate")
        zs = k.sb([128, 8], F32, "zs")
        i1f = k.sb([128, 8, 16], F32, "i1f"); i2f = k.sb([128, 8, 16], F32, "i2f"); eid = k.sb([128, 8, 16], F32, "eid")
        io_b = iota16[:].unsqueeze(1).unsqueeze(1).to_broadcast([128, 8, 16, 16])
        V16v = V16[:].rearrange("p (h two) a -> p h two a", two=2)
        I16v = I16f[:].rearrange("p (h two) a -> p h two a", two=2)
        k.dma(kTf[0][:], keysT[0], [], ["kTf"]); k.dma(kTf[1][:], keysT[1], [], ["kTf"])
        k.dma(g2b[:], norm_ffn_g.partition_broadcast(128), [], ["g2b"])
        def front(t):
            p = t % 2
            kp = "n5%d" % p
            k.dma(x1a[p][:], SX1[row(t), :], [], [("x1a", p)])
            k.act(jk[:], x1a[p][:], AF.Square, [("x1a", p)], ["jk", kp + "ss"], accum=sm5[p][:, 0:1])
            k.rstd_from_ss(sm5[p][:, 0:1], sm5[p][:, 2:3], sm5[p][:, 1:2], nh[:], D, kp)
            k.stt(h2a[p][:], x1a[p][:], sm5[p][:, 2:3], g2b[:], ALU.mult, ALU.mult, [("x1a", p), kp + "rstd", "g2b"], [("h2a", p)])
            k.dma(SH2[row(t), :], h2a[p][:], [("h2a", p)], [("sh2", t)])
            for kc in range(16):
                b = kc // 8
                k.tr(PSB(b)[:, (kc % 8) * 128:(kc % 8 + 1) * 128], h2a[p][:, kc * 128:(kc + 1) * 128], ident_b[:], [("h2a", p), "identb"], psk(b))
            for hh in range(2):
                k.cp("dve" if hh == 0 else "act", h2T[:, hh * 8:(hh + 1) * 8, :], PSB(hh).rearrange("p (a b) -> p a b", b=128),
                     psk(hh), ["h2T"])
            for cb in range(16):
                b = 4 + cb // 4
                for kc in range(16):
                    k.mm(PS(b)[:, (cb % 4) * 128:(cb % 4 + 1) * 128], wq[:, kc, cb * 128:(cb + 1) * 128], h2T[:, kc, :], kc == 0, kc == 15,
                         ["wq", "h2T"], psk(b))
            k.cp("act", qTs[:].rearrange("p a b -> p (a b)"), PS(4, 4), psk(4, 4), ["qTs"])
            for cb in range(16):
                b = 4 + cb // 4
                k.mm(PS(b)[:, (cb % 4) * 128:(cb % 4 + 1) * 128], qTs[:, cb, :], kTf[cb % 2][:], True, True, ["qTs", "kTf"], psk(b))
            k.cp("act", Sxa[p][:].rearrange("p a b -> p (a b)"), PS(4, 4), psk(4, 4), [("Sxa", p)])

        def topk(t):
            eT, gT = eidT_all[:, t, :], gateT_all[:, t, :]
            ek, gk = ("eidT", t), ("gateT", t)
            Sx = Sxa[t % 2]
            sxk = ("Sxa", t % 2)
            for hb_ in range(16):
                k.max8(V16[:, hb_, 0:8], Sx[:, hb_, :], [sxk], ["V16"])
                k.maxidx(I16u[:, hb_, 0:8], V16[:, hb_, 0:8], Sx[:, hb_, :], [sxk, "V16"], ["I16u"])
                k.matchrep(wk[:], V16[:, hb_, 0:8], Sx[:, hb_, :], -3e38, [sxk, "V16"], ["wk"])
                k.max8(V16[:, hb_, 8:16], wk[:], ["wk"], ["V16"])
                k.maxidx(I16u[:, hb_, 8:16], V16[:, hb_, 8:16], wk[:], ["wk", "V16"], ["I16u"])
            k.cp("dve", I16f[:], I16u[:], ["I16u"], ["I16f"])
            k.tt("dve", cand[:], V16v[:, :, 0, :].unsqueeze(3).to_broadcast([128, 8, 16, 16]),
                 V16v[:, :, 1, :].unsqueeze(2).to_broadcast([128, 8, 16, 16]), ALU.add, ["V16"], ["cand"])
            for h in range(8):
                ch = cand[:, h].rearrange("p a b -> p (a b)")
                k.max8(T16[:, h, 0:8], ch, ["cand"], ["T16"])
                k.maxidx(P16u[:, h, 0:8], T16[:, h, 0:8], ch, ["cand", "T16"], ["P16u"])
                k.matchrep(wk2[:], T16[:, h, 0:8], ch, -3e38, ["cand", "T16"], ["wk2"])
                k.max8(T16[:, h, 8:16], wk2[:], ["wk2"], ["T16"])
                k.maxidx(P16u[:, h, 8:16], T16[:, h, 8:16], wk2[:], ["wk2", "T16"], ["P16u"])
            k.op("dve", lambda e: e.tensor_single_scalar(out=A16u[:], in_=P16u[:], scalar=4, op=ALU.logical_shift_right), ["P16u"], ["A16u"])
            k.op("dve", lambda e: e.tensor_single_scalar(out=B16u[:], in_=P16u[:], scalar=15, op=ALU.bitwise_and), ["P16u"], ["B16u"])
            k.cp("dve", af[:], A16u[:], ["A16u"], ["af"])
            k.cp("dve", bf[:], B16u[:], ["B16u"], ["bf"])
            for (pf, col, dst, dk) in ((af, 0, i1f, "i1f"), (bf, 1, i2f, "i2f")):
                k.tt("dve", eqt[:], io_b, pf[:].unsqueeze(3).to_broadcast([128, 8, 16, 16]), ALU.is_equal, ["iota16", "af", "bf"], ["eqt"])
                k.tt("dve", eqt[:], eqt[:], I16v[:, :, col, :].unsqueeze(2).to_broadcast([128, 8, 16, 16]), ALU.mult, ["eqt", "I16f"], ["eqt"])
                k.rsum(dst[:], eqt[:], ["eqt"], [dk])
            k.stt(eid[:], i1f[:], 128.0, i2f[:], ALU.mult, ALU.add, ["i1f", "i2f"], ["eid"])
            k.ts("dve", eid[:], eid[:], 0.0, 16383.0, ALU.max, ALU.min, ["eid"], ["eid"])
            k.tr(PS(2)[:, 0:128], eid[:].rearrange("p a b -> p (a b)"), ident_f[:], ["eid", "identf"], psk(2))
            k.cp("dve", eT, PS(2)[:, 0:128], psk(2), [ek])
            k.tt("dve", ex[:], T16[:], T16[:, :, 0:1].to_broadcast([128, 8, 16]), ALU.subtract, ["T16"], ["ex"])
            k.act(ex[:], ex[:], AF.Exp, ["ex"], ["ex"])
            k.rsum(zs[:], ex[:], ["ex"], ["zs"])
            k.recip(zs[:], zs[:], ["zs"], ["zs"])
            k.tt("dve", gate[:], ex[:], zs[:].unsqueeze(2).to_broadcast([128, 8, 16]), ALU.mult, ["ex", "zs"], ["gate"])
            k.tr(PS(2)[:, 128:256], gate[:].rearrange("p a b -> p (a b)"), ident_f[:], ["gate", "identf"], psk(2))
            k.cp("dve", gT, PS(2)[:, 128:256], psk(2), [gk])


        front(0)
        for t in range(NT):
            if t + 1 < NT:
                front(t + 1)
            topk(t)
        k.release()
        k.barrier()

    if stop_after >= 5:
        k.mark()
        gfb = k.sb([128, D], F32, "gfb")
        SELR = k.sb([128, 128, 128], BF16, "SELR")
        IDm = k.sb([128, 128, 128], BF16, "IDm")
        WSEL = k.sb([128, 128, 128], BF16, "WSEL")
        Ubuf = [k.sb([128, D], F32, "Ubuf") for _ in range(2)]
        Vbuf = [k.sb([128, D], F32, "Vbuf") for _ in range(2)]
        Vbf = [k.sb([128, D], BF16, "Vbf") for _ in range(2)]
        x1t = k.sb([128, D], F32, "x1t")
        yo = k.sb([128, D], F32, "yo")
        h2b = k.sb([128, D], BF16, "h2b")
        jk = k.sb([128, 1024], BF16, "jk")
        A0 = k.sb([128, 128], F32, "A0"); A1 = k.sb([128, 128], F32, "A1")
        At = k.sb([128, 128], F32, "At"); Wt = k.sb([128, 128], F32, "Wt")
        g1 = k.sb([128, 128], F32, "g1"); g2 = k.sb([128, 128], F32, "g2")
        sm6 = k.sb([128, 8], F32, "sm6")

        k.dma(gfb[:], norm_final_g.partition_broadcast(128), [], ["gfb"])
        k.cp("dve", SELR[:], ident_f[:].unsqueeze(2).to_broadcast([128, 128, 128]), ["identf"], ["SELR"])
        idflat = cin["c_ident"].rearrange("a b -> (a b)")
        for c in range(8):
            k.dma(Ubuf[c % 2][:, :], idflat[c * 2048:(c + 1) * 2048].partition_broadcast(128), [], [("G", c % 2)])
            k.cp("dve", IDm[:, c * 16:(c + 1) * 16, :].rearrange("p a b -> p (a b)"), Ubuf[c % 2][:, :], [("G", c % 2)], ["IDm"])
        k.memset("dve", A0[:], 0.0, ["A0", "setup5b"])
        k.memset("dve", A1[:], 0.0, ["A1", "setup5b"])
        gi = [0]

        G = Ubuf + Vbuf + [k.sb([128, D], F32, "Gx") for _ in range(2)]
        NG_ = len(G)
        _unused = None

        def stage1(t):
            eT, ek = eidT_all[:, t, :], ("eidT", t)
            for tk in range(128):
                i = gi[0] % NG_
                gi[0] += 1
                k.op("pool", (lambda e, i=i, tk=tk: e.indirect_dma_start(
                    out=G[i][:, :], out_offset=None, in_=peer_u,
                    in_offset=bass.IndirectOffsetOnAxis(ap=eT[:, tk:tk + 1], axis=0))), [ek, ("gateT", t), "yo", "IDm", "SELR", "setup5b"], [("G", i)], dma=True)
                for c in range(4):
                    k.mm(PS(4 + c), SELR[:, tk, :], h2b[:, c * 512:(c + 1) * 512], True, True, ["SELR", "h2b"], psk(4 + c))
                for hf, Ax in ((0, A0), (1, A1)):
                    k.stt(jk[:, 0:1024], G[i][:, hf * 1024:(hf + 1) * 1024], 1.0, PS(4 + 2 * hf, 2), ALU.mult, ALU.mult,
                          [("G", i)] + psk(4 + 2 * hf, 2), ["jk", "A%d" % hf], accum=Ax[:, tk:tk + 1])

        def post1(t):
            gk = ("gateT", t)
            k.tt("dve", At[:], A0[:], A1[:], ALU.add, ["A0", "A1"], ["At"])
            k.act(g1[:], At[:], AF.Square, ["At"], ["g1"])
            k.ts("dve", g1[:], g1[:], 0.044715, 1.0, ALU.mult, ALU.add, ["g1"], ["g1"])
            k.tt("dve", g1[:], g1[:], At[:], ALU.mult, ["g1", "At"], ["g1"])
            k.act(g2[:], g1[:], AF.Tanh, ["g1"], ["g2"], scale=GC)
            k.ts("dve", g2[:], g2[:], 0.5, 0.5, ALU.mult, ALU.add, ["g2"], ["g2"])
            k.tt("dve", g2[:], g2[:], At[:], ALU.mult, ["g2", "At"], ["g2"])
            k.tt("dve", Wt[:], g2[:], gateT_all[:, t, :], ALU.mult, ["g2", gk], ["Wt"])
            k.tt("dve", WSEL[:], IDm[:], Wt[:].unsqueeze(2).to_broadcast([128, 128, 128]), ALU.mult, ["IDm", "Wt"], ["WSEL"])

        def stage2(t):
            eT, ek = eidT_all[:, t, :], ("eidT", t)
            for tk in range(128):
                i = gi[0] % NG_
                gi[0] += 1
                k.op("pool", (lambda e, i=i, tk=tk: e.indirect_dma_start(
                    out=G[i][:, :], out_offset=None, in_=peer_v,
                    in_offset=bass.IndirectOffsetOnAxis(ap=eT[:, tk:tk + 1], axis=0))), [ek, "WSEL"], [("G", i)], dma=True)
                j = tk % 2
                k.cp("act", Vbf[j][:], G[i][:], [("G", i)], [("Vbf", j)])
                for c in range(4):
                    k.mm(PS(c), WSEL[:, tk, :], Vbf[j][:, c * 512:(c + 1) * 512], tk == 0, tk == 127, ["WSEL", ("Vbf", j)], psk(c))

        def final(t):
            k.dma(x1t[:], SX1[row(t), :], [], ["x1t"])
            k.tt("dve", x1t[:], PS(0, 4), x1t[:], ALU.add, psk(0, 4) + ["x1t"], ["x1t"])
            k.act(yo[:], x1t[:], AF.Square, ["x1t"], ["yo", "n6ss"], accum=sm6[:, 4:5])
            k.rstd_from_ss(sm6[:, 4:5], sm6[:, 6:7], sm6[:, 5:6], nh[:], D, "n6")
            k.stt(yo[:], x1t[:], sm6[:, 6:7], gfb[:], ALU.mult, ALU.mult, ["x1t", "n6rstd", "gfb"], ["yo"])
            k.dma(y[row(t), :], yo[:], ["yo"], [("y", t)])

        for t in range(NT):
            k.dma(h2b[:], SH2[row(t), :], [], ["h2b"])
            stage1(t)
            post1(t)
            stage2(t)
            final(t)
        k.release()
        k.release()

    k.emit()
    return nc, k


_CACHE = {}


def make_in_maps(inputs):
    f = lambda a: np.ascontiguousarray(np.asarray(a, dtype=np.float32))
    consts = make_consts()
    shared = {
        "norm_mix_g": f(inputs["norm_mix_g"][0:1]),
        "w_in": f(inputs["w_in"][0]),
        "w_out": f(inputs["w_out"][0]),
        "gm_ln_g": f(inputs["gm_ln_g"][0:1]),
        "gm_ln_b": f(inputs["gm_ln_b"][0:1]),
        "gm_wT": f(np.transpose(np.asarray(inputs["gm_spatial_w"][0]), (2, 0, 1))),
        "gm_bT": f(np.transpose(np.asarray(inputs["gm_spatial_b"][0]), (1, 0))),
        "cmp_posT_k": f(np.asarray(inputs["cmp_pos_k"][0]).T),
        "cmp_posT_v": f(np.asarray(inputs["cmp_pos_v"][0]).T),
        "cmp_w1_k": f(inputs["cmp_w1_k"][0]), "cmp_w1_v": f(inputs["cmp_w1_v"][0]),
        "cmp_w2_k": f(inputs["cmp_w2_k"][0]), "cmp_w2_v": f(inputs["cmp_w2_v"][0]),
        "norm_ffn_g": f(inputs["norm_ffn_g"][0:1]),
        "peer_w_q": f(inputs["peer_w_q"][0]),
        "keys1T": f(np.asarray(inputs["peer_keys1"][0]).T),
        "keys2T": f(np.asarray(inputs["peer_keys2"][0]).T),
        "peer_u": f(inputs["peer_u"][0]),
        "peer_v": f(inputs["peer_v"][0]),
        "norm_final_g": f(np.asarray(inputs["norm_final_g"]).reshape(1, D)),
    }
    shared.update(consts)
    xs = np.asarray(inputs["x"], dtype=np.float32)
    maps = []
    for b in range(8):
        m = dict(shared)
        m["x"] = np.ascontiguousarray(xs[b])
        maps.append(m)
    return maps


def kernel(**inputs):
    if "nc" not in _CACHE:
        _CACHE["nc"] = build_program()[0]
    nc = _CACHE["nc"]
    in_maps = make_in_maps(inputs)
    res = run_bass_kernel_spmd(nc, in_maps, core_ids=list(range(8)))
    out = np.stack([np.asarray(r["y"]) for r in res.results], axis=0)
    return out.astype(np.float32)
```

```python
import numpy as np
import concourse.bass as bass
import concourse.mybir as mybir
from concourse.bass_utils import run_bass_kernel_spmd

F32 = mybir.dt.float32
BF16 = mybir.dt.bfloat16
I32 = mybir.dt.int32
U32 = mybir.dt.uint32
ALU = mybir.AluOpType
AF = mybir.ActivationFunctionType
AX = mybir.AxisListType

ENGS = ("pe", "act", "dve", "pool", "sp")
EPOCH = 30000
NDMASEM = 8
SB_BASE = 16512
SB_TOP = 229344


class Op:
    __slots__ = ("eng", "fn", "deps", "is_dma", "idx", "sig", "semref")

    def __init__(self, eng, fn, is_dma):
        self.eng = eng
        self.fn = fn
        self.deps = []
        self.is_dma = is_dma
        self.sig = False
        self.semref = None


class Prog:
    def __init__(self, nc):
        self.nc = nc
        self.ops = []
        self.last_w = {}
        self.readers = {}
        self.last_on_eng = {e: None for e in ENGS}
        self.barrier_deps = []
        self.need_barrier = {e: False for e in ENGS}
        self.sb_off = SB_BASE
        self.marks = []
        self.names = 0
        self.recent_dma = {}

    def sb(self, shape, dtype, name=None):
        self.names += 1
        name = (name or "t") + "_%d" % self.names
        esz = {F32: 4, BF16: 2, I32: 4, U32: 4}[dtype]
        n = 1
        for s in shape[1:]:
            n *= s
        nbytes = (n * esz + 31) // 32 * 32
        off = self.sb_off
        self.sb_off += nbytes
        assert self.sb_off <= SB_TOP, ("SBUF overflow", name, self.sb_off)
        return self.nc.alloc_sbuf_tensor_at(name, list(shape), dtype, offset=off)

    def mark(self):
        self.marks.append(self.sb_off)

    def release(self):
        self.sb_off = self.marks.pop()
        self.barrier()

    def barrier(self):
        self.barrier_deps = [self.last_on_eng[e] for e in ENGS if self.last_on_eng[e] is not None]
        for q in self.recent_dma.values():
            self.barrier_deps.extend(q)
        for e in ENGS:
            self.need_barrier[e] = True

    def op(self, eng, fn, reads=(), writes=(), dma=False):
        o = Op(eng, fn, dma or eng == "sp")
        o.idx = len(self.ops)
        deps = set()
        if self.need_barrier[eng]:
            deps.update(self.barrier_deps)
            self.need_barrier[eng] = False
        reads = list(reads)
        writes = list(writes)
        for k in list(reads):
            if isinstance(k, str) and k.startswith("ps"):
                reads.remove(k)
                if k not in writes:
                    writes.append(k)
        for k in reads:
            w = self.last_w.get(k)
            if w is not None:
                deps.add(w)
        for k in writes:
            w = self.last_w.get(k)
            if w is not None:
                deps.add(w)
            rd = self.readers.get(k)
            if rd:
                for v in rd.values():
                    if isinstance(v, list):
                        deps.update(v)
                    else:
                        deps.add(v)
        for k in reads:
            rd = self.readers.setdefault(k, {})
            if o.is_dma:
                rd.setdefault("dma_" + eng, []).append(o.idx)
            else:
                rd[eng] = o.idx
        for k in writes:
            self.last_w[k] = o.idx
            self.readers[k] = {}
        deps.discard(o.idx)
        o.deps = sorted(deps)
        self.ops.append(o)
        self.last_on_eng[eng] = o.idx
        if o.is_dma:
            q = self.recent_dma.setdefault(eng, [])
            q.append(o.idx)
            if len(q) > NDMASEM:
                q.pop(0)
        return o

    def emit(self):
        nc = self.nc
        ops = self.ops
        for o in ops:
            latest = {}
            keep = []
            for d in o.deps:
                p = ops[d]
                if p.is_dma:
                    keep.append(d)
                elif p.eng == "pe" and o.eng == "pe":
                    continue
                else:
                    if d > latest.get(p.eng, -1):
                        latest[p.eng] = d
            keep.extend(latest.values())
            o.deps = sorted(keep)
            for d in o.deps:
                ops[d].sig = True
        sems = {}

        def getsem(key):
            if key not in sems:
                sems[key] = nc.alloc_semaphore("s_%s_%s" % key)
            return sems[key]

        cnt = {e: 0 for e in ENGS}
        dcnt = {}
        dper = {}
        prev_on_dsem = {}
        for o in ops:
            if o.is_dma:
                q = o.eng
                i = dcnt.get(q, 0)
                dcnt[q] = i + 1
                j = i % NDMASEM
                key = ("d" + q, j)
                c = dper.get(key, 0) + 1
                dper[key] = c
                o.semref = (key, 16 * c)
                if c > 1:
                    o.deps = sorted(set(o.deps) | {prev_on_dsem[key]})
                prev_on_dsem[key] = o.idx
                o.sig = True
            elif o.sig:
                kk = cnt[o.eng]
                cnt[o.eng] = kk + 1
                o.semref = ((o.eng, kk // EPOCH), (kk % EPOCH) + 1)
        for o in ops:
            if o.semref is not None:
                getsem(o.semref[0])
        self.stats = dict(nops=len(ops), nsig=sum(1 for o in ops if o.sig), cnt=dict(cnt), dcnt=dict(dcnt),
                          nsem=len(sems))
        per_eng = {e: [o for o in ops if o.eng == e] for e in ENGS}
        final_dma = {key: 16 * c for key, c in dper.items()}

        def run(engname, eng):
            waited = {}
            for o in per_eng[engname]:
                for d in o.deps:
                    p = ops[d]
                    if p.semref is None:
                        continue
                    if p.eng == "pe" and o.eng == "pe" and not p.is_dma:
                        continue
                    key, val = p.semref
                    if waited.get(key, 0) >= val:
                        continue
                    waited[key] = val
                    eng.wait_ge(sems[key], val)
                inst = o.fn(eng)
                if o.semref is not None:
                    key, val = o.semref
                    inst.then_inc(sems[key], 16 if o.is_dma else 1)
            if engname == "sp":
                for key, val in final_dma.items():
                    if waited.get(key, 0) < val:
                        eng.wait_ge(sems[key], val)

        with nc.Block() as block:
            @block.tensor
            def _(e):
                run("pe", e)

            @block.scalar
            def _(e):
                run("act", e)

            @block.vector
            def _(e):
                run("dve", e)

            @block.gpsimd
            def _(e):
                run("pool", e)

            @block.sync
            def _(e):
                run("sp", e)


class K(Prog):
    def mm(self, out, lhsT, rhs, start, stop, r, w):
        return self.op("pe", lambda e: e.matmul(out=out, lhsT=lhsT, rhs=rhs, start=start, stop=stop), r, w)

    def tr(self, out, in_, ident, r, w):
        return self.op("pe", lambda e: e.transpose(out=out, in_=in_, identity=ident), r, w)

    def act(self, out, in_, func, r, w, scale=1.0, bias=None, accum=None):
        def f(e):
            kw = dict(out=out, in_=in_, func=func, scale=scale)
            if bias is not None:
                kw["bias"] = bias
            if accum is not None:
                kw["accum_out"] = accum
            return e.activation(**kw)
        return self.op("act", f, r, w)

    def tt(self, eng, out, a, b, op, r, w):
        return self.op(eng, lambda e: e.tensor_tensor(out=out, in0=a, in1=b, op=op), r, w)

    def ts(self, eng, out, a, s1, s2, op0, op1, r, w):
        if s2 is None:
            return self.op(eng, lambda e: e.tensor_scalar(out=out, in0=a, scalar1=s1, scalar2=None, op0=op0), r, w)
        return self.op(eng, lambda e: e.tensor_scalar(out=out, in0=a, scalar1=s1, scalar2=s2, op0=op0, op1=op1), r, w)

    def stt(self, out, a, s, b, op0, op1, r, w, accum=None):
        def f(e):
            kw = dict(out=out, in0=a, scalar=s, in1=b, op0=op0, op1=op1)
            if accum is not None:
                kw["accum_out"] = accum
            return e.scalar_tensor_tensor(**kw)
        return self.op("dve", f, r, w)

    def cp(self, eng, out, in_, r, w):
        if eng == "act":
            return self.op("act", lambda e: e.copy(out=out, in_=in_), r, w)
        return self.op(eng, lambda e: e.tensor_copy(out=out, in_=in_), r, w)

    def recip(self, out, in_, r, w):
        return self.op("dve", lambda e: e.reciprocal(out=out, in_=in_), r, w)

    def rsum(self, out, in_, r, w):
        return self.op("dve", lambda e: e.reduce_sum(out=out, in_=in_, axis=AX.X), r, w)

    def max8(self, out, in_, r, w):
        return self.op("dve", lambda e: e.max(out=out, in_=in_), r, w)

    def maxidx(self, out, in_max, in_values, r, w):
        return self.op("dve", lambda e: e.max_index(out=out, in_max=in_max, in_values=in_values), r, w)

    def matchrep(self, out, rep, vals, imm, r, w):
        return self.op("dve", lambda e: e.match_replace(out=out, in_to_replace=rep, in_values=vals, imm_value=imm), r, w)

    def memset(self, eng, ap, val, w):
        return self.op(eng, lambda e: e.memset(ap, val), (), w)

    def dma(self, out, in_, r, w):
        return self.op("sp", lambda e: e.dma_start(out=out, in_=in_), r, w)

    def rstd_from_ss(self, ss, rstd, tmp, nh, n, key):
        self.ts("dve", tmp, ss, 1.0 / n, 1e-6, ALU.mult, ALU.add, [key + "ss"], [key + "ms"])
        self.tt("pool", rstd, tmp, nh, ALU.pow, [key + "ms", "nh"], [key + "rstd"])

S = 2048
D = 2048
NT = 16
GC = 0.7978845608028654
SCALE = 128 ** -0.5
NEG = -30000.0
C_U, C_V, C_Q, C_KV, C_NG, C_MG = 0, 2048, 4096, 6144, 9216, 9264

CONST_SHAPES = {
    "c_cos": [128, S], "c_sin": [128, S], "c_rt": [128, 128], "c_caus": [128, 128], "c_winb": [128, 128],
    "c_cmpb": [128, S], "c_aagg": [128, 32], "c_am": [128, 16, 32], "c_add": [128, 16, 32],
    "c_esel": [32, 16, 128], "c_tril": [128, 16, 128], "c_ident": [128, 128], "c_iota16": [128, 16],
    "c_zrow": [128, 255],
}


def make_consts():
    c = {}
    half = 64
    inv = 10000.0 ** (-np.arange(half, dtype=np.float32) / half)
    ang = np.arange(S, dtype=np.float32)[:, None] * inv[None, :]
    cos = np.cos(ang).astype(np.float32).T
    sin = np.sin(ang).astype(np.float32).T
    c["c_cos"] = np.concatenate([cos, cos], 0)
    c["c_sin"] = np.concatenate([sin, sin], 0)
    rt = np.zeros((128, 128), np.float32)
    for m in range(128):
        if m < 64:
            rt[m + 64, m] = -1.0
        else:
            rt[m - 64, m] = 1.0
    c["c_rt"] = rt
    kk = np.arange(128)[:, None]
    qq = np.arange(128)[None, :]
    c["c_caus"] = np.where(kk <= qq, 0.0, NEG).astype(np.float32)
    c["c_winb"] = np.where(kk > qq, 0.0, NEG).astype(np.float32)
    n = np.arange(128)[:, None]
    q = np.arange(S)[None, :]
    c["c_cmpb"] = np.where((16 * n + 31 <= q) & (n < 127), 0.0, NEG).astype(np.float32)
    agg = np.array([1, 2, 2, 2, 1], np.float32)
    a = np.zeros((128, 32), np.float32)
    for nn in range(127):
        for j in range(32):
            w = nn + 1 - 4 * j
            if 0 <= w <= 4:
                a[nn, j] = agg[w]
    c["c_aagg"] = a
    pos = (np.arange(16)[None, :, None] * 128 + np.arange(128)[:, None, None])
    j = np.arange(32)[None, None, :]
    allowed = (j * 64) <= pos
    forced = (j == 0) | (j == (pos // 64))
    c["c_am"] = (allowed & ~forced).astype(np.float32)
    c["c_add"] = np.where(forced, 1e30, np.where(allowed, 0.0, -1e30)).astype(np.float32)
    e = np.zeros((32, 16, 128), np.float32)
    for kt in range(16):
        for key in range(128):
            e[2 * kt + key // 64, kt, key] = 1.0
    c["c_esel"] = e
    s_ = np.arange(128)[:, None, None]
    t_ = np.arange(128)[None, None, :]
    c["c_tril"] = np.broadcast_to((s_ <= t_), (128, 16, 128)).astype(np.float32).copy()
    c["c_ident"] = np.eye(128, dtype=np.float32)
    c["c_iota16"] = np.broadcast_to(np.arange(16, dtype=np.float32)[None, :], (128, 16)).copy()
    z = np.zeros((128, 255), np.float32)
    z[:, 127] = 1.0
    c["c_zrow"] = z
    return c


def build_program(stop_after=99, dbg=()):
    nc = bass.Bass("TRN2", target_bir_lowering=False)
    k = K(nc)

    def din(name, shape):
        return nc.dram_tensor(name, list(shape), F32, kind="ExternalInput").ap()

    def dscr(name, shape, dt=F32):
        kind = "ExternalOutput" if name in dbg else "Internal"
        return nc.dram_tensor(name, list(shape), dt, kind=kind).ap()

    x = din("x", [S, D])
    norm_mix_g = din("norm_mix_g", [1, D])
    w_in = din("w_in", [D, 13360])
    w_out = din("w_out", [D, D])
    gm_ln_g = din("gm_ln_g", [1, D])
    gm_ln_b = din("gm_ln_b", [1, D])
    gm_wT = din("gm_wT", [128, 16, 128])
    gm_bT = din("gm_bT", [128, 16])
    posT = {"k": din("cmp_posT_k", [128, 32]), "v": din("cmp_posT_v", [128, 32])}
    cw1 = {"k": din("cmp_w1_k", [4096, 256]), "v": din("cmp_w1_v", [4096, 256])}
    cw2 = {"k": din("cmp_w2_k", [256, 128]), "v": din("cmp_w2_v", [256, 128])}
    norm_ffn_g = din("norm_ffn_g", [1, D])
    peer_w_q = din("peer_w_q", [D, D])
    keysT = [din("keys1T", [128, 128]), din("keys2T", [128, 128])]
    NEXP = 16384 if stop_after >= 5 else 128
    peer_u = din("peer_u", [NEXP, D])
    peer_v = din("peer_v", [NEXP, D])
    norm_final_g = din("norm_final_g", [1, D])
    cin = {n: din(n, s) for n, s in CONST_SHAPES.items()}
    y = nc.dram_tensor("y", [S, D], F32, kind="ExternalOutput").ap()

    SU = dscr("s_u", [S, D]); SV = dscr("s_v", [S, D]); SVS = dscr("s_vs", [S, 512]); SVW = dscr("s_vw", [S, 512])
    SNG = dscr("s_ng", [S, 48]); SMG = dscr("s_mg", [S, 4096])
    SQ = dscr("s_q", [16, 128, S]); SKC = dscr("s_kc", [4, 128, S]); SVC = dscr("s_vc", [4, 128, S])
    SKS = dscr("s_ks", [4, 128, S]); SKW = dscr("s_kw", [4, 128, S])
    SMA = dscr("s_ma", [S, D]); SOB = dscr("s_ob", [S, D]); SX1 = dscr("s_x1", [S, D])
    SH2 = dscr("s_h2", [S, D], BF16); SS = dscr("s_ss", [S, 2048])

    PSALL = nc.alloc_psum_tensor("psall", [128, 4096], F32)

    def PS(b, n=1):
        return PSALL[:, b * 512:(b + n) * 512]

    def PSB(b):
        return PSALL[:, b * 512:(b + 1) * 512].bitcast(BF16)

    def psk(b, n=1):
        return ["ps%d" % i for i in range(b, b + n)]

    ident_f = k.sb([128, 128], F32, "identf")
    ident_b = k.sb([128, 128], BF16, "identb")
    ones_b = k.sb([128, 128], BF16, "onesb")
    nh = k.sb([128, 1], F32, "nh")
    k.dma(ident_f[:], cin["c_ident"], [], ["identf"])
    k.cp("dve", ident_b[:], ident_f[:], ["identf"], ["identb"])
    k.memset("pool", ones_b[:], 1.0, ["onesb"])
    k.memset("pool", nh[:], -0.5, ["nh"])

    def row(t):
        return slice(t * 128, (t + 1) * 128)

    k.mark()
    hT = k.sb([128, 16, S], BF16, "hT")
    k.mark()
    gb = k.sb([128, D], F32, "gb")
    xt = [k.sb([128, D], F32, "xt") for _ in range(2)]
    junk = k.sb([128, D], BF16, "junk")
    hb = [k.sb([128, D], BF16, "hb") for _ in range(2)]
    sm = [k.sb([128, 4], F32, "sm") for _ in range(2)]
    k.dma(gb[:], norm_mix_g.partition_broadcast(128), [], ["gb"])
    for t in range(NT):
        p = t % 2
        kp = "a%d" % p
        k.dma(xt[p][:], x[row(t), :], [], [kp + "xt"])
        k.act(junk[:], xt[p][:], AF.Square, [kp + "xt"], ["junk", kp + "ss"], accum=sm[p][:, 0:1])
        k.rstd_from_ss(sm[p][:, 0:1], sm[p][:, 2:3], sm[p][:, 1:2], nh[:], D, kp)
        k.stt(hb[p][:], xt[p][:], sm[p][:, 2:3], gb[:], ALU.mult, ALU.mult, [kp + "xt", kp + "rstd", "gb"], [kp + "hb"])
        for kc in range(16):
            b = 2 * p + kc // 8
            k.tr(PSB(b)[:, (kc % 8) * 128:(kc % 8 + 1) * 128], hb[p][:, kc * 128:(kc + 1) * 128], ident_b[:],
                 [kp + "hb", "identb"], psk(b))
        for hh in range(2):
            b = 2 * p + hh
            k.cp("dve" if hh == 0 else "act", hT[:, hh * 8:(hh + 1) * 8, row(t)],
                 PSB(b).rearrange("p (a b) -> p a b", b=128), psk(b), [("hT", t)])
    k.release()

    if stop_after >= 1:
        wf = [k.sb([128, 16, 512], F32, "wf") for _ in range(2)]
        wb = [k.sb([128, 16, 512], BF16, "wb") for _ in range(2)]
        ob = [k.sb([128, 512], F32, "ob") for _ in range(4)]
        chunks = []
        for c in range(4):
            chunks.append((C_U + c * 512, 512, "tm", (SU, c * 512)))
        for c in range(4):
            chunks.append((C_V + c * 512, 512, "tm", (SV, c * 512)))
        for c in range(4):
            chunks.append((C_Q + c * 512, 512, "fm", [SQ[4 * c + i] for i in range(4)]))
        chunks.append((C_KV + 0 * 512, 512, "fm", [SKC[i] for i in range(4)]))
        chunks.append((C_KV + 1 * 512, 512, "fm", [SVC[i] for i in range(4)]))
        chunks.append((C_KV + 2 * 512, 512, "fm", [SKS[i] for i in range(4)]))
        chunks.append((C_KV + 3 * 512, 512, "tm", (SVS, 0)))
        chunks.append((C_KV + 4 * 512, 512, "fm", [SKW[i] for i in range(4)]))
        chunks.append((C_KV + 5 * 512, 512, "tm", (SVW, 0)))
        chunks.append((C_NG, 48, "tm", (SNG, 0)))
        for c in range(8):
            chunks.append((C_MG + c * 512, 512, "tm", (SMG, c * 512)))

        def load_chunk(ci):
            c0, n, _, _ = chunks[ci]
            p = ci % 2
            k.dma(wf[p][:, :, 0:n], w_in[:, c0:c0 + n].rearrange("(kc p) n -> p kc n", p=128), [], [("wf", p)])

        cnt = 0
        load_chunk(0)
        for ci, (c0, n, kind, dest) in enumerate(chunks):
            p = ci % 2
            if ci + 1 < len(chunks):
                load_chunk(ci + 1)
            k.cp("dve", wb[p][:, 0:8, 0:n], wf[p][:, 0:8, 0:n], [("wf", p)], [("wb", p)])
            k.cp("act", wb[p][:, 8:16, 0:n], wf[p][:, 8:16, 0:n], [("wf", p)], [("wb", p)])
            for u in range(16):
                b = 2 + cnt % 6
                o = ob[cnt % 4]
                okey = ("ob", cnt % 4)
                if kind == "tm":
                    t = u
                    for kc in range(16):
                        k.mm(PS(b)[:, 0:n], hT[:, kc, row(t)], wb[p][:, kc, 0:n], kc == 0, kc == 15,
                             [("hT", t), ("wb", p)], psk(b))
                    dst = dest[0][row(t), dest[1]:dest[1] + n]
                else:
                    blk, tc = u // 4, u % 4
                    for kc in range(16):
                        k.mm(PS(b)[:, 0:512], wb[p][:, kc, blk * 128:(blk + 1) * 128], hT[:, kc, tc * 512:(tc + 1) * 512],
                             kc == 0, kc == 15, [("hT", 4 * tc + i) for i in range(4)] + [("wb", p)], psk(b))
                    dst = dest[blk][:, tc * 512:(tc + 1) * 512]
                k.cp("act" if cnt % 2 == 0 else "dve", o[:, 0:n], PS(b)[:, 0:n], psk(b), [okey])
                k.dma(dst, o[:, 0:n], [okey], [("scr", ci, u)])
                cnt += 1
    k.release()
    k.barrier()

    if stop_after >= 2:
        k.mark()
        gam = k.sb([128, D], F32, "gam")
        bet = k.sb([128, D], F32, "bet")
        BS = k.sb([128, 16, 128], F32, "BS")
        WTm = k.sb([128, 16, 128], BF16, "WTm")
        bsT = k.sb([128, 16], F32, "bsT")
        k.mark()
        st1 = k.sb([128, 16, 128], F32, "st1")
        st2 = k.sb([128, 16, 128], F32, "st2")
        k.dma(gam[:], gm_ln_g.partition_broadcast(128), [], ["gam"])
        k.dma(bet[:], gm_ln_b.partition_broadcast(128), [], ["bet"])
        k.dma(bsT[:], gm_bT, [], ["bsT"])
        k.dma(st1[:], gm_wT, [], ["st1"])
        k.dma(st2[:], cin["c_tril"], [], ["st2"])
        k.tt("dve", WTm[:], st1[:], st2[:], ALU.mult, ["st1", "st2"], ["WTm"])
        k.cp("dve", BS[:], bsT[:].unsqueeze(2).to_broadcast([128, 16, 128]), ["bsT"], ["BS"])
        k.release()
        ut = [k.sb([128, D], F32, "ut") for _ in range(2)]
        vt = [k.sb([128, D], F32, "vt") for _ in range(2)]
        gt = [k.sb([128, D], F32, "gt") for _ in range(2)]
        tA = k.sb([128, D], F32, "tA")
        tB = k.sb([128, D], F32, "tB")
        Uh = k.sb([128, D], F32, "Uh")
        G2 = k.sb([128, D], F32, "G2")
        vln = k.sb([128, D], BF16, "vln")
        mo = [k.sb([128, D], F32, "mo") for _ in range(2)]
        sm2 = [k.sb([128, 8], F32, "sm2") for _ in range(2)]
        BSf = BS[:].rearrange("p a b -> p (a b)")

        def load2(t):
            p = t % 2
            k.dma(ut[p][:], SU[row(t), :], [], [("ut", p)])
            k.dma(vt[p][:], SV[row(t), :], [], [("vt", p)])
            k.dma(gt[p][:], SMG[row(t), 0:D], [], [("gt", p)])

        def tanh_inner(src, skey):
            k.act(tA[:], src, AF.Square, [skey], ["tA"])
            k.ts("dve", tA[:], tA[:], 0.044715, 1.0, ALU.mult, ALU.add, ["tA"], ["tA"])
            k.tt("dve", tA[:], tA[:], src, ALU.mult, ["tA", skey], ["tA"])
            k.act(tB[:], tA[:], AF.Tanh, ["tA"], ["tB"], scale=GC)

        load2(0)
        for t in range(NT):
            p = t % 2
            if t + 1 < NT:
                load2(t + 1)
            s = sm2[p]
            tanh_inner(ut[p][:], ("ut", p))
            k.ts("dve", tB[:], tB[:], 0.25, 0.25, ALU.mult, ALU.add, ["tB"], ["tB"])
            k.tt("dve", Uh[:], tB[:], ut[p][:], ALU.mult, ["tB", ("ut", p)], ["Uh"])
            tanh_inner(vt[p][:], ("vt", p))
            k.stt(G2[:], tB[:], 1.0, vt[p][:], ALU.add, ALU.mult, ["tB", ("vt", p)], ["G2", ("s1", p)], accum=s[:, 0:1])
            k.act(tA[:], G2[:], AF.Square, ["G2"], ["tA", ("s2", p)], accum=s[:, 1:2])
            k.ts("dve", s[:, 2:3], s[:, 0:1], 1.0 / D, None, ALU.mult, None, [("s1", p)], [("mean2", p)])
            k.tt("dve", s[:, 3:4], s[:, 2:3], s[:, 2:3], ALU.mult, [("mean2", p)], [("msq", p)])
            k.stt(s[:, 4:5], s[:, 1:2], 1.0 / D, s[:, 3:4], ALU.mult, ALU.subtract, [("s2", p), ("msq", p)], [("var4", p)])
            k.ts("dve", s[:, 5:6], s[:, 4:5], 0.25, 1e-6, ALU.mult, ALU.add, [("var4", p)], [("v4", p)])
            k.tt("pool", s[:, 6:7], s[:, 5:6], nh[:], ALU.pow, [("v4", p), "nh"], [("rs", p)])
            k.ts("dve", s[:, 7:8], s[:, 6:7], 0.5, None, ALU.mult, None, [("rs", p)], [("rsh", p)])
            k.ts("dve", tA[:], G2[:], s[:, 2:3], s[:, 7:8], ALU.subtract, ALU.mult, ["G2", ("mean2", p), ("rsh", p)], ["tA"])
            k.tt("dve", tA[:], tA[:], gam[:], ALU.mult, ["tA", "gam"], ["tA"])
            k.tt("dve", vln[:], tA[:], bet[:], ALU.add, ["tA", "bet"], ["vln"])
            b0 = 4 * p
            for g in range(16):
                b = b0 + g // 4
                k.mm(PS(b)[:, (g % 4) * 128:(g % 4 + 1) * 128], WTm[:, g, :], vln[:, g * 128:(g + 1) * 128], True, True,
                     ["WTm", "vln"], psk(b))
            k.tt("dve", tA[:], PS(b0, 4), BSf, ALU.add, psk(b0, 4) + ["BS"], ["tA"])
            k.tt("dve", tA[:], tA[:], Uh[:], ALU.mult, ["tA", "Uh"], ["tA"])
            k.act(tB[:], gt[p][:], AF.Tanh, [("gt", p)], ["tB"], scale=0.5)
            k.stt(mo[p][:], tB[:], 1.0, tA[:], ALU.add, ALU.mult, ["tB", "tA"], [("mo", p)])
            k.dma(SMA[row(t), :], mo[p][:], [("mo", p)], [("sma", t)])
        k.release()
        k.barrier()

    if stop_after >= 3:
        k.mark()
        cosT = k.sb([128, S], F32, "cosT"); sinT = k.sb([128, S], F32, "sinT")
        RT = k.sb([128, 128], F32, "RT")
        CAUS = k.sb([128, 128], BF16, "CAUS"); WINB = k.sb([128, 128], BF16, "WINB")
        CMPB = k.sb([128, S], BF16, "CMPB")
        Aagg = k.sb([128, 32], F32, "Aagg")
        AM = k.sb([128, 16, 32], F32, "AM"); ADDM = k.sb([128, 16, 32], F32, "ADDM")
        ESEL = k.sb([32, 16, 128], BF16, "ESEL")
        w1b = {m: k.sb([128, 32, 256], BF16, "w1b") for m in "kv"}
        w2b = {m: k.sb([128, 2, 128], BF16, "w2b") for m in "kv"}
        posb = {m: k.sb([128, 32], BF16, "posb") for m in "kv"}
        cvec = {m: k.sb([128, 2], F32, "cvec") for m in "kv"}
        stg = k.sb([128, 2 * S], F32, "stg")
        kcb = k.sb([128, S], BF16, "kcb"); vcb = k.sb([128, S], BF16, "vcb")
        ksr = k.sb([128, S], BF16, "ksr"); kwr = k.sb([128, S], BF16, "kwr")
        vsa = k.sb([128, 16, 129], BF16, "vsa"); vwa = k.sb([128, 16, 129], BF16, "vwa")
        qb = [k.sb([128, S], BF16, "qb") for _ in range(4)]
        qr = [k.sb([128, S], BF16, "qr") for _ in range(4)]
        sgr = k.sb([128, 16, 12], F32, "sgr"); sg = k.sb([128, 16, 12], F32, "sg")
        kcT = k.sb([128, 128], BF16, "kcT"); vca = k.sb([128, 130], BF16, "vca")
        hid = k.sb([128, 2, 128], BF16, "hid")
        hx = k.sb([128, 128], F32, "hx"); hy = k.sb([128, 128], F32, "hy"); hz = k.sb([128, 128], F32, "hz")
        biasT = k.sb([32, S], BF16, "biasT")
        PcT = [k.sb([128, 512], BF16, "PcT") for _ in range(2)]
        pn = k.sb([128, 512], F32, "pn"); rZ = [k.sb([128, 512], F32, "rZ") for _ in range(2)]; impS = k.sb([32, 512], F32, "impS")
        sc = k.sb([128, 128], F32, "sc"); selb = k.sb([128, 128], F32, "selb"); wk32 = k.sb([128, 32], F32, "wk32")
        m8 = k.sb([128, 16], F32, "m8")
        accp = [k.sb([128, 4, 4, 128], F32, "acc") for _ in range(2)]
        PT = [k.sb([128, 512], BF16, "PT") for _ in range(3)]
        t1 = [k.sb([128, 512], F32, "t1") for _ in range(2)]
        t2 = [k.sb([128, 512], F32, "t2") for _ in range(2)]
        czs = [k.sb([128, 2], F32, "cz") for _ in range(4)]

        def stgv(b):
            return stg[:, b * S:(b + 1) * S]

        k.dma(cosT[:], cin["c_cos"], [], ["cosT"]); k.dma(sinT[:], cin["c_sin"], [], ["sinT"])
        k.dma(RT[:], cin["c_rt"], [], ["RT"]); k.dma(Aagg[:], cin["c_aagg"], [], ["Aagg"])
        k.dma(AM[:], cin["c_am"], [], ["AM"]); k.dma(ADDM[:], cin["c_add"], [], ["ADDM"])
        k.dma(stgv(0), cin["c_cmpb"], [], [("stg", 0)])
        k.cp("dve", CMPB[:], stgv(0), [("stg", 0)], ["CMPB"])
        k.dma(stg[:, S:S + 128], cin["c_caus"], [], [("stg", 1)])
        k.dma(stg[:, S + 128:S + 256], cin["c_winb"], [], [("stg", 1)])
        k.cp("dve", CAUS[:], stg[:, S:S + 128], [("stg", 1)], ["CAUS"])
        k.cp("dve", WINB[:], stg[:, S + 128:S + 256], [("stg", 1)], ["WINB"])
        k.dma(stg[0:32, 0:S].rearrange("p (a b) -> p a b", b=128), cin["c_esel"], [("stg", 0)], [("stg", 0)])
        k.cp("dve", ESEL[:], stg[0:32, 0:S].rearrange("p (a b) -> p a b", b=128), [("stg", 0)], ["ESEL"])
        for m in "kv":
            for hf in range(2):
                k.dma(stg[:].rearrange("p (l n) -> p l n", n=256),
                      cw1[m][hf * 2048:(hf + 1) * 2048, :].rearrange("(l d) n -> d l n", d=128), [], [("stg", 0), ("stg", 1)])
                k.cp("dve" if hf == 0 else "act", w1b[m][:, hf * 16:(hf + 1) * 16, :], stg[:].rearrange("p (l n) -> p l n", n=256),
                     [("stg", 0), ("stg", 1)], [("w1b", m)])
            k.dma(stg[:, 0:256].rearrange("p (h d) -> p h d", d=128), cw2[m].rearrange("(h p) d -> p h d", p=128), [], [("stg", 0)])
            k.cp("dve", w2b[m][:], stg[:, 0:256].rearrange("p (h d) -> p h d", d=128), [("stg", 0)], [("w2b", m)])
            k.dma(stg[:, S:S + 32], posT[m], [], [("stg", 1)])
            k.cp("dve", posb[m][:], stg[:, S:S + 32], [("stg", 1)], [("posb", m)])
            for hf in range(2):
                for l in range(32):
                    k.mm(PS(7)[:, hf:hf + 1], w1b[m][:, l, hf * 128:(hf + 1) * 128], posb[m][:, l:l + 1], l == 0, l == 31,
                         [("w1b", m), ("posb", m)], psk(7))
            k.cp("dve", cvec[m][:], PS(7)[:, 0:2], psk(7), [("cvec", m)])
        k.memset("pool", vsa[:, :, 128:129], 1.0, ["vsa"])
        k.memset("pool", vwa[:, :, 128:129], 1.0, ["vwa"])
        k.memset("pool", vca[:, 128:129], 1.0, ["vca"])

        rot_i = [0]

        def rotary(src, skey, dst, dkey):
            for tc in range(4):
                i = rot_i[0] % 2
                rot_i[0] += 1
                sl = slice(tc * 512, (tc + 1) * 512)
                b = 4 + i
                k.mm(PS(b), RT[:], src[:, sl], True, True, [skey, "RT"], psk(b))
                k.tt("dve", t1[i][:], PS(b), sinT[:, sl], ALU.mult, psk(b) + ["sinT"], [("t1", i)])
                k.tt("pool", t2[i][:], src[:, sl], cosT[:, sl], ALU.mult, [skey, "cosT"], [("t2", i)])
                k.tt("dve", dst[:, sl], t1[i][:], t2[i][:], ALU.add, [("t1", i), ("t2", i)], [dkey])

        def compress(m, src, skey):
            for hf in range(2):
                b = 4 + hf
                for l in range(32):
                    k.mm(PS(b)[:, 0:127], w1b[m][:, l, hf * 128:(hf + 1) * 128], src[:, l:l + 16 * 126 + 1:16], l == 0, l == 31,
                         [("w1b", m), skey], psk(b))
                k.ts("dve", hx[:, 0:127], PS(b)[:, 0:127], cvec[m][:, hf:hf + 1], None, ALU.add, None, psk(b) + [("cvec", m)], ["hx"])
                k.act(hy[:, 0:127], hx[:, 0:127], AF.Square, ["hx"], ["hy"])
                k.ts("dve", hy[:, 0:127], hy[:, 0:127], 0.044715, 1.0, ALU.mult, ALU.add, ["hy"], ["hy"])
                k.tt("dve", hy[:, 0:127], hy[:, 0:127], hx[:, 0:127], ALU.mult, ["hy", "hx"], ["hy"])
                k.act(hz[:, 0:127], hy[:, 0:127], AF.Tanh, ["hy"], ["hz"], scale=GC)
                k.ts("dve", hz[:, 0:127], hz[:, 0:127], 0.5, 0.5, ALU.mult, ALU.add, ["hz"], ["hz"])
                k.tt("dve", hid[:, hf, 0:127], hz[:, 0:127], hx[:, 0:127], ALU.mult, ["hz", "hx"], ["hid"])
            if m == "k":
                for hf in range(2):
                    k.mm(PS(6)[:, 0:127], w2b[m][:, hf, :], hid[:, hf, 0:127], hf == 0, hf == 1, [("w2b", m), "hid"], psk(6))
                k.cp("act", kcT[:, 0:127], PS(6)[:, 0:127], psk(6), ["kcT"])
            else:
                for hf in range(2):
                    k.mm(PS(6)[0:127, 0:128], hid[:, hf, 0:127], w2b[m][:, hf, :], hf == 0, hf == 1, [("w2b", m), "hid"], psk(6))
                k.cp("act", vca[0:127, 0:128], PS(6)[0:127, 0:128], psk(6), ["vca"])

        sb_i = [0]
        ob_i = [0]
        pt_i = [0]
        cz_i = [0]
        OBANKS = [2, 3, 7]

        def attn(kT, kkey, va, vkey, qrt, qkey, qt, ktl, use_sel):
            obk = OBANKS[ob_i[0] % 3]
            ob_i[0] += 1
            nk = len(ktl)
            done = 0
            for g0 in range(0, nk, 4):
                grp = ktl[g0:g0 + 4]
                sbk = sb_i[0] % 2
                sb_i[0] += 1
                for j, (kt, eb) in enumerate(grp):
                    out = PS(sbk)[:, j * 128:(j + 1) * 128]
                    nmm = 1 + (1 if use_sel else 0) + (1 if eb else 0)
                    k.mm(out, kT[:, kt * 128:(kt + 1) * 128], qrt[:, row(qt)], True, nmm == 1, [kkey, qkey], psk(sbk))
                    i = 1
                    if use_sel:
                        i += 1
                        k.mm(out, ESEL[0:32, kt, :], biasT[0:32, row(qt)], False, i == nmm, ["ESEL", "biasT"], psk(sbk))
                    if eb:
                        k.mm(out, ident_b[:], (CAUS if eb == "caus" else WINB)[:], False, True, ["identb", "CAUS", "WINB"], psk(sbk))
                n = len(grp) * 128
                pi = pt_i[0] % 3
                pt_i[0] += 1
                k.act(PT[pi][:, 0:n], PS(sbk)[:, 0:n], AF.Exp, psk(sbk), [("PT", pi)], scale=SCALE)
                for j, (kt, eb) in enumerate(grp):
                    k.mm(PS(obk)[:, 0:129], PT[pi][:, j * 128:(j + 1) * 128], va[:, kt, :], done == 0, done == nk - 1,
                         [("PT", pi), vkey], psk(obk))
                    done += 1
            return obk

        def combine(obk, qt, qtl, g, br, ap, akey, first, guard):
            cz = czs[cz_i[0] % 4]
            ck = ("cz", cz_i[0] % 4)
            cz_i[0] += 1
            if guard:
                k.ts("dve", cz[:, 0:1], PS(obk)[:, 128:129], 1e-30, None, ALU.max, None, psk(obk), [ck])
                k.recip(cz[:, 0:1], cz[:, 0:1], [ck], [ck])
            else:
                k.recip(cz[:, 0:1], PS(obk)[:, 128:129], psk(obk), [ck])
            gate = sg[:, qt, g * 3 + br:g * 3 + br + 1]
            if first:
                k.ts("dve", ap[:, qtl, g, :], PS(obk)[:, 0:128], cz[:, 0:1], gate, ALU.mult, ALU.mult, psk(obk) + [ck, "sg"], [akey])
            else:
                k.tt("dve", cz[:, 1:2], cz[:, 0:1], gate, ALU.mult, [ck, "sg"], [ck])
                k.stt(ap[:, qtl, g, :], PS(obk)[:, 0:128], cz[:, 1:2], ap[:, qtl, g, :], ALU.mult, ALU.add, psk(obk) + [ck, akey], [akey])

        for hk in range(4 if stop_after >= 3 else 0):
            si = [0]

            def nxt():
                b = si[0] % 2
                si[0] += 1
                return b
            for nm, src, dst in (("kcb", SKC[hk], kcb), ("vcb", SVC[hk], vcb)):
                b = nxt()
                k.dma(stgv(b), src, [], [("stg", b)])
                k.cp("act", dst[:], stgv(b), [("stg", b)], [nm])
            for nm, src, dst in (("ksr", SKS[hk], ksr), ("kwr", SKW[hk], kwr)):
                b = nxt()
                k.dma(stgv(b), src, [], [("stg", b)])
                rotary(stgv(b), ("stg", b), dst, nm)
            for g in range(4):
                b = nxt()
                k.dma(stgv(b), SQ[4 * hk + g], [], [("stg", b)])
                k.cp("act", qb[g][:], stgv(b), [("stg", b)], [("qb", g)])
                rotary(stgv(b), ("stg", b), qr[g], ("qr", g))
            for nm, src, dst in (("vsa", SVS, vsa), ("vwa", SVW, vwa)):
                b = nxt()
                k.dma(stgv(b).rearrange("p (t d) -> p t d", d=128),
                      src[:, hk * 128:(hk + 1) * 128].rearrange("(t p) d -> p t d", p=128), [], [("stg", b)])
                k.cp("dve", dst[:, :, 0:128], stgv(b).rearrange("p (t d) -> p t d", d=128), [("stg", b)], [nm])
            k.dma(sgr[:], SNG[:, hk * 12:(hk + 1) * 12].rearrange("(t p) c -> p t c", p=128), [], ["sgr"])
            k.act(sg[:], sgr[:], AF.Tanh, ["sgr"], ["sg"], scale=0.5)
            k.ts("dve", sg[:], sg[:], 0.5, 0.5, ALU.mult, ALU.add, ["sg"], ["sg"])
            compress("k", kcb, "kcb")
            compress("v", vcb, "vcb")

            for qc in range(4):
                qsl = slice(qc * 512, (qc + 1) * 512)
                par = qc % 2
                ap = accp[par]
                def cmp_a(g):
                    pc = PcT[g % 2]
                    pk = ("pc", g % 2)
                    k.mm(PS(5)[0:127, :], kcT[:, 0:127], qb[g][:, qsl], True, False, ["kcT", ("qb", g)], psk(5))
                    k.mm(PS(5)[0:127, :], ident_b[0:127, 0:127], CMPB[0:127, qsl], False, True, ["identb", "CMPB"], psk(5))
                    k.act(pc[0:127, :], PS(5)[0:127, :], AF.Exp, psk(5), [pk], scale=SCALE)
                    k.mm(PS(5), ones_b[0:127, :], pc[0:127, :], True, True, ["onesb", pk], psk(5))
                    k.ts("dve", rZ[g % 2][:], PS(5), 1e-30, None, ALU.max, None, psk(5), [("rZ", g % 2)])

                def cmp_b(g):
                    pc = PcT[g % 2]
                    pk = ("pc", g % 2)
                    rz = rZ[g % 2]
                    rk = ("rZ", g % 2)
                    k.recip(rz[:], rz[:], [rk], [rk])
                    k.tt("dve", pn[0:127, :], pc[0:127, :], rz[0:127, :], ALU.mult, [pk, rk], ["pn"])
                    k.mm(PS(6)[0:32, :], Aagg[0:127, :], pn[0:127, :], g == 0, g == 3, ["Aagg", "pn"], psk(6))
                    for qtl in range(4):
                        qt = qc * 4 + qtl
                        obk = OBANKS[ob_i[0] % 3]
                        ob_i[0] += 1
                        k.mm(PS(obk)[:, 0:129], pc[0:127, qtl * 128:(qtl + 1) * 128], vca[0:127, 0:129], True, True, [pk, "vca"], psk(obk))
                        combine(obk, qt, qtl, g, 0, ap, ("acc", par, qtl), True, True)

                cmp_a(0)
                for g in range(4):
                    if g + 1 < 4:
                        cmp_a(g + 1)
                    cmp_b(g)
                k.cp("act", impS[0:32, :], PS(6)[0:32, :], psk(6), ["impS"])
                for qtl in range(4):
                    k.tr(PS(5)[:, qtl * 32:(qtl + 1) * 32], impS[0:32, qtl * 128:(qtl + 1) * 128], ident_f[0:32, 0:32],
                         ["impS", "identf"], psk(5))
                sc3 = sc[:].rearrange("p (a b) -> p a b", b=32)
                k.tt("dve", sc3, PS(5)[:, 0:128].rearrange("p (a b) -> p a b", b=32), AM[:, qc * 4:(qc + 1) * 4, :], ALU.mult,
                     psk(5) + ["AM"], ["sc"])
                k.tt("dve", sc3, sc3, ADDM[:, qc * 4:(qc + 1) * 4, :], ALU.add, ["sc", "ADDM"], ["sc"])
                for qtl in range(4):
                    ssl = sc[:, qtl * 32:(qtl + 1) * 32]
                    k.max8(m8[:, 0:8], ssl, ["sc"], ["m8"])
                    k.matchrep(wk32[:], m8[:, 0:8], ssl, -3e38, ["sc", "m8"], ["wk32"])
                    k.max8(m8[:, 8:16], wk32[:], ["wk32"], ["m8"])
                    k.ts("dve", selb[:, qtl * 32:(qtl + 1) * 32], ssl, m8[:, 15:16], -NEG, ALU.is_ge, ALU.mult, ["sc", "m8"], ["selb"])
                k.ts("dve", selb[:], selb[:], NEG, None, ALU.add, None, ["selb"], ["selb"])
                for qtl in range(4):
                    k.tr(PS(6)[0:32, qtl * 128:(qtl + 1) * 128], selb[:, qtl * 32:(qtl + 1) * 32], ident_f[:], ["selb", "identf"], psk(6))
                k.cp("act", biasT[0:32, qsl], PS(6)[0:32, :], psk(6), ["biasT"])
                branches = []
                for qtl in range(4):
                    qt = qc * 4 + qtl
                    for g in range(4):
                        ktl = [(kt, "caus" if kt == qt else None) for kt in range(qt + 1)]
                        branches.append(dict(kT=ksr, kkey="ksr", va=vsa, vkey="vsa", g=g, qt=qt, qtl=qtl, ktl=ktl, sel=True, br=1, last=False))
                        ktl = [(kt, "caus" if kt == qt else ("winb" if kt == qt - 4 else None)) for kt in range(max(0, qt - 4), qt + 1)]
                        branches.append(dict(kT=kwr, kkey="kwr", va=vwa, vkey="vwa", g=g, qt=qt, qtl=qtl, ktl=ktl, sel=False, br=2, last=(g == 3)))
                groups = []
                for bi, B in enumerate(branches):
                    nk = len(B["ktl"])
                    for g0 in range(0, nk, 4):
                        groups.append(dict(b=bi, kts=B["ktl"][g0:g0 + 4], first=(g0 == 0), lastg=(g0 + 4 >= nk), base=g0))

                def emit_scores(G_):
                    B = branches[G_["b"]]
                    sbk = (0, 1, 4)[sb_i[0] % 3]
                    sb_i[0] += 1
                    G_["sbk"] = sbk
                    qt, g = B["qt"], B["g"]
                    for j, (kt, eb) in enumerate(G_["kts"]):
                        out = PS(sbk)[:, j * 128:(j + 1) * 128]
                        nmm = 1 + (1 if B["sel"] else 0) + (1 if eb else 0)
                        k.mm(out, B["kT"][:, kt * 128:(kt + 1) * 128], qr[g][:, row(qt)], True, nmm == 1, [B["kkey"], ("qr", g)], psk(sbk))
                        i = 1
                        if B["sel"]:
                            i += 1
                            k.mm(out, ESEL[0:32, kt, :], biasT[0:32, row(qt)], False, i == nmm, ["ESEL", "biasT"], psk(sbk))
                        if eb:
                            k.mm(out, ident_b[:], (CAUS if eb == "caus" else WINB)[:], False, True, ["identb", "CAUS", "WINB"], psk(sbk))

                def emit_exp_pv(G_):
                    B = branches[G_["b"]]
                    sbk = G_["sbk"]
                    if G_["first"]:
                        B["obk"] = OBANKS[ob_i[0] % 3]
                        ob_i[0] += 1
                    obk = B["obk"]
                    n = len(G_["kts"]) * 128
                    pi = pt_i[0] % 3
                    pt_i[0] += 1
                    k.act(PT[pi][:, 0:n], PS(sbk)[:, 0:n], AF.Exp, psk(sbk), [("PT", pi)], scale=SCALE)
                    nk = len(B["ktl"])
                    for j, (kt, eb) in enumerate(G_["kts"]):
                        idx = G_["base"] + j
                        k.mm(PS(obk)[:, 0:129], PT[pi][:, j * 128:(j + 1) * 128], B["va"][:, kt, :], idx == 0, idx == nk - 1,
                             [("PT", pi), B["vkey"]], psk(obk))
                    if G_["lastg"]:
                        akey = ("acc", par, B["qtl"])
                        combine(obk, B["qt"], B["qtl"], B["g"], B["br"], ap, akey, False, False)
                        if B["last"]:
                            k.dma(SOB[row(B["qt"]), hk * 512:(hk + 1) * 512], ap[:, B["qtl"]].rearrange("p g d -> p (g d)"), [akey],
                                  [("sob", hk, B["qt"])])

                emit_scores(groups[0])
                if len(groups) > 1:
                    emit_scores(groups[1])
                for gi_ in range(len(groups)):
                    if gi_ + 2 < len(groups):
                        emit_scores(groups[gi_ + 2])
                    emit_exp_pv(groups[gi_])
        k.release()
        k.barrier()

    if stop_after >= 4:
        k.mark()
        wo = k.sb([128, 16, D], BF16, "wo")
        k.mark()
        stg4 = k.sb([128, 16, 512], F32, "stg4")
        for c in range(4):
            k.dma(stg4[:], w_out[:, c * 512:(c + 1) * 512].rearrange("(kc p) n -> p kc n", p=128), [], ["stg4"])
            k.cp("dve", wo[:, 0:8, c * 512:(c + 1) * 512], stg4[:, 0:8, :], ["stg4"], ["wo"])
            k.cp("act", wo[:, 8:16, c * 512:(c + 1) * 512], stg4[:, 8:16, :], ["stg4"], ["wo"])
        k.release()
        obt = [k.sb([128, D], F32, "obt") for _ in range(2)]
        mat = [k.sb([128, D], F32, "mat") for _ in range(2)]
        gbt = [k.sb([128, D], F32, "gbt") for _ in range(2)]
        xtt = [k.sb([128, D], F32, "xtt") for _ in range(2)]
        mb = k.sb([128, D], BF16, "mb")
        mT = k.sb([128, 16, 128], BF16, "mT")
        x1o = [k.sb([128, D], F32, "x1o") for _ in range(2)]

        def load4(t):
            p = t % 2
            k.dma(obt[p][:], SOB[row(t), :], [], [("obt", p)])
            k.dma(mat[p][:], SMA[row(t), :], [], [("mat", p)])
            k.dma(gbt[p][:], SMG[row(t), D:2 * D], [], [("gbt", p)])
            k.dma(xtt[p][:], x[row(t), :], [], [("xtt", p)])

        load4(0)
        for t in range(NT):
            p = t % 2
            if t + 1 < NT:
                load4(t + 1)
            k.act(gbt[p][:], gbt[p][:], AF.Tanh, [("gbt", p)], [("gbt", p)], scale=0.5)
            k.ts("dve", gbt[p][:], gbt[p][:], 0.5, 0.5, ALU.mult, ALU.add, [("gbt", p)], [("gbt", p)])
            k.tt("dve", obt[p][:], obt[p][:], gbt[p][:], ALU.mult, [("obt", p), ("gbt", p)], [("obt", p)])
            k.tt("dve", mb[:], obt[p][:], mat[p][:], ALU.add, [("obt", p), ("mat", p)], ["mb"])
            for kc in range(16):
                b = kc // 8
                k.tr(PSB(b)[:, (kc % 8) * 128:(kc % 8 + 1) * 128], mb[:, kc * 128:(kc + 1) * 128], ident_b[:], ["mb", "identb"], psk(b))
            for hh in range(2):
                k.cp("dve" if hh == 0 else "act", mT[:, hh * 8:(hh + 1) * 8, :], PSB(hh).rearrange("p (a b) -> p a b", b=128),
                     psk(hh), ["mT"])
            for c in range(4):
                b = 2 + c
                for kc in range(16):
                    k.mm(PS(b), mT[:, kc, :], wo[:, kc, c * 512:(c + 1) * 512], kc == 0, kc == 15, ["mT", "wo"], psk(b))
                k.tt("dve", x1o[p][:, c * 512:(c + 1) * 512], PS(b), xtt[p][:, c * 512:(c + 1) * 512], ALU.add,
                     psk(b) + [("xtt", p)], [("x1o", p)])
            k.dma(SX1[row(t), :], x1o[p][:], [("x1o", p)], [("sx1", t)])
        k.release()
        k.barrier()

    if stop_after >= 5:
        k.mark()
        eidT_all = k.sb([128, 16, 128], I32, "eidTall")
        gateT_all = k.sb([128, 16, 128], F32, "gateTall")
        iota16 = k.sb([128, 16], F32, "iota16")
        k.dma(iota16[:], cin["c_iota16"], [], ["iota16"])
        k.mark()
        wq = k.sb([128, 16, D], BF16, "wq")
        k.mark()
        stg5 = k.sb([128, 16, 512], F32, "stg5")
        for c in range(4):
            k.dma(stg5[:], peer_w_q[:, c * 512:(c + 1) * 512].rearrange("(kc p) n -> p kc n", p=128), [], ["stg5"])
            k.cp("dve", wq[:, 0:8, c * 512:(c + 1) * 512], stg5[:, 0:8, :], ["stg5"], ["wq"])
            k.cp("act", wq[:, 8:16, c * 512:(c + 1) * 512], stg5[:, 8:16, :], ["stg5"], ["wq"])
        k.release()
        kTf = [k.sb([128, 128], F32, "kTf") for _ in range(2)]
        g2b = k.sb([128, D], F32, "g2b")
        x1a = [k.sb([128, D], F32, "x1a") for _ in range(2)]
        h2a = [k.sb([128, D], BF16, "h2a") for _ in range(2)]
        jk = k.sb([128, D], BF16, "jk")
        h2T = k.sb([128, 16, 128], BF16, "h2T")
        qTs = k.sb([128, 16, 128], F32, "qTs")
        Sxa = [k.sb([128, 16, 128], F32, "Sxa") for _ in range(2)]
        sm5 = [k.sb([128, 4], F32, "sm5") for _ in range(2)]
        cand = k.sb([128, 8, 16, 16], F32, "cand"); eqt = k.sb([128, 8, 16, 16], F32, "eqt")
        V16 = k.sb([128, 16, 16], F32, "V16"); I16u = k.sb([128, 16, 16], U32, "I16u"); I16f = k.sb([128, 16, 16], F32, "I16f")
        wk = k.sb([128, 128], F32, "wk"); wk2 = k.sb([128, 256], F32, "wk2")
        T16 = k.sb([128, 8, 16], F32, "T16"); P16u = k.sb([128, 8, 16], U32, "P16u")
        A16u = k.sb([128, 8, 16], U32, "A16u"); B16u = k.sb([128, 8, 16], U32, "B16u")
        af = k.sb([128, 8, 16], F32, "af"); bf = k.sb([128, 8, 16], F32, "bf")
        ex = k.sb([128, 8, 16], F32, "ex"); gate = k.sb([128, 8, 16], F32, "gate")
        zs = k.sb([128, 8], F32, "zs")
        i1f = k.sb([128, 8, 16], F32, "i1f"); i2f = k.sb([128, 8, 16], F32, "i2f"); eid = k.sb([128, 8, 16], F32, "eid")
        io_b = iota16[:].unsqueeze(1).unsqueeze(1).to_broadcast([128, 8, 16, 16])
        V16v = V16[:].rearrange("p (h two) a -> p h two a", two=2)
        I16v = I16f[:].rearrange("p (h two) a -> p h two a", two=2)
        k.dma(kTf[0][:], keysT[0], [], ["kTf"]); k.dma(kTf[1][:], keysT[1], [], ["kTf"])
        k.dma(g2b[:], norm_ffn_g.partition_broadcast(128), [], ["g2b"])
        def front(t):
            p = t % 2
            kp = "n5%d" % p
            k.dma(x1a[p][:], SX1[row(t), :], [], [("x1a", p)])
            k.act(jk[:], x1a[p][:], AF.Square, [("x1a", p)], ["jk", kp + "ss"], accum=sm5[p][:, 0:1])
            k.rstd_from_ss(sm5[p][:, 0:1], sm5[p][:, 2:3], sm5[p][:, 1:2], nh[:], D, kp)
            k.stt(h2a[p][:], x1a[p][:], sm5[p][:, 2:3], g2b[:], ALU.mult, ALU.mult, [("x1a", p), kp + "rstd", "g2b"], [("h2a", p)])
            k.dma(SH2[row(t), :], h2a[p][:], [("h2a", p)], [("sh2", t)])
            for kc in range(16):
                b = kc // 8
                k.tr(PSB(b)[:, (kc % 8) * 128:(kc % 8 + 1) * 128], h2a[p][:, kc * 128:(kc + 1) * 128], ident_b[:], [("h2a", p), "identb"], psk(b))
            for hh in range(2):
                k.cp("dve" if hh == 0 else "act", h2T[:, hh * 8:(hh + 1) * 8, :], PSB(hh).rearrange("p (a b) -> p a b", b=128),
                     psk(hh), ["h2T"])
            for cb in range(16):
                b = 4 + cb // 4
                for kc in range(16):
                    k.mm(PS(b)[:, (cb % 4) * 128:(cb % 4 + 1) * 128], wq[:, kc, cb * 128:(cb + 1) * 128], h2T[:, kc, :], kc == 0, kc == 15,
                         ["wq", "h2T"], psk(b))
            k.cp("act", qTs[:].rearrange("p a b -> p (a b)"), PS(4, 4), psk(4, 4), ["qTs"])
            for cb in range(16):
                b = 4 + cb // 4
                k.mm(PS(b)[:, (cb % 4) * 128:(cb % 4 + 1) * 128], qTs[:, cb, :], kTf[cb % 2][:], True, True, ["qTs", "kTf"], psk(b))
            k.cp("act", Sxa[p][:].rearrange("p a b -> p (a b)"), PS(4, 4), psk(4, 4), [("Sxa", p)])

        def topk(t):
            eT, gT = eidT_all[:, t, :], gateT_all[:, t, :]
            ek, gk = ("eidT", t), ("gateT", t)
            Sx = Sxa[t % 2]
            sxk = ("Sxa", t % 2)
            for hb_ in range(16):
                k.max8(V16[:, hb_, 0:8], Sx[:, hb_, :], [sxk], ["V16"])
                k.maxidx(I16u[:, hb_, 0:8], V16[:, hb_, 0:8], Sx[:, hb_, :], [sxk, "V16"], ["I16u"])
                k.matchrep(wk[:], V16[:, hb_, 0:8], Sx[:, hb_, :], -3e38, [sxk, "V16"], ["wk"])
                k.max8(V16[:, hb_, 8:16], wk[:], ["wk"], ["V16"])
                k.maxidx(I16u[:, hb_, 8:16], V16[:, hb_, 8:16], wk[:], ["wk", "V16"], ["I16u"])
            k.cp("dve", I16f[:], I16u[:], ["I16u"], ["I16f"])
            k.tt("dve", cand[:], V16v[:, :, 0, :].unsqueeze(3).to_broadcast([128, 8, 16, 16]),
                 V16v[:, :, 1, :].unsqueeze(2).to_broadcast([128, 8, 16, 16]), ALU.add, ["V16"], ["cand"])
            for h in range(8):
                ch = cand[:, h].rearrange("p a b -> p (a b)")
                k.max8(T16[:, h, 0:8], ch, ["cand"], ["T16"])
                k.maxidx(P16u[:, h, 0:8], T16[:, h, 0:8], ch, ["cand", "T16"], ["P16u"])
                k.matchrep(wk2[:], T16[:, h, 0:8], ch, -3e38, ["cand", "T16"], ["wk2"])
                k.max8(T16[:, h, 8:16], wk2[:], ["wk2"], ["T16"])
                k.maxidx(P16u[:, h, 8:16], T16[:, h, 8:16], wk2[:], ["wk2", "T16"], ["P16u"])
            k.op("dve", lambda e: e.tensor_single_scalar(out=A16u[:], in_=P16u[:], scalar=4, op=ALU.logical_shift_right), ["P16u"], ["A16u"])
            k.op("dve", lambda e: e.tensor_single_scalar(out=B16u[:], in_=P16u[:], scalar=15, op=ALU.bitwise_and), ["P16u"], ["B16u"])
            k.cp("dve", af[:], A16u[:], ["A16u"], ["af"])
            k.cp("dve", bf[:], B16u[:], ["B16u"], ["bf"])
            for (pf, col, dst, dk) in ((af, 0, i1f, "i1f"), (bf, 1, i2f, "i2f")):
                k.tt("dve", eqt[:], io_b, pf[:].unsqueeze(3).to_broadcast([128, 8, 16, 16]), ALU.is_equal, ["iota16", "af", "bf"], ["eqt"])
                k.tt("dve", eqt[:], eqt[:], I16v[:, :, col, :].unsqueeze(2).to_broadcast([128, 8, 16, 16]), ALU.mult, ["eqt", "I16f"], ["eqt"])
                k.rsum(dst[:], eqt[:], ["eqt"], [dk])
            k.stt(eid[:], i1f[:], 128.0, i2f[:], ALU.mult, ALU.add, ["i1f", "i2f"], ["eid"])
            k.ts("dve", eid[:], eid[:], 0.0, 16383.0, ALU.max, ALU.min, ["eid"], ["eid"])
            k.tr(PS(2)[:, 0:128], eid[:].rearrange("p a b -> p (a b)"), ident_f[:], ["eid", "identf"], psk(2))
            k.cp("dve", eT, PS(2)[:, 0:128], psk(2), [ek])
            k.tt("dve", ex[:], T16[:], T16[:, :, 0:1].to_broadcast([128, 8, 16]), ALU.subtract, ["T16"], ["ex"])
            k.act(ex[:], ex[:], AF.Exp, ["ex"], ["ex"])
            k.rsum(zs[:], ex[:], ["ex"], ["zs"])
            k.recip(zs[:], zs[:], ["zs"], ["zs"])
            k.tt("dve", gate[:], ex[:], zs[:].unsqueeze(2).to_broadcast([128, 8, 16]), ALU.mult, ["ex", "zs"], ["gate"])
            k.tr(PS(2)[:, 128:256], gate[:].rearrange("p a b -> p (a b)"), ident_f[:], ["gate", "identf"], psk(2))
            k.cp("dve", gT, PS(2)[:, 128:256], psk(2), [gk])


        front(0)
        for t in range(NT):
            if t + 1 < NT:
                front(t + 1)
            topk(t)
        k.release()
        k.barrier()

    if stop_after >= 5:
        k.mark()
        gfb = k.sb([128, D], F32, "gfb")
        SELR = k.sb([128, 128, 128], BF16, "SELR")
        IDm = k.sb([128, 128, 128], BF16, "IDm")
        WSEL = k.sb([128, 128, 128], BF16, "WSEL")
        Ubuf = [k.sb([128, D], F32, "Ubuf") for _ in range(2)]
        Vbuf = [k.sb([128, D], F32, "Vbuf") for _ in range(2)]
        Vbf = [k.sb([128, D], BF16, "Vbf") for _ in range(2)]
        x1t = k.sb([128, D], F32, "x1t")
        yo = k.sb([128, D], F32, "yo")
        h2b = k.sb([128, D], BF16, "h2b")
        jk = k.sb([128, 1024], BF16, "jk")
        A0 = k.sb([128, 128], F32, "A0"); A1 = k.sb([128, 128], F32, "A1")
        At = k.sb([128, 128], F32, "At"); Wt = k.sb([128, 128], F32, "Wt")
        g1 = k.sb([128, 128], F32, "g1"); g2 = k.sb([128, 128], F32, "g2")
        sm6 = k.sb([128, 8], F32, "sm6")

        k.dma(gfb[:], norm_final_g.partition_broadcast(128), [], ["gfb"])
        k.cp("dve", SELR[:], ident_f[:].unsqueeze(2).to_broadcast([128, 128, 128]), ["identf"], ["SELR"])
        idflat = cin["c_ident"].rearrange("a b -> (a b)")
        for c in range(8):
            k.dma(Ubuf[c % 2][:, :], idflat[c * 2048:(c + 1) * 2048].partition_broadcast(128), [], [("G", c % 2)])
            k.cp("dve", IDm[:, c * 16:(c + 1) * 16, :].rearrange("p a b -> p (a b)"), Ubuf[c % 2][:, :], [("G", c % 2)], ["IDm"])
        k.memset("dve", A0[:], 0.0, ["A0", "setup5b"])
        k.memset("dve", A1[:], 0.0, ["A1", "setup5b"])
        gi = [0]

        G = Ubuf + Vbuf + [k.sb([128, D], F32, "Gx") for _ in range(2)]
        NG_ = len(G)
        _unused = None

        def stage1(t):
            eT, ek = eidT_all[:, t, :], ("eidT", t)
            for tk in range(128):
                i = gi[0] % NG_
                gi[0] += 1
                k.op("pool", (lambda e, i=i, tk=tk: e.indirect_dma_start(
                    out=G[i][:, :], out_offset=None, in_=peer_u,
                    in_offset=bass.IndirectOffsetOnAxis(ap=eT[:, tk:tk + 1], axis=0))), [ek, ("gateT", t), "yo", "IDm", "SELR", "setup5b"], [("G", i)], dma=True)
                for c in range(4):
                    k.mm(PS(4 + c), SELR[:, tk, :], h2b[:, c * 512:(c + 1) * 512], True, True, ["SELR", "h2b"], psk(4 + c))
                for hf, Ax in ((0, A0), (1, A1)):
                    k.stt(jk[:, 0:1024], G[i][:, hf * 1024:(hf + 1) * 1024], 1.0, PS(4 + 2 * hf, 2), ALU.mult, ALU.mult,
                          [("G", i)] + psk(4 + 2 * hf, 2), ["jk", "A%d" % hf], accum=Ax[:, tk:tk + 1])

        def post1(t):
            gk = ("gateT", t)
            k.tt("dve", At[:], A0[:], A1[:], ALU.add, ["A0", "A1"], ["At"])
            k.act(g1[:], At[:], AF.Square, ["At"], ["g1"])
            k.stt(g1[:], g1[:], 0.044715, At[:], ALU.mult, ALU.mult, ["g1", "At"], ["g1"])
            k.tt("dve", g1[:], g1[:], At[:], ALU.add, ["g1", "At"], ["g1"])
            k.act(g2[:], g1[:], AF.Tanh, ["g1"], ["g2"], scale=GC)
            k.stt(g2[:], g2[:], 1.0, At[:], ALU.add, ALU.mult, ["g2", "At"], ["g2"])
            k.stt(Wt[:], g2[:], 0.5, gateT_all[:, t, :], ALU.mult, ALU.mult, ["g2", gk], ["Wt"])
            k.tt("dve", WSEL[:], IDm[:], Wt[:].unsqueeze(2).to_broadcast([128, 128, 128]), ALU.mult, ["IDm", "Wt"], ["WSEL"])

        def stage2(t):
            eT, ek = eidT_all[:, t, :], ("eidT", t)
            for tk in range(128):
                i = gi[0] % NG_
                gi[0] += 1
                k.op("pool", (lambda e, i=i, tk=tk: e.indirect_dma_start(
                    out=G[i][:, :], out_offset=None, in_=peer_v,
                    in_offset=bass.IndirectOffsetOnAxis(ap=eT[:, tk:tk + 1], axis=0))), [ek], [("G", i)], dma=True)
                j = tk % 2
                k.cp("act", Vbf[j][:], G[i][:], [("G", i)], [("Vbf", j)])
                for c in range(4):
                    k.mm(PS(c), WSEL[:, tk, :], Vbf[j][:, c * 512:(c + 1) * 512], tk == 0, tk == 127, ["WSEL", ("Vbf", j)], psk(c))

        def final(t):
            k.dma(x1t[:], SX1[row(t), :], [], ["x1t"])
            k.tt("dve", x1t[:], PS(0, 4), x1t[:], ALU.add, psk(0, 4) + ["x1t"], ["x1t"])
            k.act(yo[:], x1t[:], AF.Square, ["x1t"], ["yo", "n6ss"], accum=sm6[:, 4:5])
            k.rstd_from_ss(sm6[:, 4:5], sm6[:, 6:7], sm6[:, 5:6], nh[:], D, "n6")
            k.stt(yo[:], x1t[:], sm6[:, 6:7], gfb[:], ALU.mult, ALU.mult, ["x1t", "n6rstd", "gfb"], ["yo"])
            k.dma(y[row(t), :], yo[:], ["yo"], [("y", t)])

        for t in range(NT):
            k.dma(h2b[:], SH2[row(t), :], [], ["h2b"])
            stage1(t)
            post1(t)
            stage2(t)
            final(t)
        k.release()
        k.release()

    k.emit()
    return nc, k


_CACHE = {}


def make_in_maps(inputs):
    f = lambda a: np.ascontiguousarray(np.asarray(a, dtype=np.float32))
    consts = make_consts()
    shared = {
        "norm_mix_g": f(inputs["norm_mix_g"][0:1]),
        "w_in": f(inputs["w_in"][0]),
        "w_out": f(inputs["w_out"][0]),
        "gm_ln_g": f(inputs["gm_ln_g"][0:1]),
        "gm_ln_b": f(inputs["gm_ln_b"][0:1]),
        "gm_wT": f(np.transpose(np.asarray(inputs["gm_spatial_w"][0]), (2, 0, 1))),
        "gm_bT": f(np.transpose(np.asarray(inputs["gm_spatial_b"][0]), (1, 0))),
        "cmp_posT_k": f(np.asarray(inputs["cmp_pos_k"][0]).T),
        "cmp_posT_v": f(np.asarray(inputs["cmp_pos_v"][0]).T),
        "cmp_w1_k": f(inputs["cmp_w1_k"][0]), "cmp_w1_v": f(inputs["cmp_w1_v"][0]),
        "cmp_w2_k": f(inputs["cmp_w2_k"][0]), "cmp_w2_v": f(inputs["cmp_w2_v"][0]),
        "norm_ffn_g": f(inputs["norm_ffn_g"][0:1]),
        "peer_w_q": f(inputs["peer_w_q"][0]),
        "keys1T": f(np.asarray(inputs["peer_keys1"][0]).T),
        "keys2T": f(np.asarray(inputs["peer_keys2"][0]).T),
        "peer_u": f(inputs["peer_u"][0]),
        "peer_v": f(inputs["peer_v"][0]),
        "norm_final_g": f(np.asarray(inputs["norm_final_g"]).reshape(1, D)),
    }
    shared.update(consts)
    xs = np.asarray(inputs["x"], dtype=np.float32)
    maps = []
    for b in range(8):
        m = dict(shared)
        m["x"] = np.ascontiguousarray(xs[b])
        maps.append(m)
    return maps


def kernel(**inputs):
    if "nc" not in _CACHE:
        _CACHE["nc"] = build_program()[0]
    nc = _CACHE["nc"]
    in_maps = make_in_maps(inputs)
    res = run_bass_kernel_spmd(nc, in_maps, core_ids=list(range(8)))
    out = np.stack([np.asarray(r["y"]) for r in res.results], axis=0)
    return out.astype(np.float32)
```
